# Optimizing a Trainium2 kernel written in Bass

```python
import jax
import jax.numpy as jnp
from jax import lax
import numpy as np


D_MODEL = 2048
BATCH = 4
SEQ = 4096
DEPTH = 2

N_MIXERS = 2
EPS = 1e-6
HG_DK = 128
HG_HEADS = D_MODEL // HG_DK
HG_DV = D_MODEL // HG_HEADS
HG_CHUNK = 64
ATT_HEAD_DIM = 128
ATT_HEADS = D_MODEL // ATT_HEAD_DIM
DIL_BRANCHES = ((128, 1), (512, 4), (2048, 16))
ROPE_THETA = 10000.0
NEG_INF = -1e30
FFN_DENSE = 5504
N_EXPERTS = 8
TOP_K = 2
FFN_EXPERT = 7168
MOE_BLOCK = 512
N_HGRN = (DEPTH + 1) // 2
N_ATTN = DEPTH // 2
N_DENSE = (DEPTH + 1) // 2
N_MOE = DEPTH // 2

kernel_name = 'hybrid_hgrn2_dilated_moe_encoder'


def rms_norm(x, gain):
    xf = x.astype(jnp.float32)
    y = xf * lax.rsqrt(jnp.mean(xf * xf, axis=-1, keepdims=True) + EPS)
    return (y * gain.astype(jnp.float32)).astype(x.dtype)


def rope(t, pos):
    hd = t.shape[-1]
    half = hd // 2
    inv_freq = ROPE_THETA ** (-jnp.arange(half, dtype=jnp.float32) * 2.0 / hd)
    ang = pos[:, None] * inv_freq[None, :]
    cos, sin = jnp.cos(ang), jnp.sin(ang)
    t1, t2 = t[..., :half], t[..., half:]
    return jnp.concatenate([t1 * cos - t2 * sin, t2 * cos + t1 * sin], axis=-1)


def gla_chunk_scan(q, k, v, log_f):
    b, h, l, dk = q.shape
    dv = v.shape[-1]
    c = HG_CHUNK
    n = l // c

    def chunks(t):
        return jnp.moveaxis(t.reshape(b, h, n, c, t.shape[-1]), 2, 0)

    qc, kc, vc = chunks(q), chunks(k), chunks(v)
    g = jnp.cumsum(chunks(log_f), axis=-2)
    incl = jnp.tril(jnp.ones((c, c), dtype=bool))

    def step(state, inp):
        q_i, k_i, v_i, g_i = inp
        o_inter = jnp.einsum('bhtk,bhkv->bhtv', q_i * jnp.exp(g_i), state)
        diff = g_i[:, :, :, None, :] - g_i[:, :, None, :, :]
        decay = jnp.exp(jnp.where(incl[:, :, None], diff, -jnp.inf))
        a = jnp.einsum('bhtk,bhsk,bhtsk->bhts', q_i, k_i, decay)
        o_intra = jnp.einsum('bhts,bhsv->bhtv', a, v_i)
        g_last = g_i[:, :, -1:, :]
        new_state = (jnp.exp(g_last[:, :, 0, :])[..., None] * state
                     + jnp.einsum('bhsk,bhsv->bhkv', k_i * jnp.exp(g_last - g_i), v_i))
        return new_state, o_inter + o_intra

    s0 = jnp.zeros((b, h, dk, dv), jnp.float32)
    _, o = lax.scan(step, s0, (qc, kc, vc, g))
    return jnp.moveaxis(o, 0, 2).reshape(b, h, l, dv)


def hgrn2_mixer(h, w_in, lower_bound, o_gain, w_out):
    b, l, _ = h.shape
    hk = HG_HEADS * HG_DK
    hv = HG_HEADS * HG_DV
    proj = h @ w_in
    q, f_fwd, f_bwd, inp, gate = jnp.split(proj, [hk, 2 * hk, 3 * hk, 3 * hk + hv], axis=-1)

    def heads(t, dh):
        return t.astype(jnp.float32).reshape(b, l, HG_HEADS, dh).transpose(0, 2, 1, 3)

    qh = heads(jax.nn.silu(q.astype(jnp.float32)), HG_DK)
    vh = heads(inp, HG_DV)

    def direction(f_pre, lb):
        f = lb + (1.0 - lb) * jax.nn.sigmoid(f_pre.astype(jnp.float32))
        return heads(1.0 - f, HG_DK), heads(jnp.log(f), HG_DK)

    k_f, lf_f = direction(f_fwd, lower_bound[0])
    k_b, lf_b = direction(f_bwd, lower_bound[1])
    o_f = gla_chunk_scan(qh, k_f, vh, lf_f)
    flip = lambda t: jnp.flip(t, axis=2)
    o_b = flip(gla_chunk_scan(flip(qh), flip(k_b), flip(vh), flip(lf_b)))
    o = (o_f + o_b).transpose(0, 2, 1, 3)
    o = rms_norm(o, o_gain.reshape(HG_HEADS, HG_DV)).reshape(b, l, hv)
    o = o * jax.nn.silu(gate.astype(jnp.float32))
    return o.astype(h.dtype) @ w_out


def dilated_branch(q, k, v, dilation, steps):
    b, h, l, hd = q.shape
    r = dilation
    w = steps
    n = l // r

    def to_res(t):
        return t.reshape(b, h, n, r, hd).transpose(0, 1, 3, 2, 4)

    qr, kr, vr = to_res(q), to_res(k), to_res(v)
    nb = -(-n // w)
    npad = nb * w
    qb = jnp.pad(qr, ((0, 0), (0, 0), (0, 0), (0, npad - n), (0, 0))).reshape(b, h, r, nb, w, hd)

    def windows(t):
        tp = jnp.pad(t, ((0, 0), (0, 0), (0, 0), (w, npad - n + w), (0, 0))).reshape(b, h, r, nb + 2, w, hd)
        return jnp.concatenate([tp[:, :, :, 0:nb], tp[:, :, :, 1:nb + 1], tp[:, :, :, 2:nb + 2]], axis=4)

    kw, vw = windows(kr), windows(vr)
    s = jnp.einsum('bhrnqd,bhrnkd->bhrnqk', qb, kw)
    qq = jnp.arange(w)[:, None]
    kk = jnp.arange(3 * w)[None, :]
    band = (kk >= qq) & (kk <= qq + 2 * w)
    keypos = jnp.arange(nb)[:, None] * w - w + jnp.arange(3 * w)[None, :]
    inrange = (keypos >= 0) & (keypos < n)
    mask = band[None, :, :] & inrange[:, None, :]
    s = jnp.where(mask, s, NEG_INF)
    m = jnp.max(s, axis=-1, keepdims=True)
    p = jnp.exp(s - m)
    den = jnp.sum(p, axis=-1, keepdims=True)
    o = jnp.einsum('bhrnqk,bhrnkd->bhrnqd', p, vw) / den
    lse = (m + jnp.log(den))[..., 0]
    o = o.reshape(b, h, r, npad, hd)[:, :, :, :n].transpose(0, 1, 3, 2, 4).reshape(b, h, l, hd)
    lse = lse.reshape(b, h, r, npad)[:, :, :, :n].transpose(0, 1, 3, 2).reshape(b, h, l)
    return o, lse


def dilated_attention_mixer(h, w_qkv, q_gain, k_gain, w_out):
    b, l, d = h.shape
    proj = h @ w_qkv
    q, k, v = jnp.split(proj, 3, axis=-1)

    def heads(t):
        return t.astype(jnp.float32).reshape(b, l, ATT_HEADS, ATT_HEAD_DIM).transpose(0, 2, 1, 3)

    pos = jnp.arange(l, dtype=jnp.float32)
    qh = rope(rms_norm(heads(q), q_gain), pos) * (ATT_HEAD_DIM ** -0.5)
    kh = rope(rms_norm(heads(k), k_gain), pos)
    vh = heads(v)
    outs, lses = [], []
    for window, dil in DIL_BRANCHES:
        o_br, lse_br = dilated_branch(qh, kh, vh, dil, window // (2 * dil))
        outs.append(o_br)
        lses.append(lse_br)
    wts = jax.nn.softmax(jnp.stack(lses, axis=0), axis=0)
    o = jnp.sum(wts[..., None] * jnp.stack(outs, axis=0), axis=0)
    o = o.transpose(0, 2, 1, 3).reshape(b, l, d)
    return o.astype(h.dtype) @ w_out


def swiglu(h, w_gate, w_up, w_down):
    return (jax.nn.silu(h @ w_gate) * (h @ w_up)) @ w_down


def moe_swiglu(h, w_router, w_gate, w_up, w_down):
    b, l, d = h.shape
    t = b * l
    xf = h.reshape(t, d)
    logits = (xf @ w_router).astype(jnp.float32)
    top_logit, top_e = lax.top_k(logits, TOP_K)
    gates = jax.nn.softmax(top_logit, axis=-1)
    e_flat = top_e.reshape(-1).astype(jnp.int32)
    g_flat = gates.reshape(-1)
    tok_flat = jnp.repeat(jnp.arange(t, dtype=jnp.int32), TOP_K)
    order = jnp.argsort(e_flat, stable=True)
    se, stok, sg = e_flat[order], tok_flat[order], g_flat[order]
    counts = jax.ops.segment_sum(jnp.ones_like(e_flat), e_flat, num_segments=N_EXPERTS)
    padded = (counts + MOE_BLOCK - 1) // MOE_BLOCK * MOE_BLOCK
    pend = jnp.cumsum(padded)
    pstart = pend - padded
    ustart = jnp.cumsum(counts) - counts
    dest = pstart[se] + (jnp.arange(t * TOP_K, dtype=jnp.int32) - ustart[se])
    nblk = -(-(t * TOP_K) // MOE_BLOCK) + N_EXPERTS
    p = nblk * MOE_BLOCK
    slot_tok = jnp.zeros((p,), jnp.int32).at[dest].set(stok)
    slot_gate = jnp.zeros((p,), jnp.float32).at[dest].set(sg)
    blk_start = jnp.arange(nblk, dtype=pend.dtype) * MOE_BLOCK
    blk_e = jnp.minimum(jnp.searchsorted(pend, blk_start, side='right'), N_EXPERTS - 1)

    def expert_block(args):
        e, toks, g = args
        xb = xf[toks]
        y = swiglu(xb, w_gate[e], w_up[e], w_down[e])
        return y * g[:, None].astype(y.dtype)

    y = lax.map(expert_block, (blk_e, slot_tok.reshape(nblk, MOE_BLOCK), slot_gate.reshape(nblk, MOE_BLOCK)))
    out = jnp.zeros((t, d), h.dtype).at[slot_tok].add(y.reshape(p, d).astype(h.dtype))
    return out.reshape(b, l, d)


def setup_inputs(seed: int = 0) -> dict:
    key = jax.random.key(seed)
    ks = jax.random.split(key, 18)
    f32 = jnp.float32
    hk = HG_HEADS * HG_DK
    hv = HG_HEADS * HG_DV

    def nrm(k, shape, fan_in):
        return jax.random.normal(k, shape, f32) * (fan_in ** -0.5)

    def gain(k, shape):
        return 1.0 + 0.02 * jax.random.normal(k, shape, f32)

    return {
        'x': jax.random.normal(ks[0], (BATCH, SEQ, D_MODEL), f32),
        'norm_gains': gain(ks[1], (DEPTH, 2, D_MODEL)),
        'hgrn_w_in': nrm(ks[2], (N_HGRN, D_MODEL, 3 * hk + 2 * hv), D_MODEL),
        'hgrn_lb_logits': 0.5 * jax.random.normal(ks[3], (DEPTH + 1, 2, hk), f32),
        'hgrn_onorm': gain(ks[4], (N_HGRN, hv)),
        'hgrn_w_out': nrm(ks[5], (N_HGRN, hv, D_MODEL), hv),
        'attn_w_qkv': nrm(ks[6], (N_ATTN, D_MODEL, 3 * D_MODEL), D_MODEL),
        'attn_q_gain': gain(ks[7], (N_ATTN, ATT_HEAD_DIM)),
        'attn_k_gain': gain(ks[8], (N_ATTN, ATT_HEAD_DIM)),
        'attn_w_out': nrm(ks[9], (N_ATTN, D_MODEL, D_MODEL), D_MODEL),
        'ffn_w_gate': nrm(ks[10], (N_DENSE, D_MODEL, FFN_DENSE), D_MODEL),
        'ffn_w_up': nrm(ks[11], (N_DENSE, D_MODEL, FFN_DENSE), D_MODEL),
        'ffn_w_down': nrm(ks[12], (N_DENSE, FFN_DENSE, D_MODEL), FFN_DENSE),
        'moe_w_router': nrm(ks[13], (N_MOE, D_MODEL, N_EXPERTS), D_MODEL),
        'moe_w_gate': nrm(ks[14], (N_MOE, N_EXPERTS, D_MODEL, FFN_EXPERT), D_MODEL),
        'moe_w_up': nrm(ks[15], (N_MOE, N_EXPERTS, D_MODEL, FFN_EXPERT), D_MODEL),
        'moe_w_down': nrm(ks[16], (N_MOE, N_EXPERTS, FFN_EXPERT, D_MODEL), FFN_EXPERT),
    }


def reference(x, norm_gains, hgrn_w_in, hgrn_lb_logits, hgrn_onorm, hgrn_w_out,
              attn_w_qkv, attn_q_gain, attn_k_gain, attn_w_out,
              ffn_w_gate, ffn_w_up, ffn_w_down,
              moe_w_router, moe_w_gate, moe_w_up, moe_w_down):
    lb_all = jnp.cumsum(jax.nn.softmax(hgrn_lb_logits.astype(jnp.float32), axis=0), axis=0)
    for i in range(DEPTH):
        j = i // N_MIXERS
        hn = rms_norm(x, norm_gains[i, 0])
        if i % N_MIXERS == 0:
            mix = hgrn2_mixer(hn, hgrn_w_in[j], lb_all[i], hgrn_onorm[j], hgrn_w_out[j])
        else:
            mix = dilated_attention_mixer(hn, attn_w_qkv[j], attn_q_gain[j], attn_k_gain[j], attn_w_out[j])
        x = x + mix.astype(x.dtype)
        hn = rms_norm(x, norm_gains[i, 1])
        jf = i // 2
        if i % 2 == 0:
            ff = swiglu(hn, ffn_w_gate[jf], ffn_w_up[jf], ffn_w_down[jf])
        else:
            ff = moe_swiglu(hn, moe_w_router[jf], moe_w_gate[jf], moe_w_up[jf], moe_w_down[jf])
        x = x + ff.astype(x.dtype)
    return x
```

```python
import numpy as np
from contextlib import ExitStack
import concourse.bass as bass
import concourse.mybir as mybir

F32 = mybir.dt.float32
BF16 = mybir.dt.bfloat16
I32 = mybir.dt.int32
U32 = mybir.dt.uint32
AF = mybir.ActivationFunctionType
ALU = mybir.AluOpType
AX = mybir.AxisListType


class Tok:
    __slots__ = ("w", "r", "name")

    def __init__(self, name=""):
        self.w = {}
        self.r = {}
        self.name = name


class Eng:
    def __init__(self, raw, sem, name):
        self.raw = raw
        self.sem = sem
        self.name = name
        self.count = 0
        self.seen = {}


class FW:
    def __init__(self, nc, es, n_dma_sems=24, n_gdma_sems=12):
        self.nc = nc
        self.es = es
        mk = lambda n: es.enter_context(nc.semaphore(n))
        self.pe = Eng(nc.tensor, mk("s_pe"), "pe")
        self.act = Eng(nc.scalar, mk("s_act"), "act")
        self.dve = Eng(nc.vector, mk("s_dve"), "dve")
        self.pool = Eng(nc.gpsimd, mk("s_pool"), "pool")
        self.sp = Eng(nc.sync, mk("s_sp"), "sp")
        self.engs = [self.pe, self.act, self.dve, self.pool, self.sp]
        self.dsems = {"sp": [[mk(f"d_sp{i}"), 0] for i in range(n_dma_sems)],
                      "pool": [[mk(f"d_pl{i}"), 0] for i in range(n_gdma_sems)],
                      "act": [[mk(f"d_ac{i}"), 0] for i in range(8)]}
        self.drr = {"sp": 0, "pool": 0, "act": 0}
        self.ninst = 0

    def _wait(self, eng, sem, val):
        key = id(sem)
        if eng.seen.get(key, 0) >= val:
            return
        eng.raw.wait_ge(sem, val)
        eng.seen[key] = val
        self.ninst += 1

    def _deps(self, eng, reads, writes, skip_self=False):
        for t in reads:
            for k, (s, v) in t.w.items():
                if skip_self and s is eng.sem:
                    continue
                self._wait(eng, s, v)
        for t in writes:
            for k, (s, v) in t.w.items():
                if skip_self and s is eng.sem:
                    continue
                self._wait(eng, s, v)
            for k, (s, v) in t.r.items():
                if skip_self and s is eng.sem:
                    continue
                self._wait(eng, s, v)

    def _record(self, ev, reads, writes, accumulate=False):
        s, v = ev
        for t in writes:
            if not accumulate:
                t.w = {}
            t.w[id(s)] = (s, v)
            t.r = {}
        for t in reads:
            t.r[id(s)] = (s, v)

    def op(self, eng, fn, reads=(), writes=(), skip_self=False, acc=False):
        self._deps(eng, reads, writes, skip_self=skip_self)
        ins = fn()
        eng.count += 1
        ins.then_inc(eng.sem, 1)
        self.ninst += 1
        self._record((eng.sem, eng.count), reads, writes, accumulate=acc)
        return ins

    def mm_group(self, fns, reads=(), writes=()):
        eng = self.pe
        self._deps(eng, reads, writes, skip_self=True)
        ins = None
        for f in fns:
            ins = f()
            self.ninst += 1
        eng.count += 1
        ins.then_inc(eng.sem, 1)
        self._record((eng.sem, eng.count), reads, writes)

    def dma(self, q, out, in_, reads=(), writes=(), acc=False, **kw):
        eng = {"sp": self.sp, "pool": self.pool, "act": self.act}[q]
        pool = self.dsems[q]
        j = self.drr[q]
        self.drr[q] = (j + 1) % len(pool)
        sem, tot = pool[j]
        if tot:
            self._wait(eng, sem, tot)
        self._deps(eng, reads, writes)
        ins = eng.raw.dma_start(out=out, in_=in_, **kw)
        tot += 16
        pool[j][1] = tot
        ins.then_inc(sem, 16)
        self.ninst += 1
        self._record((sem, tot), reads, writes, accumulate=acc)
        return ins

    def all_events(self):
        evs = [(e.sem, e.count) for e in self.engs if e.count]
        for q in self.dsems:
            for sem, tot in self.dsems[q]:
                if tot:
                    evs.append((sem, tot))
        return evs

    def barrier(self, engines=None):
        evs = self.all_events()
        for e in (engines or self.engs):
            for s, v in evs:
                if s is e.sem:
                    continue
                self._wait(e, s, v)

    def final_wait(self, toks):
        for t in toks:
            for k, (s, v) in t.w.items():
                self._wait(self.sp, s, v)


import numpy as np
import ml_dtypes

D = 2048
EPS = 1e-6
FF = 5504
NFC = FF // 128


class Ctx:
    pass


_UC = [0]


def U(n):
    _UC[0] += 1
    return f"{n}_{_UC[0]}"


def make_consts():
    c = {}
    c["ident"] = np.eye(128, dtype=np.float32).astype(ml_dtypes.bfloat16)
    s = np.arange(128)[:, None]
    t = np.arange(128)[None, :]
    same = (s // 64) == (t // 64)
    c["m2f"] = (same & (s <= t)).astype(np.float32).astype(ml_dtypes.bfloat16)
    c["m2b"] = (same & (s >= t)).astype(np.float32).astype(ml_dtypes.bfloat16)
    return c


def psum_pools(nc, es, fw):
    P = Ctx()
    P.f32 = [es.enter_context(nc.psum_tensor(f"psf{i}", [128, 512], F32)) for i in range(6)]
    P.f32t = [Tok(f"psf{i}") for i in range(6)]
    P.bf = [es.enter_context(nc.psum_tensor(f"psb{i}", [128, 1024], BF16)) for i in range(2)]
    P.bft = [Tok(f"psb{i}") for i in range(2)]
    P.i = 0
    P.j = 0
    return P


def get_ps(P):
    k = P.i % len(P.f32)
    P.i += 1
    return P.f32[k], P.f32t[k]


def get_psb(P):
    k = P.j % len(P.bf)
    P.j += 1
    return P.bf[k], P.bft[k]


def cast_weight(fw, nc, dst, src, rows, cols, tok):
    for r in range(0, rows, 128):
        for c in range(0, cols, 2048):
            w = min(2048, cols - c)
            fw.dma("pool", dst[r:r + 128, c:c + w], src[r:r + 128, c:c + w], writes=[tok], acc=True)


def norm_T(fw, nc, P, S, x_src, row0, tt, gain_bc, t_gain):
    k = S.xi % 2
    S.xi += 1
    xt, t_x = S.xt[k], S.t_xt[k]
    fw.dma("sp", xt[:], x_src[row0:row0 + 128, :], writes=[t_x])
    fw.op(fw.act, lambda: nc.scalar.activation(out=S.junk[:], in_=xt[:], func=AF.Square),
          reads=[t_x], writes=[S.t_junk])
    fw.op(fw.dve, lambda: nc.vector.tensor_reduce(out=S.ss[:, 0:1], in_=S.junk[:], axis=AX.X, op=ALU.add),
          reads=[S.t_junk], writes=[S.t_ss])
    fw.op(fw.act, lambda: nc.scalar.activation(out=S.ss[:, 1:2], in_=S.ss[:, 0:1], func=AF.Sqrt,
                                               scale=1.0 / D, bias=S.eps[:, 0:1]),
          reads=[S.t_ss, S.t_eps], writes=[S.t_ss2])
    fw.op(fw.dve, lambda: nc.vector.reciprocal(out=S.ss[:, 2:3], in_=S.ss[:, 1:2]),
          reads=[S.t_ss2], writes=[S.t_rstd])
    fw.op(fw.dve, lambda: nc.vector.scalar_tensor_tensor(out=S.hn[:], in0=xt[:], scalar=S.ss[:, 2:3],
                                                         in1=gain_bc[:], op0=ALU.mult, op1=ALU.mult),
          reads=[t_x, S.t_rstd, t_gain], writes=[S.t_hn])
    for g in range(4):
        pb, t_pb = get_psb(P)
        fns = []
        for j in range(4):
            c = 4 * g + j
            fns.append(lambda j=j, c=c: nc.tensor.transpose(out=pb[:, j * 128:(j + 1) * 128],
                                                            in_=S.hn[:, c * 128:(c + 1) * 128],
                                                            identity=S.ident[:]))
        fw.mm_group(fns, reads=[S.t_hn, S.t_ident], writes=[t_pb])
        dst = S.hnT[:, 4 * g:4 * g + 4, tt * 128:(tt + 1) * 128]
        src = pb[:, 0:512].rearrange("p (j t) -> p j t", t=128)
        if g % 2 == 0:
            fw.op(fw.act, lambda: nc.scalar.copy(out=dst, in_=src), reads=[t_pb], writes=[S.t_hnT], acc=True)
        else:
            fw.op(fw.dve, lambda: nc.vector.tensor_copy(out=dst, in_=src), reads=[t_pb], writes=[S.t_hnT], acc=True)


def alloc_norm(nc, es, S, consts_ap):
    S.xt = [es.enter_context(nc.sbuf_tensor(U(f"xt{i}"), [128, D], F32)) for i in range(2)]
    S.t_xt = [Tok("xt0"), Tok("xt1")]
    S.xi = 0
    S.junk = es.enter_context(nc.sbuf_tensor(U("junk"), [128, D], F32)); S.t_junk = Tok("junk")
    S.ss = es.enter_context(nc.sbuf_tensor(U("ss"), [128, 4], F32))
    S.t_ss = Tok("ss"); S.t_ss2 = Tok("ss2"); S.t_rstd = Tok("rstd")
    S.hn = es.enter_context(nc.sbuf_tensor(U("hn"), [128, D], BF16)); S.t_hn = Tok("hn")
    S.hnT = es.enter_context(nc.sbuf_tensor(U("hnT"), [128, 16, 512], BF16)); S.t_hnT = Tok("hnT")


def alloc_common(nc, es, fw, S, consts):
    S.eps = es.enter_context(nc.sbuf_tensor(U("eps"), [128, 1], F32)); S.t_eps = Tok("eps")
    fw.op(fw.dve, lambda: nc.vector.memset(S.eps[:], EPS), writes=[S.t_eps])
    S.ident = es.enter_context(nc.sbuf_tensor(U("ident"), [128, 128], BF16)); S.t_ident = Tok("ident")
    fw.dma("sp", S.ident[:], consts["ident"][:, :], writes=[S.t_ident])
    S.gain = es.enter_context(nc.sbuf_tensor(U("gainbc"), [128, D], F32)); S.t_gain = Tok("gain")


def load_gain(fw, nc, S, gains, idx):
    fw.dma("sp", S.gain[:], gains[idx].partition_broadcast(128), writes=[S.t_gain])


def phase_hgrn_proj(fw, nc, P, S, A, L):
    with ExitStack() as es:
        alloc_norm(nc, es, S, None)
        wsl = [es.enter_context(nc.sbuf_tensor(U(f"wsl{i}"), [128, 16, 512], BF16)) for i in range(2)]
        t_wsl = [Tok("wsl0"), Tok("wsl1")]
        ob = [es.enter_context(nc.sbuf_tensor(U(f"ob{i}"), [128, 512], F32)) for i in range(3)]
        t_ob = [Tok(f"ob{i}") for i in range(3)]
        ob16 = [es.enter_context(nc.sbuf_tensor(U(f"obh{i}"), [128, 512], BF16)) for i in range(2)]
        t_ob16 = [Tok(f"obh{i}") for i in range(2)]
        load_gain(fw, nc, S, A.gains, 0)
        win = A.w_in_bf.rearrange("(c p) n -> p c n", p=128)
        oi = 0
        wi = 0
        for tb in range(L // 512):
            for tt in range(4):
                norm_T(fw, nc, P, S, A.x, tb * 512 + tt * 128, tt, S.gain, S.t_gain)
            for s in range(20):
                w, t_w = wsl[wi % 2], t_wsl[wi % 2]
                wi += 1
                fw.dma("sp", w[:], win[:, :, s * 512:(s + 1) * 512], reads=[A.t_w_in], writes=[t_w])
                tokmajor = 12 <= s < 16
                for j in range(4):
                    ps, t_ps = get_ps(P)
                    if not tokmajor:
                        fns = [lambda c=c: nc.tensor.matmul(ps[:, :], lhsT=w[:, c, j * 128:(j + 1) * 128],
                                                            rhs=S.hnT[:, c, :], start=(c == 0), stop=(c == 15))
                               for c in range(16)]
                        fw.mm_group(fns, reads=[t_w, S.t_hnT], writes=[t_ps])
                        o, t_o = ob[oi % 3], t_ob[oi % 3]
                        oi += 1
                        if oi % 2:
                            fw.op(fw.act, lambda: nc.scalar.copy(out=o[:], in_=ps[:, :]), reads=[t_ps], writes=[t_o])
                        else:
                            fw.op(fw.dve, lambda: nc.vector.tensor_copy(out=o[:], in_=ps[:, :]), reads=[t_ps], writes=[t_o])
                        srow = s if s < 12 else s - 4
                        r0 = srow * 512 + j * 128
                        fw.dma("sp", A.projT[r0:r0 + 128, tb * 512:(tb + 1) * 512], o[:], reads=[t_o],
                               writes=[A.t_projT], acc=True)
                    else:
                        tt = j
                        fns = [lambda c=c: nc.tensor.matmul(ps[:, :], lhsT=S.hnT[:, c, tt * 128:(tt + 1) * 128],
                                                            rhs=w[:, c, :], start=(c == 0), stop=(c == 15))
                               for c in range(16)]
                        fw.mm_group(fns, reads=[t_w, S.t_hnT], writes=[t_ps])
                        o, t_o = ob16[oi % 2], t_ob16[oi % 2]
                        oi += 1
                        if oi % 2:
                            fw.op(fw.act, lambda: nc.scalar.copy(out=o[:], in_=ps[:, :]), reads=[t_ps], writes=[t_o])
                        else:
                            fw.op(fw.dve, lambda: nc.vector.tensor_copy(out=o[:], in_=ps[:, :]), reads=[t_ps], writes=[t_o])
                        r0 = tb * 512 + tt * 128
                        fw.dma("sp", A.v_tm[r0:r0 + 128, (s - 12) * 512:(s - 11) * 512], o[:], reads=[t_o],
                               writes=[A.t_v_tm], acc=True)
        fw.barrier()


def phase_hgrn_scan(fw, nc, P, S, A, L):
    NT = L // 128
    NCH = L // 64
    with ExitStack() as es:
        sb = lambda n, shp, dt: es.enter_context(nc.sbuf_tensor(U(n), shp, dt))
        lbl = sb("lbl", [128, 3, 2, 16], F32); t_lbl = Tok("lbl")
        with nc.allow_non_contiguous_dma(reason="tiny param load"):
            for l in range(3):
                for d in range(2):
                    fw.dma("sp", lbl[:, l, d, :], A.lb_logits[l, d].rearrange("(h k) -> k h", k=128),
                           writes=[t_lbl], acc=True)
        ong = sb("ong", [128, 16], F32); t_ong = Tok("ong")
        with nc.allow_non_contiguous_dma(reason="tiny param load"):
            fw.dma("sp", ong[:], A.onorm.rearrange("(h k) -> k h", k=128), writes=[t_ong])
        lbe = sb("lbe", [128, 3, 32], F32); t_lbe = Tok("lbe")
        fw.op(fw.act, lambda: nc.scalar.activation(out=lbe[:], in_=lbl[:].rearrange("p l d h -> p l (d h)"), func=AF.Exp),
              reads=[t_lbl], writes=[t_lbe])
        lbs = sb("lbs", [128, 4, 32], F32); t_lbs = Tok("lbs")
        fw.op(fw.dve, lambda: nc.vector.tensor_tensor(out=lbs[:, 0, :], in0=lbe[:, 0, :], in1=lbe[:, 1, :], op=ALU.add),
              reads=[t_lbe], writes=[t_lbs])
        fw.op(fw.dve, lambda: nc.vector.tensor_tensor(out=lbs[:, 1, :], in0=lbs[:, 0, :], in1=lbe[:, 2, :], op=ALU.add),
              reads=[t_lbe, t_lbs], writes=[t_lbs])
        fw.op(fw.dve, lambda: nc.vector.reciprocal(out=lbs[:, 0, :], in_=lbs[:, 1, :]), reads=[t_lbs], writes=[t_lbs])
        fw.op(fw.dve, lambda: nc.vector.tensor_tensor(out=lbs[:, 2, :], in0=lbe[:, 0, :], in1=lbs[:, 0, :], op=ALU.mult),
              reads=[t_lbe, t_lbs], writes=[t_lbs])
        fw.op(fw.dve, lambda: nc.vector.tensor_scalar(out=lbs[:, 3, :], in0=lbs[:, 2, :], scalar1=-1.0, scalar2=1.0,
                                                      op0=ALU.mult, op1=ALU.add), reads=[t_lbs], writes=[t_lbs])
        rmask = sb("rmask", [128, L], BF16); t_rmask = Tok("rmask")
        fw.op(fw.dve, lambda: nc.vector.memset(rmask[:], 1.0), writes=[t_rmask])
        fw.op(fw.dve, lambda: nc.vector.memset(rmask[:, 0::64], 0.0), writes=[t_rmask])
        m2 = [sb("m2f", [128, 128], BF16), sb("m2b", [128, 128], BF16)]
        t_m2 = Tok("m2")
        fw.dma("sp", m2[0][:], A.consts["m2f"][:, :], writes=[t_m2], acc=True)
        fw.dma("sp", m2[1][:], A.consts["m2b"][:, :], writes=[t_m2], acc=True)
        ones = sb("ones", [128, 128], F32); t_ones = Tok("ones")
        fw.op(fw.dve, lambda: nc.vector.memset(ones[:], 1.0), writes=[t_ones])
        qs = sb("qs", [128, L], F32); t_qs = Tok("qs")
        w1 = sb("w1", [128, L], F32); t_w1 = Tok("w1")
        w2 = sb("w2", [128, L], F32); t_w2 = Tok("w2")
        w3 = sb("w3", [128, L], F32); t_w3 = Tok("w3")
        qt = [sb("qt0", [128, L], BF16), sb("qt1", [128, L], BF16)]; t_qt = [Tok("qt0"), Tok("qt1")]
        kt = [sb("kt0", [128, L], BF16), sb("kt1", [128, L], BF16)]; t_kt = [Tok("kt0"), Tok("kt1")]
        etot = [sb("etot0", [128, NCH], F32), sb("etot1", [128, NCH], F32)]; t_etot = [Tok("et0"), Tok("et1")]
        vtm = sb("vtm", [128, NT, 128], BF16); t_vtm = Tok("vtm")
        ktm = [sb("ktm0", [128, NT, 128], BF16), sb("ktm1", [128, NT, 128], BF16)]; t_ktm = [Tok("ktm0"), Tok("ktm1")]
        osum = sb("osum", [128, L], F32); t_osum = Tok("osum")
        Sf = [sb("Sf0", [128, 128], F32), sb("Sf1", [128, 128], F32)]; t_Sf = [Tok("Sf0"), Tok("Sf1")]
        Sb = [sb("Sb0", [128, 128], BF16), sb("Sb1", [128, 128], BF16)]; t_Sb = [Tok("Sb0"), Tok("Sb1")]
        tmp = [sb("tmp0", [128, 128], F32), sb("tmp1", [128, 128], F32)]; t_tmp = [Tok("tmp0"), Tok("tmp1")]
        atm = [sb("atm0", [128, 128], BF16), sb("atm1", [128, 128], BF16)]; t_atm = [Tok("atm0"), Tok("atm1")]
        oTb = sb("oTb", [128, L], BF16); t_oTb = Tok("oTb")

        for h in range(16):
            r = h * 128
            fw.dma("sp", qs[:], A.projT[r:r + 128, :], reads=[A.t_projT], writes=[t_qs])
            fw.dma("sp", vtm[:], A.v_tm[:, r:r + 128].rearrange("(n p) v -> p n v", p=128), reads=[A.t_v_tm],
                   writes=[t_vtm])
            fw.op(fw.act, lambda: nc.scalar.activation(out=qs[:], in_=qs[:], func=AF.Silu), reads=[t_qs], writes=[t_qs])
            for d in range(2):
                lbc = lbs[:, 2, d * 16 + h:d * 16 + h + 1]
                omc = lbs[:, 3, d * 16 + h:d * 16 + h + 1]
                fw.dma("sp", w1[:], A.projT[2048 * (d + 1) + r:2048 * (d + 1) + r + 128, :], reads=[A.t_projT], writes=[t_w1])
                fw.op(fw.act, lambda: nc.scalar.activation(out=w1[:], in_=w1[:], func=AF.Sigmoid),
                      reads=[t_w1], writes=[t_w1])
                fw.op(fw.dve, lambda: nc.vector.tensor_scalar(out=w1[:], in0=w1[:], scalar1=omc, scalar2=lbc,
                                                              op0=ALU.mult, op1=ALU.add), reads=[t_w1, t_lbs], writes=[t_w1])
                fw.op(fw.act, lambda: nc.scalar.activation(out=w2[:], in_=w1[:], func=AF.Ln), reads=[t_w1], writes=[t_w2])
                fw.op(fw.dve, lambda: nc.vector.tensor_scalar(out=w1[:], in0=w1[:], scalar1=-1.0, scalar2=1.0,
                                                              op0=ALU.mult, op1=ALU.add), reads=[t_w1], writes=[t_w1])
                fw.op(fw.dve, lambda: nc.vector.tensor_tensor_scan(out=w3[:], data0=rmask[:], data1=w2[:], initial=0.0,
                                                                   op0=ALU.mult, op1=ALU.add),
                      reads=[t_rmask, t_w2], writes=[t_w3])
                fw.op(fw.act, lambda: nc.scalar.activation(out=etot[d][:], in_=w3[:, 63::64], func=AF.Exp),
                      reads=[t_w3], writes=[t_etot[d]])
                if d == 1:
                    fw.op(fw.dve, lambda: nc.vector.tensor_tensor(out=w3[:], in0=w3[:], in1=w2[:], op=ALU.subtract),
                          reads=[t_w3, t_w2], writes=[t_w3])
                sgn = 1.0 if d == 0 else -1.0
                fw.op(fw.act, lambda: nc.scalar.activation(out=w2[:], in_=w3[:], func=AF.Exp, scale=sgn),
                      reads=[t_w3], writes=[t_w2])
                fw.op(fw.dve, lambda: nc.vector.tensor_tensor(out=qt[d][:], in0=qs[:], in1=w2[:], op=ALU.mult),
                      reads=[t_qs, t_w2], writes=[t_qt[d]])
                fw.op(fw.act, lambda: nc.scalar.activation(out=w2[:], in_=w3[:], func=AF.Exp, scale=-sgn),
                      reads=[t_w3], writes=[t_w2])
                fw.op(fw.dve, lambda: nc.vector.tensor_tensor(out=kt[d][:], in0=w1[:], in1=w2[:], op=ALU.mult),
                      reads=[t_w1, t_w2], writes=[t_kt[d]])
                for g in range(0, NT, 4):
                    pb, t_pb = get_psb(P)
                    fns = [lambda j=j: nc.tensor.transpose(out=pb[:, j * 128:(j + 1) * 128],
                                                           in_=kt[d][:, (g + j) * 128:(g + j + 1) * 128],
                                                           identity=S.ident[:]) for j in range(4)]
                    fw.mm_group(fns, reads=[t_kt[d], S.t_ident], writes=[t_pb])
                    fw.op(fw.act, lambda: nc.scalar.copy(out=ktm[d][:, g:g + 4, :],
                                                         in_=pb[:, 0:512].rearrange("p (j t) -> p j t", t=128)),
                          reads=[t_pb], writes=[t_ktm[d]], acc=True)
                fw.op(fw.dve, lambda: nc.vector.memset(Sf[d][:], 0.0), writes=[t_Sf[d]])
                fw.op(fw.dve, lambda: nc.vector.memset(Sb[d][:], 0.0), writes=[t_Sb[d]])

            for i in range(NT):
                for d in range(2):
                    n = i if d == 0 else NT - 1 - i
                    c0 = n * 128
                    ps, t_ps = get_ps(P)
                    fw.mm_group([lambda: nc.tensor.matmul(ps[:, 0:128], lhsT=kt[d][:, c0:c0 + 128],
                                                          rhs=qt[d][:, c0:c0 + 128], start=True, stop=True)],
                                reads=[t_kt[d], t_qt[d]], writes=[t_ps])
                    fw.op(fw.dve, lambda: nc.vector.tensor_tensor(out=atm[d][:], in0=ps[:, 0:128], in1=m2[d][:], op=ALU.mult),
                          reads=[t_ps, t_m2], writes=[t_atm[d]])
                    po, t_po = get_ps(P)
                    fw.mm_group([lambda: nc.tensor.matmul(po[:, 0:128], lhsT=vtm[:, n, :], rhs=atm[d][:],
                                                          start=True, stop=False)],
                                reads=[t_vtm, t_atm[d]], writes=[t_po])
                    order = [0, 1] if d == 0 else [1, 0]
                    for ci, half in enumerate(order):
                        ch = 2 * n + half
                        cs = c0 + half * 64
                        rows = slice(half * 64, half * 64 + 64)
                        last = (ci == 1)
                        if d == 0:
                            fw.mm_group([lambda: nc.tensor.matmul(po[:, half * 64:half * 64 + 64], lhsT=Sb[d][:],
                                                                  rhs=qt[d][:, cs:cs + 64], start=False, stop=last)],
                                        reads=[t_Sb[d], t_qt[d]], writes=[t_po])
                            pk, t_pk = get_ps(P)
                            fw.mm_group([lambda: nc.tensor.matmul(pk[:, 0:128], lhsT=ktm[d][rows, n, :],
                                                                  rhs=vtm[rows, n, :], start=True, stop=True)],
                                        reads=[t_ktm[d], t_vtm], writes=[t_pk])
                            fw.op(fw.dve, lambda: nc.vector.tensor_tensor(out=tmp[d][:], in0=pk[:, 0:128], in1=Sf[d][:], op=ALU.add),
                                  reads=[t_pk, t_Sf[d]], writes=[t_tmp[d]])
                            fw.op(fw.dve, lambda: nc.vector.tensor_scalar(out=Sf[d][:], in0=tmp[d][:], scalar1=etot[d][:, ch:ch + 1],
                                                                          scalar2=None, op0=ALU.mult),
                                  reads=[t_tmp[d], t_etot[d]], writes=[t_Sf[d]])
                            fw.op(fw.act, lambda: nc.scalar.activation(out=Sb[d][:], in_=tmp[d][:], func=AF.Copy,
                                                                       scale=etot[d][:, ch:ch + 1]),
                                  reads=[t_tmp[d], t_etot[d]], writes=[t_Sb[d]])
                        else:
                            fw.op(fw.dve, lambda: nc.vector.tensor_scalar(out=tmp[d][:], in0=Sf[d][:], scalar1=etot[d][:, ch:ch + 1],
                                                                          scalar2=None, op0=ALU.mult),
                                  reads=[t_Sf[d], t_etot[d]], writes=[t_tmp[d]])
                            fw.op(fw.act, lambda: nc.scalar.activation(out=Sb[d][:], in_=Sf[d][:], func=AF.Copy,
                                                                       scale=etot[d][:, ch:ch + 1]),
                                  reads=[t_Sf[d], t_etot[d]], writes=[t_Sb[d]])
                            fw.mm_group([lambda: nc.tensor.matmul(po[:, half * 64:half * 64 + 64], lhsT=Sb[d][:],
                                                                  rhs=qt[d][:, cs:cs + 64], start=False, stop=last)],
                                        reads=[t_Sb[d], t_qt[d]], writes=[t_po])
                            pk, t_pk = get_ps(P)
                            fw.mm_group([lambda: nc.tensor.matmul(pk[:, 0:128], lhsT=ktm[d][rows, n, :],
                                                                  rhs=vtm[rows, n, :], start=True, stop=True)],
                                        reads=[t_ktm[d], t_vtm], writes=[t_pk])
                            fw.op(fw.dve, lambda: nc.vector.tensor_tensor(out=Sf[d][:], in0=pk[:, 0:128], in1=tmp[d][:], op=ALU.add),
                                  reads=[t_pk, t_tmp[d]], writes=[t_Sf[d]])
                    first = (n < NT // 2) if d == 0 else (n >= NT // 2)
                    if first:
                        fw.op(fw.act, lambda: nc.scalar.copy(out=osum[:, c0:c0 + 128], in_=po[:, 0:128]),
                              reads=[t_po], writes=[t_osum], acc=True)
                    else:
                        fw.op(fw.dve, lambda: nc.vector.tensor_tensor(out=osum[:, c0:c0 + 128], in0=po[:, 0:128],
                                                                      in1=osum[:, c0:c0 + 128], op=ALU.add),
                              reads=[t_po, t_osum], writes=[t_osum], acc=True)
            fw.dma("sp", w1[:], A.projT[6144 + r:6144 + r + 128, :], reads=[A.t_projT], writes=[t_w1])
            fw.op(fw.act, lambda: nc.scalar.activation(out=w2[:], in_=osum[:], func=AF.Square), reads=[t_osum], writes=[t_w2])
            for b in range(L // 512):
                ps, t_ps = get_ps(P)
                fw.mm_group([lambda: nc.tensor.matmul(ps[:, :], lhsT=ones[:], rhs=w2[:, b * 512:(b + 1) * 512],
                                                      start=True, stop=True)], reads=[t_ones, t_w2], writes=[t_ps])
                fw.op(fw.act, lambda: nc.scalar.activation(out=w3[:, b * 512:(b + 1) * 512], in_=ps[:, :], func=AF.Sqrt,
                                                           scale=1.0 / 128, bias=S.eps[:, 0:1]),
                      reads=[t_ps, S.t_eps], writes=[t_w3], acc=True)
            fw.op(fw.dve, lambda: nc.vector.reciprocal(out=w3[:], in_=w3[:]), reads=[t_w3], writes=[t_w3])
            fw.op(fw.dve, lambda: nc.vector.scalar_tensor_tensor(out=w2[:], in0=osum[:], scalar=ong[:, h:h + 1], in1=w3[:],
                                                                 op0=ALU.mult, op1=ALU.mult),
                  reads=[t_osum, t_ong, t_w3], writes=[t_w2])
            fw.op(fw.act, lambda: nc.scalar.activation(out=w1[:], in_=w1[:], func=AF.Silu), reads=[t_w1], writes=[t_w1])
            fw.op(fw.dve, lambda: nc.vector.tensor_tensor(out=oTb[:], in0=w2[:], in1=w1[:], op=ALU.mult),
                  reads=[t_w2, t_w1], writes=[t_oTb])
            fw.dma("sp", A.oT[r:r + 128, :], oTb[:], reads=[t_oTb], writes=[A.t_oT], acc=True)
        fw.barrier()


def phase_wout(fw, nc, P, S, A, L, oT, t_oT, w_bf, t_w, x_in, t_xin, x_out, t_xout):
    with ExitStack() as es:
        sb = lambda n, shp, dt: es.enter_context(nc.sbuf_tensor(U(n), shp, dt))
        wo = sb("wo", [128, 16, D], BF16); t_wo = Tok("wo")
        wv = w_bf.rearrange("(c p) n -> p c n", p=128)
        for c in range(0, 16, 4):
            fw.dma("sp", wo[:, c:c + 4, :], wv[:, c:c + 4, :], reads=[t_w], writes=[t_wo], acc=True)
        ob = [sb("oblk0", [128, 16, 512], BF16), sb("oblk1", [128, 16, 512], BF16)]; t_ob = [Tok("oblk0"), Tok("oblk1")]
        xt = [sb("xr0", [128, D], F32), sb("xr1", [128, D], F32)]; t_xt = [Tok("xr0"), Tok("xr1")]
        ov = oT.rearrange("(h p) t -> p h t", p=128)
        xi = 0
        for tb in range(L // 512):
            o, t_o = ob[tb % 2], t_ob[tb % 2]
            fw.dma("sp", o[:], ov[:, :, tb * 512:(tb + 1) * 512], reads=[t_oT], writes=[t_o])
            for tt in range(4):
                r0 = tb * 512 + tt * 128
                x, t_x = xt[xi % 2], t_xt[xi % 2]
                xi += 1
                fw.dma("sp", x[:], x_in[r0:r0 + 128, :], reads=[t_xin], writes=[t_x])
                for nb in range(4):
                    ps, t_ps = get_ps(P)
                    fns = [lambda h=h: nc.tensor.matmul(ps[:, :], lhsT=o[:, h, tt * 128:(tt + 1) * 128],
                                                        rhs=wo[:, h, nb * 512:(nb + 1) * 512], start=(h == 0), stop=(h == 15))
                           for h in range(16)]
                    fw.mm_group(fns, reads=[t_o, t_wo], writes=[t_ps])
                    fw.op(fw.dve, lambda: nc.vector.tensor_tensor(out=x[:, nb * 512:(nb + 1) * 512], in0=ps[:, :],
                                                                  in1=x[:, nb * 512:(nb + 1) * 512], op=ALU.add),
                          reads=[t_ps, t_x], writes=[t_x], acc=True)
                fw.dma("sp", x_out[r0:r0 + 128, :], x[:], reads=[t_x], writes=[t_xout], acc=True)
        fw.barrier()


def phase_ffn(fw, nc, P, S, L, F, wg_bf, wu_bf, wd_bf, t_w, x_in, t_xin, x_out, t_xout,
              gains=None, gain_idx=None, hn_in=None, t_hn_in=None, rowscale=None, t_rowscale=None, barrier=True):
    nfc = F // 128
    h1 = (nfc + 1) // 2
    with ExitStack() as es:
        sb = lambda n, shp, dt: es.enter_context(nc.sbuf_tensor(U(n), shp, dt))
        if hn_in is None:
            alloc_norm(nc, es, S, None)
            load_gain(fw, nc, S, gains, gain_idx)
        else:
            S.hn2 = [sb("hnl0", [128, D], BF16), sb("hnl1", [128, D], BF16)]; S.t_hn2 = [Tok("hnl0"), Tok("hnl1")]
            S.hnT = sb("hnT", [128, 16, 512], BF16); S.t_hnT = Tok("hnT")
            rs = sb("rs", [128, L // 128], F32); t_rs = Tok("rs")
            with nc.allow_non_contiguous_dma(reason="tiny"):
                fw.dma("sp", rs[:], rowscale.rearrange("(n p) -> p n", p=128), reads=[t_rowscale], writes=[t_rs])
        hT = sb("hT", [128, nfc, 512], BF16); t_hT = Tok("hT")
        wgs = [sb("wgs0", [128, 16, 256], BF16), sb("wgs1", [128, 16, 256], BF16)]; t_wgs = [Tok("wgs0"), Tok("wgs1")]
        wus = [sb("wus0", [128, 16, 256], BF16), sb("wus1", [128, 16, 256], BF16)]; t_wus = [Tok("wus0"), Tok("wus1")]
        wds = [sb("wds0", [128, h1, 512], BF16), sb("wds1", [128, h1, 512], BF16)]; t_wds = [Tok("wds0"), Tok("wds1")]
        sg = [sb("sg0", [128, 512], F32), sb("sg1", [128, 512], F32)]; t_sg = [Tok("sg0"), Tok("sg1")]
        xs = [sb(f"xs{i}", [128, 512], F32) for i in range(3)]; t_xs = [Tok(f"xs{i}") for i in range(3)]
        wgv = wg_bf.rearrange("(c p) n -> p c n", p=128)
        wuv = wu_bf.rearrange("(c p) n -> p c n", p=128)
        wdv = wd_bf.rearrange("(c p) n -> p c n", p=128)
        slabs = []
        c0 = 0
        while c0 < F:
            w = min(256, F - c0)
            slabs.append((c0, w))
            c0 += w
        wi = 0; si = 0; xi = 0; di = 0
        for tb in range(L // 512):
            for tt in range(4):
                if hn_in is None:
                    norm_T(fw, nc, P, S, x_in, tb * 512 + tt * 128, tt, S.gain, S.t_gain)
                else:
                    r0 = tb * 512 + tt * 128
                    hb, t_hb = S.hn2[tt % 2], S.t_hn2[tt % 2]
                    fw.dma("sp", hb[:], hn_in[r0:r0 + 128, :], reads=[t_hn_in], writes=[t_hb])
                    for g in range(4):
                        pb, t_pb = get_psb(P)
                        fns = [lambda j=j: nc.tensor.transpose(out=pb[:, j * 128:(j + 1) * 128],
                                                               in_=hb[:, (4 * g + j) * 128:(4 * g + j + 1) * 128],
                                                               identity=S.ident[:]) for j in range(4)]
                        fw.mm_group(fns, reads=[t_hb, S.t_ident], writes=[t_pb])
                        dst = S.hnT[:, 4 * g:4 * g + 4, tt * 128:(tt + 1) * 128]
                        src = pb[:, 0:512].rearrange("p (j t) -> p j t", t=128)
                        if g % 2 == 0:
                            fw.op(fw.act, lambda: nc.scalar.copy(out=dst, in_=src), reads=[t_pb], writes=[S.t_hnT], acc=True)
                        else:
                            fw.op(fw.dve, lambda: nc.vector.tensor_copy(out=dst, in_=src), reads=[t_pb], writes=[S.t_hnT], acc=True)
            for (c0, w) in slabs:
                wg, t_wg = wgs[wi % 2], t_wgs[wi % 2]
                wu, t_wu = wus[wi % 2], t_wus[wi % 2]
                wi += 1
                fw.dma("sp", wg[:, :, 0:w], wgv[:, :, c0:c0 + w], reads=[t_w], writes=[t_wg])
                fw.dma("sp", wu[:, :, 0:w], wuv[:, :, c0:c0 + w], reads=[t_w], writes=[t_wu])
                for j in range(w // 128):
                    fc = c0 // 128 + j
                    psg, t_psg = get_ps(P)
                    fw.mm_group([lambda c=c: nc.tensor.matmul(psg[:, :], lhsT=wg[:, c, j * 128:(j + 1) * 128],
                                                              rhs=S.hnT[:, c, :], start=(c == 0), stop=(c == 15))
                                 for c in range(16)], reads=[t_wg, S.t_hnT], writes=[t_psg])
                    psu, t_psu = get_ps(P)
                    fw.mm_group([lambda c=c: nc.tensor.matmul(psu[:, :], lhsT=wu[:, c, j * 128:(j + 1) * 128],
                                                              rhs=S.hnT[:, c, :], start=(c == 0), stop=(c == 15))
                                 for c in range(16)], reads=[t_wu, S.t_hnT], writes=[t_psu])
                    s_, t_s = sg[si % 2], t_sg[si % 2]
                    si += 1
                    fw.op(fw.act, lambda: nc.scalar.activation(out=s_[:], in_=psg[:, :], func=AF.Silu),
                          reads=[t_psg], writes=[t_s])
                    fw.op(fw.dve, lambda: nc.vector.tensor_tensor(out=hT[:, fc, :], in0=s_[:], in1=psu[:, :], op=ALU.mult),
                          reads=[t_s, t_psu], writes=[t_hT], acc=True)
            for nb in range(4):
                banks = [get_ps(P) for _ in range(4)]
                for half in range(2):
                    f0 = 0 if half == 0 else h1
                    f1 = h1 if half == 0 else nfc
                    wd, t_wd = wds[di % 2], t_wds[di % 2]
                    di += 1
                    fw.dma("sp", wd[:, 0:f1 - f0, :], wdv[:, f0:f1, nb * 512:(nb + 1) * 512], reads=[t_w], writes=[t_wd])
                    for tt in range(4):
                        ps, t_ps = banks[tt]
                        fw.mm_group([lambda fc=fc: nc.tensor.matmul(ps[:, :], lhsT=hT[:, fc, tt * 128:(tt + 1) * 128],
                                                                    rhs=wd[:, fc - f0, :], start=(fc == 0), stop=(fc == nfc - 1))
                                     for fc in range(f0, f1)], reads=[t_hT, t_wd], writes=[t_ps])
                for tt in range(4):
                    ps, t_ps = banks[tt]
                    r0 = tb * 512 + tt * 128
                    x, t_x = xs[xi % 3], t_xs[xi % 3]
                    xi += 1
                    if hn_in is None:
                        fw.dma("sp", x[:], x_in[r0:r0 + 128, nb * 512:(nb + 1) * 512], reads=[t_xin], writes=[t_x])
                        fw.op(fw.dve, lambda: nc.vector.tensor_tensor(out=x[:], in0=ps[:, :], in1=x[:], op=ALU.add),
                              reads=[t_ps, t_x], writes=[t_x])
                    else:
                        n = tb * 4 + tt
                        fw.op(fw.act, lambda: nc.scalar.activation(out=x[:], in_=ps[:, :], func=AF.Copy, scale=rs[:, n:n + 1]),
                              reads=[t_ps, t_rs], writes=[t_x])
                    fw.dma("sp", x_out[r0:r0 + 128, nb * 512:(nb + 1) * 512], x[:], reads=[t_x], writes=[t_xout], acc=True)
        if barrier:
            fw.barrier()


def attn_consts(L):
    c = {}
    half = 64
    inv_freq = (np.float32(10000.0) ** (-np.arange(half, dtype=np.float32) * np.float32(2.0) / np.float32(128))).astype(np.float32)
    pos = np.arange(L, dtype=np.float32)
    ang = (pos[:, None] * inv_freq[None, :]).astype(np.float32)
    c["cos"] = np.cos(ang).astype(np.float32)
    c["sin"] = np.sin(ang).astype(np.float32)
    sl = np.arange(128)[:, None, None]
    j = np.arange(23)[None, :, None]
    tl = np.arange(128)[None, None, :]
    off = (tl - sl) - 128 * (11 - j)
    a = np.abs(off)
    m = (a <= 64).astype(np.float32) + ((off % 4 == 0) & (a <= 256)).astype(np.float32) + ((off % 16 == 0) & (a <= 1024)).astype(np.float32)
    c["amask"] = m.reshape(128, 23 * 128).astype(ml_dtypes.bfloat16)
    c["ident32"] = np.eye(128, dtype=np.float32)
    return c


def get_ps_sub(P, lo, hi, key):
    n = hi - lo
    i = getattr(P, key, 0)
    setattr(P, key, i + 1)
    k = lo + (i % n)
    return P.f32[k], P.f32t[k]


def phase_attn_proj(fw, nc, P, S, A, L):
    NT = L // 128
    with ExitStack() as es:
        sb = lambda n, shp, dt: es.enter_context(nc.sbuf_tensor(U(n), shp, dt))
        alloc_norm(nc, es, S, None)
        load_gain(fw, nc, S, A.gains, 2)
        wsl = [sb("awsl0", [128, 16, 512], BF16), sb("awsl1", [128, 16, 512], BF16)]; t_wsl = [Tok("awsl0"), Tok("awsl1")]
        cosT = sb("cosT", [128, NT, 64], F32); sinT = sb("sinT", [128, NT, 64], F32); t_cs = Tok("cs")
        fw.dma("sp", cosT[:], A.consts["cos"].rearrange("(n p) f -> p n f", p=128), writes=[t_cs], acc=True)
        fw.dma("sp", sinT[:], A.consts["sin"].rearrange("(n p) f -> p n f", p=128), writes=[t_cs], acc=True)
        gqk = sb("gqk", [128, 2, 128], F32); t_gqk = Tok("gqk")
        fw.dma("sp", gqk[:, 0, :], A.q_gain.partition_broadcast(128), writes=[t_gqk], acc=True)
        fw.dma("sp", gqk[:, 1, :], A.k_gain.partition_broadcast(128), writes=[t_gqk], acc=True)
        fw.op(fw.act, lambda: nc.scalar.mul(out=gqk[:, 0, :], in_=gqk[:, 0, :], mul=float(128 ** -0.5)), reads=[t_gqk], writes=[t_gqk])
        sq = sb("asq", [128, 512], F32); t_sq = Tok("asq")
        st = sb("ast", [128, 12], F32); t_st = Tok("ast")
        t1 = sb("at1", [128, 512], F32); t_t1 = Tok("at1")
        ra = sb("ara", [128, 4, 64], F32); t_ra = Tok("ara")
        rb = sb("arb", [128, 4, 64], F32); t_rb = Tok("arb")
        qr = [sb("aqr0", [128, 512], BF16), sb("aqr1", [128, 512], BF16)]; t_qr = [Tok("aqr0"), Tok("aqr1")]
        stg = [sb("astg0", [128, 4, 512], BF16), sb("astg1", [128, 4, 512], BF16)]; t_stg = [Tok("astg0"), Tok("astg1")]
        vo = [sb("avo0", [128, 512], BF16), sb("avo1", [128, 512], BF16)]; t_vo = [Tok("avo0"), Tok("avo1")]
        wv = A.wqkv_bf.rearrange("(c p) n -> p c n", p=128)
        wi = 0; qi = 0; vi = 0; gi = 0
        for tb in range(L // 512):
            for tt in range(4):
                norm_T(fw, nc, P, S, A.x2, tb * 512 + tt * 128, tt, S.gain, S.t_gain)
            for s in range(12):
                w, t_w = wsl[wi % 2], t_wsl[wi % 2]
                wi += 1
                fw.dma("sp", w[:], wv[:, :, s * 512:(s + 1) * 512], reads=[A.t_wqkv], writes=[t_w])
                if s < 8:
                    sg_, t_sg = stg[gi % 2], t_stg[gi % 2]
                    gi += 1
                for tt in range(4):
                    n = tb * 4 + tt
                    r0 = n * 128
                    ps, t_ps = get_ps(P)
                    fw.mm_group([lambda c=c: nc.tensor.matmul(ps[:, :], lhsT=S.hnT[:, c, tt * 128:(tt + 1) * 128],
                                                              rhs=w[:, c, :], start=(c == 0), stop=(c == 15))
                                 for c in range(16)], reads=[t_w, S.t_hnT], writes=[t_ps])
                    if s >= 8:
                        o, t_o = vo[vi % 2], t_vo[vi % 2]
                        vi += 1
                        fw.op(fw.act, lambda: nc.scalar.copy(out=o[:], in_=ps[:, :]), reads=[t_ps], writes=[t_o])
                        fw.dma("sp", A.v_att[r0:r0 + 128, (s - 8) * 512:(s - 7) * 512], o[:], reads=[t_o],
                               writes=[A.t_v_att], acc=True)
                        continue
                    which = 0 if s < 4 else 1
                    fw.op(fw.act, lambda: nc.scalar.activation(out=sq[:], in_=ps[:, :], func=AF.Square), reads=[t_ps], writes=[t_sq])
                    fw.op(fw.dve, lambda: nc.vector.tensor_reduce(out=st[:, 0:4], in_=sq[:].rearrange("p (h d) -> p h d", d=128),
                                                                  axis=AX.X, op=ALU.add), reads=[t_sq], writes=[t_st])
                    fw.op(fw.act, lambda: nc.scalar.activation(out=st[:, 4:8], in_=st[:, 0:4], func=AF.Sqrt, scale=1.0 / 128,
                                                               bias=S.eps[:, 0:1]), reads=[t_st, S.t_eps], writes=[t_st])
                    fw.op(fw.dve, lambda: nc.vector.reciprocal(out=st[:, 8:12], in_=st[:, 4:8]), reads=[t_st], writes=[t_st])
                    t1v = t1[:].rearrange("p (h d) -> p h d", d=128)
                    fw.op(fw.dve, lambda: nc.vector.tensor_tensor(out=t1v, in0=ps[:, :].rearrange("p (h d) -> p h d", d=128),
                                                                  in1=st[:, 8:12].unsqueeze(2).to_broadcast([128, 4, 128]), op=ALU.mult),
                          reads=[t_ps, t_st], writes=[t_t1])
                    fw.op(fw.dve, lambda: nc.vector.tensor_tensor(out=t1v, in0=t1v,
                                                                  in1=gqk[:, which:which + 1, :].to_broadcast([128, 4, 128]), op=ALU.mult),
                          reads=[t_t1, t_gqk], writes=[t_t1])
                    q_, t_q = qr[qi % 2], t_qr[qi % 2]
                    qi += 1
                    x1v = t1v[:, :, 0:64]
                    x2v = t1v[:, :, 64:128]
                    cb = cosT[:, n:n + 1, :].to_broadcast([128, 4, 64])
                    sbb = sinT[:, n:n + 1, :].to_broadcast([128, 4, 64])
                    qv = q_[:].rearrange("p (h d) -> p h d", d=128)
                    fw.op(fw.dve, lambda: nc.vector.tensor_tensor(out=ra[:], in0=x1v, in1=cb, op=ALU.mult), reads=[t_t1, t_cs], writes=[t_ra])
                    fw.op(fw.dve, lambda: nc.vector.tensor_tensor(out=rb[:], in0=x2v, in1=sbb, op=ALU.mult), reads=[t_t1, t_cs], writes=[t_rb])
                    fw.op(fw.dve, lambda: nc.vector.tensor_tensor(out=qv[:, :, 0:64], in0=ra[:], in1=rb[:], op=ALU.subtract),
                          reads=[t_ra, t_rb], writes=[t_q])
                    fw.op(fw.dve, lambda: nc.vector.tensor_tensor(out=ra[:], in0=x2v, in1=cb, op=ALU.mult), reads=[t_t1, t_cs], writes=[t_ra])
                    fw.op(fw.dve, lambda: nc.vector.tensor_tensor(out=rb[:], in0=x1v, in1=sbb, op=ALU.mult), reads=[t_t1, t_cs], writes=[t_rb])
                    fw.op(fw.dve, lambda: nc.vector.tensor_tensor(out=qv[:, :, 64:128], in0=ra[:], in1=rb[:], op=ALU.add),
                          reads=[t_ra, t_rb], writes=[t_q], acc=True)
                    pb, t_pb = get_psb(P)
                    fw.mm_group([lambda j=j: nc.tensor.transpose(out=pb[:, j * 128:(j + 1) * 128], in_=q_[:, j * 128:(j + 1) * 128],
                                                                 identity=S.ident[:]) for j in range(4)],
                                reads=[t_q, S.t_ident], writes=[t_pb])
                    fw.op(fw.act, lambda: nc.scalar.copy(out=sg_[:, :, tt * 128:(tt + 1) * 128],
                                                         in_=pb[:, 0:512].rearrange("p (j t) -> p j t", t=128)),
                          reads=[t_pb], writes=[t_sg], acc=True)
                if s < 8:
                    dstT = A.qT if s < 4 else A.kT
                    t_dst = A.t_qT if s < 4 else A.t_kT
                    h0 = (s % 4) * 4
                    for j in range(4):
                        fw.dma("sp", dstT[(h0 + j) * 128:(h0 + j + 1) * 128, tb * 512:(tb + 1) * 512], sg_[:, j, :],
                               reads=[t_sg], writes=[t_dst], acc=True)
        fw.barrier()


def phase_attn_core(fw, nc, P, S, A, L):
    NT = L // 128
    NG = L // 512
    with ExitStack() as es:
        sb = lambda n, shp, dt: es.enter_context(nc.sbuf_tensor(U(n), shp, dt))
        amask = sb("amask", [128, 23 * 128], BF16); t_amask = Tok("amask")
        fw.dma("sp", amask[:], A.consts["amask"][:, :], writes=[t_amask])
        onesb = sb("onesb", [128, 128], BF16); t_onesb = Tok("onesb")
        fw.op(fw.dve, lambda: nc.vector.memset(onesb[:], 1.0), writes=[t_onesb])
        qT = [sb("aq0", [128, L], BF16), sb("aq1", [128, L], BF16)]; t_q = [Tok("aq0"), Tok("aq1")]
        kT = [sb("ak0", [128, L], BF16), sb("ak1", [128, L], BF16)]; t_k = [Tok("ak0"), Tok("ak1")]
        vh = [sb("av0", [128, NT, 128], BF16), sb("av1", [128, NT, 128], BF16)]; t_v = [Tok("av0"), Tok("av1")]
        oh = [sb("ao0", [128, L], BF16), sb("ao1", [128, L], BF16)]; t_o = [Tok("ao0"), Tok("ao1")]
        pe_ = [sb(f"ape{i}", [128, 512], BF16) for i in range(3)]; t_pe = [Tok(f"ape{i}") for i in range(3)]
        pm = [sb(f"apm{i}", [128, 512], BF16) for i in range(3)]; t_pm = [Tok(f"apm{i}") for i in range(3)]
        rden = sb("arden", [128, 512], F32); t_rden = Tok("arden")
        pi = 0
        for h in range(16):
            b = h % 2
            r = h * 128
            fw.dma("sp", qT[b][:], A.qT[r:r + 128, :], reads=[A.t_qT], writes=[t_q[b]])
            fw.dma("sp", kT[b][:], A.kT[r:r + 128, :], reads=[A.t_kT], writes=[t_k[b]])
            fw.dma("sp", vh[b][:], A.v_att[:, r:r + 128].rearrange("(n p) v -> p n v", p=128), reads=[A.t_v_att], writes=[t_v[b]])
            for g in range(NG):
                qb0 = 4 * g
                t0 = g * 512
                kbs = list(range(max(0, qb0 - 8), min(NT, qb0 + 12)))
                po, t_po = P.f32[4], P.f32t[4]
                pd, t_pd = P.f32[5], P.f32t[5]
                for ki, kb in enumerate(kbs):
                    ps, t_ps = get_ps_sub(P, 0, 4, "ia")
                    fw.mm_group([lambda: nc.tensor.matmul(ps[:, :], lhsT=kT[b][:, kb * 128:(kb + 1) * 128],
                                                          rhs=qT[b][:, t0:t0 + 512], start=True, stop=True)],
                                reads=[t_k[b], t_q[b]], writes=[t_ps])
                    e_, t_e = pe_[pi % 3], t_pe[pi % 3]
                    m_, t_m = pm[pi % 3], t_pm[pi % 3]
                    pi += 1
                    fw.op(fw.act, lambda: nc.scalar.activation(out=e_[:], in_=ps[:, :], func=AF.Exp), reads=[t_ps], writes=[t_e])
                    j0 = 11 - (kb - qb0)
                    eng = fw.dve if pi % 2 else fw.pool
                    raw = nc.vector if pi % 2 else nc.gpsimd
                    fw.op(eng, lambda: raw.tensor_tensor(out=m_[:], in0=e_[:], in1=amask[:, j0 * 128:j0 * 128 + 512], op=ALU.mult),
                          reads=[t_e, t_amask], writes=[t_m])
                    first = ki == 0
                    last = ki == len(kbs) - 1
                    fw.mm_group([lambda: nc.tensor.matmul(po[:, :], lhsT=vh[b][:, kb, :], rhs=m_[:], start=first, stop=last)],
                                reads=[t_v[b], t_m], writes=[t_po])
                    fw.mm_group([lambda: nc.tensor.matmul(pd[:, :], lhsT=onesb[:], rhs=m_[:], start=first, stop=last)],
                                reads=[t_onesb, t_m], writes=[t_pd])
                fw.op(fw.dve, lambda: nc.vector.reciprocal(out=rden[:], in_=pd[:, :]), reads=[t_pd], writes=[t_rden])
                fw.op(fw.dve, lambda: nc.vector.tensor_tensor(out=oh[b][:, t0:t0 + 512], in0=po[:, :], in1=rden[:], op=ALU.mult),
                      reads=[t_po, t_rden], writes=[t_o[b]], acc=True)
            fw.dma("sp", A.oT[r:r + 128, :], oh[b][:], reads=[t_o[b]], writes=[A.t_oT], acc=True)
        fw.barrier()


def phase_router(fw, nc, P, S, A, L):
    with ExitStack() as es:
        sb = lambda n, shp, dt: es.enter_context(nc.sbuf_tensor(U(n), shp, dt))
        alloc_norm(nc, es, S, None)
        load_gain(fw, nc, S, A.gains, 3)
        id32 = sb("id32", [128, 128], F32); t_id32 = Tok("id32")
        fw.dma("sp", id32[:], A.consts["ident32"][:, :], writes=[t_id32])
        wr = sb("wr", [128, 16, 8], F32); t_wr = Tok("wr")
        with nc.allow_non_contiguous_dma(reason="small router weight"):
            fw.dma("sp", wr[:], A.w_router.rearrange("(c p) e -> p c e", p=128), writes=[t_wr])
        hn32 = sb("hn32", [128, D], F32); t_hn32 = Tok("hn32")
        hT32 = sb("hT32", [128, 16, 128], F32); t_hT32 = Tok("hT32")
        lg = sb("lg", [128, 8], F32); t_lg = Tok("lg")
        l2 = sb("l2", [128, 8], F32); t_l2 = Tok("l2")
        sm = sb("sm", [128, 8], F32); t_sm = Tok("sm")
        gt = sb("gt", [128, 8], F32); t_gt = Tok("gt")
        for n in range(L // 128):
            r0 = n * 128
            k = S.xi % 2
            S.xi += 1
            xt, t_x = S.xt[k], S.t_xt[k]
            fw.dma("sp", xt[:], A.x3[r0:r0 + 128, :], reads=[A.t_x3], writes=[t_x])
            fw.op(fw.act, lambda: nc.scalar.activation(out=S.junk[:], in_=xt[:], func=AF.Square), reads=[t_x], writes=[S.t_junk])
            fw.op(fw.dve, lambda: nc.vector.tensor_reduce(out=S.ss[:, 0:1], in_=S.junk[:], axis=AX.X, op=ALU.add),
                  reads=[S.t_junk], writes=[S.t_ss])
            fw.op(fw.act, lambda: nc.scalar.activation(out=S.ss[:, 1:2], in_=S.ss[:, 0:1], func=AF.Sqrt, scale=1.0 / D,
                                                       bias=S.eps[:, 0:1]), reads=[S.t_ss, S.t_eps], writes=[S.t_ss2])
            fw.op(fw.dve, lambda: nc.vector.reciprocal(out=S.ss[:, 2:3], in_=S.ss[:, 1:2]), reads=[S.t_ss2], writes=[S.t_rstd])
            fw.op(fw.dve, lambda: nc.vector.scalar_tensor_tensor(out=hn32[:], in0=xt[:], scalar=S.ss[:, 2:3], in1=S.gain[:],
                                                                 op0=ALU.mult, op1=ALU.mult),
                  reads=[t_x, S.t_rstd, S.t_gain], writes=[t_hn32])
            fw.op(fw.act, lambda: nc.scalar.copy(out=S.hn[:], in_=hn32[:]), reads=[t_hn32], writes=[S.t_hn])
            fw.dma("sp", A.hn3[r0:r0 + 128, :], S.hn[:], reads=[S.t_hn], writes=[A.t_hn3], acc=True)
            for g in range(4):
                ps, t_ps = get_ps(P)
                fw.mm_group([lambda j=j: nc.tensor.transpose(out=ps[:, j * 128:(j + 1) * 128],
                                                             in_=hn32[:, (4 * g + j) * 128:(4 * g + j + 1) * 128],
                                                             identity=id32[:]) for j in range(4)],
                            reads=[t_hn32, t_id32], writes=[t_ps])
                fw.op(fw.act if g % 2 else fw.dve,
                      (lambda: nc.scalar.copy(out=hT32[:, 4 * g:4 * g + 4, :], in_=ps[:, :].rearrange("p (j t) -> p j t", t=128))) if g % 2 else
                      (lambda: nc.vector.tensor_copy(out=hT32[:, 4 * g:4 * g + 4, :], in_=ps[:, :].rearrange("p (j t) -> p j t", t=128))),
                      reads=[t_ps], writes=[t_hT32], acc=True)
            ps, t_ps = get_ps(P)
            fw.mm_group([lambda c=c: nc.tensor.matmul(ps[:, 0:8], lhsT=hT32[:, c, :], rhs=wr[:, c, :], start=(c == 0), stop=(c == 15))
                         for c in range(16)], reads=[t_hT32, t_wr], writes=[t_ps])
            fw.op(fw.dve, lambda: nc.vector.tensor_copy(out=lg[:], in_=ps[:, 0:8]), reads=[t_ps], writes=[t_lg])
            fw.op(fw.dve, lambda: nc.vector.tensor_reduce(out=sm[:, 0:1], in_=lg[:], axis=AX.X, op=ALU.max), reads=[t_lg], writes=[t_sm])
            fw.op(fw.dve, lambda: nc.vector.tensor_scalar(out=l2[:], in0=lg[:], scalar1=sm[:, 0:1], scalar2=-1e30,
                                                          op0=ALU.is_equal, op1=ALU.mult), reads=[t_lg, t_sm], writes=[t_l2])
            fw.op(fw.dve, lambda: nc.vector.tensor_tensor(out=l2[:], in0=l2[:], in1=lg[:], op=ALU.add), reads=[t_l2, t_lg], writes=[t_l2])
            fw.op(fw.dve, lambda: nc.vector.tensor_reduce(out=sm[:, 1:2], in_=l2[:], axis=AX.X, op=ALU.max), reads=[t_l2], writes=[t_sm])
            fw.op(fw.dve, lambda: nc.vector.tensor_scalar(out=l2[:], in0=lg[:], scalar1=sm[:, 1:2], scalar2=None, op0=ALU.is_ge),
                  reads=[t_lg, t_sm], writes=[t_l2])
            fw.op(fw.dve, lambda: nc.vector.tensor_scalar(out=sm[:, 2:3], in0=sm[:, 0:1], scalar1=-1.0, scalar2=None, op0=ALU.mult),
                  reads=[t_sm], writes=[t_sm])
            fw.op(fw.act, lambda: nc.scalar.activation(out=gt[:], in_=lg[:], func=AF.Exp, bias=sm[:, 2:3], scale=1.0),
                  reads=[t_lg, t_sm], writes=[t_gt])
            fw.op(fw.dve, lambda: nc.vector.tensor_tensor(out=gt[:], in0=gt[:], in1=l2[:], op=ALU.mult), reads=[t_gt, t_l2], writes=[t_gt])
            fw.op(fw.dve, lambda: nc.vector.tensor_reduce(out=sm[:, 3:4], in_=gt[:], axis=AX.X, op=ALU.add), reads=[t_gt], writes=[t_sm])
            fw.op(fw.dve, lambda: nc.vector.reciprocal(out=sm[:, 4:5], in_=sm[:, 3:4]), reads=[t_sm], writes=[t_sm])
            fw.op(fw.dve, lambda: nc.vector.tensor_scalar(out=gt[:], in0=gt[:], scalar1=sm[:, 4:5], scalar2=None, op0=ALU.mult),
                  reads=[t_gt, t_sm], writes=[t_gt])
            fw.dma("sp", A.gates[r0:r0 + 128, :], gt[:], reads=[t_gt], writes=[A.t_gates], acc=True)
        fw.barrier()


def build_A(L, stages=("hgrn", "ffn", "attn", "router"), dbg=False):
    nc = bass.Bass("TRN2", target_bir_lowering=False)
    A = Ctx(); S = Ctx()
    din = lambda n, shp, dt=F32: nc.dram_tensor(n, shp, dt, kind="ExternalInput").ap()
    dint = lambda n, shp, dt: nc.dram_tensor(n, shp, dt).ap()
    dout = lambda n, shp, dt: nc.dram_tensor(n, shp, dt, kind="ExternalOutput").ap()
    A.x = din("x", [L, D]); A.gains = din("gains", [4, D])
    w_in = din("w_in", [D, 10240]); A.lb_logits = din("lb_logits", [3, 2, 2048]); A.onorm = din("onorm", [2048])
    w_out0 = din("w_out0", [D, D])
    wg = din("wg", [D, FF]); wu = din("wu", [D, FF]); wd = din("wd", [FF, D])
    wqkv = din("wqkv", [D, 3 * D]); A.q_gain = din("q_gain", [128]); A.k_gain = din("k_gain", [128]); w_out1 = din("w_out1", [D, D])
    A.w_router = din("w_router", [D, 8])
    cn = make_consts(); cn.update(attn_consts(L))
    A.consts = {k: din("c_" + k, list(v.shape), BF16 if v.dtype == ml_dtypes.bfloat16 else F32) for k, v in cn.items()}
    A.w_in_bf = dint("w_in_bf", [D, 10240], BF16); A.t_w_in = Tok("w_in")
    w_out0_bf = dint("w_out0_bf", [D, D], BF16); t_w_out0 = Tok("w_out0")
    wg_bf = dint("wg_bf", [D, FF], BF16); wu_bf = dint("wu_bf", [D, FF], BF16); wd_bf = dint("wd_bf", [FF, D], BF16); t_wffn = Tok("wffn")
    A.wqkv_bf = dint("wqkv_bf", [D, 3 * D], BF16); A.t_wqkv = Tok("wqkv")
    w_out1_bf = dint("w_out1_bf", [D, D], BF16); t_w_out1 = Tok("w_out1")
    A.projT = dint("projT", [8192, L], F32); A.t_projT = Tok("projT")
    A.v_tm = dint("v_tm", [L, 2048], BF16); A.t_v_tm = Tok("v_tm")
    A.oT = dint("oT", [2048, L], BF16); A.t_oT = Tok("oT")
    mk = (lambda n, shp, dt: dout(n, shp, dt)) if dbg else (lambda n, shp, dt: dint(n, shp, dt))
    x1 = mk("x1", [L, D], F32); t_x1 = Tok("x1")
    A.x2 = mk("x2", [L, D], F32); A.t_x2 = Tok("x2")
    A.qT = dint("qT", [2048, L], BF16); A.t_qT = Tok("qT")
    A.kT = dint("kT", [2048, L], BF16); A.t_kT = Tok("kT")
    A.v_att = dint("v_att", [L, 2048], BF16); A.t_v_att = Tok("v_att")
    A.x3 = dout("x3", [L, D], F32); A.t_x3 = Tok("x3")
    A.hn3 = dout("hn3", [L, D], BF16); A.t_hn3 = Tok("hn3")
    A.gates = dout("gates", [L, 8], F32); A.t_gates = Tok("gates")
    t_x = Tok("x")
    with ExitStack() as es:
        fw = FW(nc, es)
        P = psum_pools(nc, es, fw)
        alloc_common(nc, es, fw, S, A.consts)
        cast_weight(fw, nc, A.w_in_bf, w_in, D, 10240, A.t_w_in)
        cast_weight(fw, nc, w_out0_bf, w_out0, D, D, t_w_out0)
        cast_weight(fw, nc, wg_bf, wg, D, FF, t_wffn)
        cast_weight(fw, nc, wu_bf, wu, D, FF, t_wffn)
        cast_weight(fw, nc, wd_bf, wd, FF, D, t_wffn)
        cast_weight(fw, nc, A.wqkv_bf, wqkv, D, 3 * D, A.t_wqkv)
        cast_weight(fw, nc, w_out1_bf, w_out1, D, D, t_w_out1)
        phase_hgrn_proj(fw, nc, P, S, A, L)
        phase_hgrn_scan(fw, nc, P, S, A, L)
        phase_wout(fw, nc, P, S, A, L, A.oT, A.t_oT, w_out0_bf, t_w_out0, A.x, t_x, x1, t_x1)
        phase_ffn(fw, nc, P, S, L, FF, wg_bf, wu_bf, wd_bf, t_wffn, x1, t_x1, A.x2, A.t_x2, gains=A.gains, gain_idx=1)
        phase_attn_proj(fw, nc, P, S, A, L)
        phase_attn_core(fw, nc, P, S, A, L)
        phase_wout(fw, nc, P, S, A, L, A.oT, A.t_oT, w_out1_bf, t_w_out1, A.x2, A.t_x2, A.x3, A.t_x3)
        phase_router(fw, nc, P, S, A, L)
        fw.final_wait([A.t_x3, A.t_hn3, A.t_gates] + ([t_x1, A.t_x2] if dbg else []))
        print("ninst", fw.ninst, "counts", [(e.name, e.count) for e in fw.engs], flush=True)
    return nc, cn


def inputs_A(d, b, L, cn):
    inm = {"x": np.ascontiguousarray(d['x'][b, :L]), "gains": d['norm_gains'].reshape(4, D), "w_in": d['hgrn_w_in'][0],
           "lb_logits": d['hgrn_lb_logits'], "onorm": d['hgrn_onorm'][0], "w_out0": d['hgrn_w_out'][0],
           "wg": d['ffn_w_gate'][0], "wu": d['ffn_w_up'][0], "wd": d['ffn_w_down'][0],
           "wqkv": d['attn_w_qkv'][0], "q_gain": d['attn_q_gain'][0], "k_gain": d['attn_k_gain'][0], "w_out1": d['attn_w_out'][0],
           "w_router": d['moe_w_router'][0]}
    for k, v in cn.items():
        inm["c_" + k] = v
    return inm


FE = 7168


def build_B(cap):
    nc = bass.Bass("TRN2", target_bir_lowering=False)
    S = Ctx()
    din = lambda n, shp, dt=F32: nc.dram_tensor(n, shp, dt, kind="ExternalInput").ap()
    dint = lambda n, shp, dt: nc.dram_tensor(n, shp, dt).ap()
    hn_g = din("hn_g", [cap, D], BF16); rs = din("rs", [cap]); t_in = Tok("in")
    wg = din("wg", [D, FE]); wu = din("wu", [D, FE]); wd = din("wd", [FE, D])
    cn = make_consts()
    consts = {k: din("c_" + k, list(v.shape), BF16) for k, v in cn.items()}
    wg_bf = dint("wg_bf", [D, FE], BF16); wu_bf = dint("wu_bf", [D, FE], BF16); wd_bf = dint("wd_bf", [FE, D], BF16)
    t_w = Tok("w")
    y = nc.dram_tensor("y", [cap, D], F32, kind="ExternalOutput").ap(); t_y = Tok("y")
    with ExitStack() as es:
        fw = FW(nc, es)
        P = psum_pools(nc, es, fw)
        alloc_common(nc, es, fw, S, consts)
        cast_weight(fw, nc, wg_bf, wg, D, FE, t_w)
        cast_weight(fw, nc, wu_bf, wu, D, FE, t_w)
        cast_weight(fw, nc, wd_bf, wd, FE, D, t_w)
        phase_ffn(fw, nc, P, S, cap, FE, wg_bf, wu_bf, wd_bf, t_w, None, None, y, t_y,
                  hn_in=hn_g, t_hn_in=t_in, rowscale=rs, t_rowscale=t_in)
        fw.final_wait([t_y])
        print("B ninst", fw.ninst, "counts", [(e.name, e.count) for e in fw.engs], flush=True)
    return nc, cn


def build_C(T):
    nc = bass.Bass("TRN2", target_bir_lowering=False)
    din = lambda n, shp, dt=F32: nc.dram_tensor(n, shp, dt, kind="ExternalInput").ap()
    x3 = din("x3", [T, D]); y0 = din("y0", [T, D]); y1 = din("y1", [T, D])
    out = nc.dram_tensor("out", [T, D], F32, kind="ExternalOutput").ap(); t_out = Tok("out")
    with ExitStack() as es:
        fw = FW(nc, es)
        sb = lambda n, shp, dt: es.enter_context(nc.sbuf_tensor(U(n), shp, dt))
        a = [sb(f"ca{i}", [128, D], F32) for i in range(2)]; ta = [Tok("a0"), Tok("a1")]
        b = [sb(f"cb{i}", [128, D], F32) for i in range(2)]; tb_ = [Tok("b0"), Tok("b1")]
        c = [sb(f"cc{i}", [128, D], F32) for i in range(2)]; tc = [Tok("c0"), Tok("c1")]
        for n in range(T // 128):
            k = n % 2
            r0 = n * 128
            fw.dma("sp", a[k][:], x3[r0:r0 + 128, :], writes=[ta[k]])
            fw.dma("sp", b[k][:], y0[r0:r0 + 128, :], writes=[tb_[k]])
            fw.dma("sp", c[k][:], y1[r0:r0 + 128, :], writes=[tc[k]])
            fw.op(fw.dve, lambda: nc.vector.tensor_tensor(out=b[k][:], in0=b[k][:], in1=c[k][:], op=ALU.add),
                  reads=[tb_[k], tc[k]], writes=[tb_[k]])
            fw.op(fw.dve, lambda: nc.vector.tensor_tensor(out=a[k][:], in0=a[k][:], in1=b[k][:], op=ALU.add),
                  reads=[ta[k], tb_[k]], writes=[ta[k]])
            fw.dma("sp", out[r0:r0 + 128, :], a[k][:], reads=[ta[k]], writes=[t_out], acc=True)
        fw.final_wait([t_out])
    return nc


def kernel(**inputs):
    from concourse.bass_utils import run_bass_kernel_spmd
    d = {k: np.asarray(v) for k, v in inputs.items()}
    B, L = d['x'].shape[0], d['x'].shape[1]
    T = B * L
    ncA, cnA = build_A(L)
    in_maps = [inputs_A(d, b, L, cnA) for b in range(B)]
    resA = run_bass_kernel_spmd(ncA, in_maps, core_ids=list(range(B))).results
    x3 = np.concatenate([np.asarray(r["x3"]) for r in resA], axis=0)
    hn3 = np.concatenate([np.asarray(r["hn3"]) for r in resA], axis=0)
    gates = np.concatenate([np.asarray(r["gates"]) for r in resA], axis=0)
    sel = gates > 0
    idx = [np.nonzero(sel[:, e])[0] for e in range(8)]
    cap = max(512, int(-(-max(len(i) for i in idx) // 512) * 512))
    ncB, cnB = build_B(cap)
    in_maps = []
    for e in range(8):
        hg = np.zeros((cap, D), dtype=hn3.dtype)
        hg[:len(idx[e])] = hn3[idx[e]]
        rs = np.zeros((cap,), np.float32)
        rs[:len(idx[e])] = gates[idx[e], e]
        m = {"hn_g": hg, "rs": rs, "wg": d['moe_w_gate'][0, e], "wu": d['moe_w_up'][0, e], "wd": d['moe_w_down'][0, e]}
        for k, v in cnB.items():
            m["c_" + k] = v
        in_maps.append(m)
    resB = run_bass_kernel_spmd(ncB, in_maps, core_ids=list(range(8))).results
    y0 = np.zeros((T, D), np.float32)
    y1 = np.zeros((T, D), np.float32)
    rank = np.cumsum(sel, axis=1) - 1
    for e in range(8):
        ye = np.asarray(resB[e]["y"])[:len(idx[e])]
        sl = rank[idx[e], e]
        y0[idx[e][sl == 0]] = ye[sl == 0]
        y1[idx[e][sl >= 1]] = ye[sl >= 1]
    TC = T // 8
    ncC = build_C(TC)
    in_maps = [{"x3": x3[c * TC:(c + 1) * TC], "y0": y0[c * TC:(c + 1) * TC], "y1": y1[c * TC:(c + 1) * TC]} for c in range(8)]
    resC = run_bass_kernel_spmd(ncC, in_maps, core_ids=list(range(8))).results
    out = np.concatenate([np.asarray(r["out"]) for r in resC], axis=0).reshape(B, L, D).astype(np.float32)
    return out
```

```python
import numpy as np
from contextlib import ExitStack
import concourse.bass as bass
import concourse.mybir as mybir

F32 = mybir.dt.float32
BF16 = mybir.dt.bfloat16
I32 = mybir.dt.int32
U32 = mybir.dt.uint32
AF = mybir.ActivationFunctionType
ALU = mybir.AluOpType
AX = mybir.AxisListType


class Tok:
    __slots__ = ("w", "r", "name")

    def __init__(self, name=""):
        self.w = {}
        self.r = {}
        self.name = name


class Eng:
    def __init__(self, raw, sem, name):
        self.raw = raw
        self.sem = sem
        self.name = name
        self.count = 0
        self.seen = {}


class FW:
    def __init__(self, nc, es, n_dma_sems=24, n_gdma_sems=12):
        self.nc = nc
        self.es = es
        mk = lambda n: es.enter_context(nc.semaphore(n))
        self.pe = Eng(nc.tensor, mk("s_pe"), "pe")
        self.act = Eng(nc.scalar, mk("s_act"), "act")
        self.dve = Eng(nc.vector, mk("s_dve"), "dve")
        self.pool = Eng(nc.gpsimd, mk("s_pool"), "pool")
        self.sp = Eng(nc.sync, mk("s_sp"), "sp")
        self.engs = [self.pe, self.act, self.dve, self.pool, self.sp]
        self.dsems = {"sp": [[mk(f"d_sp{i}"), 0] for i in range(n_dma_sems)],
                      "pool": [[mk(f"d_pl{i}"), 0] for i in range(n_gdma_sems)],
                      "act": [[mk(f"d_ac{i}"), 0] for i in range(8)]}
        self.drr = {"sp": 0, "pool": 0, "act": 0}
        self.ninst = 0

    def _wait(self, eng, sem, val):
        key = id(sem)
        if eng.seen.get(key, 0) >= val:
            return
        eng.raw.wait_ge(sem, val)
        eng.seen[key] = val
        self.ninst += 1

    def _deps(self, eng, reads, writes, skip_self=False):
        for t in reads:
            for k, (s, v) in t.w.items():
                if skip_self and s is eng.sem:
                    continue
                self._wait(eng, s, v)
        for t in writes:
            for k, (s, v) in t.w.items():
                if skip_self and s is eng.sem:
                    continue
                self._wait(eng, s, v)
            for k, (s, v) in t.r.items():
                if skip_self and s is eng.sem:
                    continue
                self._wait(eng, s, v)

    def _record(self, ev, reads, writes, accumulate=False):
        s, v = ev
        for t in writes:
            if not accumulate:
                t.w = {}
            t.w[id(s)] = (s, v)
            t.r = {}
        for t in reads:
            t.r[id(s)] = (s, v)

    def op(self, eng, fn, reads=(), writes=(), skip_self=False, acc=False):
        self._deps(eng, reads, writes, skip_self=skip_self)
        ins = fn()
        eng.count += 1
        ins.then_inc(eng.sem, 1)
        self.ninst += 1
        self._record((eng.sem, eng.count), reads, writes, accumulate=acc)
        return ins

    def mm_group(self, fns, reads=(), writes=()):
        eng = self.pe
        self._deps(eng, reads, writes, skip_self=True)
        ins = None
        for f in fns:
            ins = f()
            self.ninst += 1
        eng.count += 1
        ins.then_inc(eng.sem, 1)
        self._record((eng.sem, eng.count), reads, writes)

    def dma(self, q, out, in_, reads=(), writes=(), acc=False, **kw):
        eng = {"sp": self.sp, "pool": self.pool, "act": self.act}[q]
        pool = self.dsems[q]
        j = self.drr[q]
        self.drr[q] = (j + 1) % len(pool)
        sem, tot = pool[j]
        if tot:
            self._wait(eng, sem, tot)
        self._deps(eng, reads, writes)
        ins = eng.raw.dma_start(out=out, in_=in_, **kw)
        tot += 16
        pool[j][1] = tot
        ins.then_inc(sem, 16)
        self.ninst += 1
        self._record((sem, tot), reads, writes, accumulate=acc)
        return ins

    def all_events(self):
        evs = [(e.sem, e.count) for e in self.engs if e.count]
        for q in self.dsems:
            for sem, tot in self.dsems[q]:
                if tot:
                    evs.append((sem, tot))
        return evs

    def barrier(self, engines=None):
        evs = self.all_events()
        for e in (engines or self.engs):
            for s, v in evs:
                if s is e.sem:
                    continue
                self._wait(e, s, v)

    def final_wait(self, toks):
        for t in toks:
            for k, (s, v) in t.w.items():
                self._wait(self.sp, s, v)


import numpy as np
import ml_dtypes

D = 2048
EPS = 1e-6
FF = 5504
NFC = FF // 128


class Ctx:
    pass


_UC = [0]


def U(n):
    _UC[0] += 1
    return f"{n}_{_UC[0]}"


def make_consts():
    c = {}
    c["ident"] = np.eye(128, dtype=np.float32).astype(ml_dtypes.bfloat16)
    s = np.arange(128)[:, None]
    t = np.arange(128)[None, :]
    same = (s // 64) == (t // 64)
    c["m2f"] = (same & (s <= t)).astype(np.float32).astype(ml_dtypes.bfloat16)
    c["m2b"] = (same & (s >= t)).astype(np.float32).astype(ml_dtypes.bfloat16)
    return c


def psum_pools(nc, es, fw):
    P = Ctx()
    P.f32 = [es.enter_context(nc.psum_tensor(f"psf{i}", [128, 512], F32)) for i in range(6)]
    P.f32t = [Tok(f"psf{i}") for i in range(6)]
    P.bf = [es.enter_context(nc.psum_tensor(f"psb{i}", [128, 1024], BF16)) for i in range(2)]
    P.bft = [Tok(f"psb{i}") for i in range(2)]
    P.i = 0
    P.j = 0
    return P


def get_ps(P):
    k = P.i % len(P.f32)
    P.i += 1
    return P.f32[k], P.f32t[k]


def get_psb(P):
    k = P.j % len(P.bf)
    P.j += 1
    return P.bf[k], P.bft[k]


def cast_weight(fw, nc, dst, src, rows, cols, name="w", chunks=None):
    toks = []
    for c in range(0, cols, 2048):
        if chunks is not None and (c // 2048) not in chunks:
            toks.append(None)
            continue
        w = min(2048, cols - c)
        tok = Tok(f"{name}{c}")
        for r in range(0, rows, 128):
            fw.dma("pool", dst[r:r + 128, c:c + w], src[r:r + 128, c:c + w], writes=[tok], acc=True)
        toks.append(tok)
    return toks


def cast_weights_interleaved(fw, nc, items):
    nch = max(-(-it[3] // 2048) for it in items)
    toks = [[None] * (-(-it[3] // 2048)) for it in items]
    for ch in range(nch):
        for k, (dst, src, rows, cols, name) in enumerate(items):
            if ch * 2048 < cols:
                t = cast_weight(fw, nc, dst, src, rows, cols, name, chunks=[ch])
                toks[k][ch] = t[ch]
    return toks


def norm_T(fw, nc, P, S, x_src, row0, tt, gain_bc, t_gain):
    k = S.xi % 2
    S.xi += 1
    xt, t_x = S.xt[k], S.t_xt[k]
    fw.dma("sp", xt[:], x_src[row0:row0 + 128, :], writes=[t_x])
    fw.op(fw.act, lambda: nc.scalar.activation(out=S.junk[:], in_=xt[:], func=AF.Square),
          reads=[t_x], writes=[S.t_junk])
    fw.op(fw.dve, lambda: nc.vector.tensor_reduce(out=S.ss[:, 0:1], in_=S.junk[:], axis=AX.X, op=ALU.add),
          reads=[S.t_junk], writes=[S.t_ss])
    fw.op(fw.act, lambda: nc.scalar.activation(out=S.ss[:, 1:2], in_=S.ss[:, 0:1], func=AF.Sqrt,
                                               scale=1.0 / D, bias=S.eps[:, 0:1]),
          reads=[S.t_ss, S.t_eps], writes=[S.t_ss2])
    fw.op(fw.dve, lambda: nc.vector.reciprocal(out=S.ss[:, 2:3], in_=S.ss[:, 1:2]),
          reads=[S.t_ss2], writes=[S.t_rstd])
    fw.op(fw.dve, lambda: nc.vector.scalar_tensor_tensor(out=S.hn[:], in0=xt[:], scalar=S.ss[:, 2:3],
                                                         in1=gain_bc[:], op0=ALU.mult, op1=ALU.mult),
          reads=[t_x, S.t_rstd, t_gain], writes=[S.t_hn])
    for g in range(4):
        pb, t_pb = get_psb(P)
        fns = []
        for j in range(4):
            c = 4 * g + j
            fns.append(lambda j=j, c=c: nc.tensor.transpose(out=pb[:, j * 128:(j + 1) * 128],
                                                            in_=S.hn[:, c * 128:(c + 1) * 128],
                                                            identity=S.ident[:]))
        fw.mm_group(fns, reads=[S.t_hn, S.t_ident], writes=[t_pb])
        dst = S.hnT[:, 4 * g:4 * g + 4, tt * 128:(tt + 1) * 128]
        src = pb[:, 0:512].rearrange("p (j t) -> p j t", t=128)
        if g % 2 == 0:
            fw.op(fw.act, lambda: nc.scalar.copy(out=dst, in_=src), reads=[t_pb], writes=[S.t_hnT], acc=True)
        else:
            fw.op(fw.dve, lambda: nc.vector.tensor_copy(out=dst, in_=src), reads=[t_pb], writes=[S.t_hnT], acc=True)


def alloc_norm(nc, es, S, consts_ap):
    S.xt = [es.enter_context(nc.sbuf_tensor(U(f"xt{i}"), [128, D], F32)) for i in range(2)]
    S.t_xt = [Tok("xt0"), Tok("xt1")]
    S.xi = 0
    S.junk = es.enter_context(nc.sbuf_tensor(U("junk"), [128, D], F32)); S.t_junk = Tok("junk")
    S.ss = es.enter_context(nc.sbuf_tensor(U("ss"), [128, 4], F32))
    S.t_ss = Tok("ss"); S.t_ss2 = Tok("ss2"); S.t_rstd = Tok("rstd")
    S.hn = es.enter_context(nc.sbuf_tensor(U("hn"), [128, D], BF16)); S.t_hn = Tok("hn")
    S.hnT = es.enter_context(nc.sbuf_tensor(U("hnT"), [128, 16, 512], BF16)); S.t_hnT = Tok("hnT")


def alloc_common(nc, es, fw, S, consts):
    S.eps = es.enter_context(nc.sbuf_tensor(U("eps"), [128, 1], F32)); S.t_eps = Tok("eps")
    fw.op(fw.dve, lambda: nc.vector.memset(S.eps[:], EPS), writes=[S.t_eps])
    S.ident = es.enter_context(nc.sbuf_tensor(U("ident"), [128, 128], BF16)); S.t_ident = Tok("ident")
    fw.dma("sp", S.ident[:], consts["ident"][:, :], writes=[S.t_ident])
    S.gain = es.enter_context(nc.sbuf_tensor(U("gainbc"), [128, D], F32)); S.t_gain = Tok("gain")


def load_gain(fw, nc, S, gains, idx):
    fw.dma("sp", S.gain[:], gains[idx].partition_broadcast(128), writes=[S.t_gain])


def phase_hgrn_proj(fw, nc, P, S, A, L):
    with ExitStack() as es:
        alloc_norm(nc, es, S, None)
        wsl = [es.enter_context(nc.sbuf_tensor(U(f"wsl{i}"), [128, 16, 512], BF16)) for i in range(3)]
        t_wsl = [Tok("wsl0"), Tok("wsl1"), Tok("wsl2")]
        ob = [es.enter_context(nc.sbuf_tensor(U(f"ob{i}"), [128, 512], F32)) for i in range(3)]
        t_ob = [Tok(f"ob{i}") for i in range(3)]
        ob16 = [es.enter_context(nc.sbuf_tensor(U(f"obh{i}"), [128, 512], BF16)) for i in range(2)]
        t_ob16 = [Tok(f"obh{i}") for i in range(2)]
        load_gain(fw, nc, S, A.gains, 0)
        win = A.w_in_bf.rearrange("(c p) n -> p c n", p=128)
        oi = 0
        wi = 0
        for tb in range(L // 512):
            for tt in range(4):
                norm_T(fw, nc, P, S, A.x, tb * 512 + tt * 128, tt, S.gain, S.t_gain)
            for s in range(20):
                w, t_w = wsl[wi % 3], t_wsl[wi % 3]
                wi += 1
                fw.dma("sp", w[:], win[:, :, s * 512:(s + 1) * 512], reads=[A.t_w_in[s // 4]], writes=[t_w])
                tokmajor = 12 <= s < 16
                for j in range(4):
                    ps, t_ps = get_ps(P)
                    if not tokmajor:
                        fns = [lambda c=c: nc.tensor.matmul(ps[:, :], lhsT=w[:, c, j * 128:(j + 1) * 128],
                                                            rhs=S.hnT[:, c, :], start=(c == 0), stop=(c == 15))
                               for c in range(16)]
                        fw.mm_group(fns, reads=[t_w, S.t_hnT], writes=[t_ps])
                        o, t_o = ob[oi % 3], t_ob[oi % 3]
                        oi += 1
                        if oi % 2:
                            fw.op(fw.act, lambda: nc.scalar.copy(out=o[:], in_=ps[:, :]), reads=[t_ps], writes=[t_o])
                        else:
                            fw.op(fw.dve, lambda: nc.vector.tensor_copy(out=o[:], in_=ps[:, :]), reads=[t_ps], writes=[t_o])
                        srow = s if s < 12 else s - 4
                        r0 = srow * 512 + j * 128
                        fw.dma("sp", A.projT[r0:r0 + 128, tb * 512:(tb + 1) * 512], o[:], reads=[t_o],
                               writes=[A.t_projT], acc=True)
                    else:
                        tt = j
                        fns = [lambda c=c: nc.tensor.matmul(ps[:, :], lhsT=S.hnT[:, c, tt * 128:(tt + 1) * 128],
                                                            rhs=w[:, c, :], start=(c == 0), stop=(c == 15))
                               for c in range(16)]
                        fw.mm_group(fns, reads=[t_w, S.t_hnT], writes=[t_ps])
                        o, t_o = ob16[oi % 2], t_ob16[oi % 2]
                        oi += 1
                        if oi % 2:
                            fw.op(fw.act, lambda: nc.scalar.copy(out=o[:], in_=ps[:, :]), reads=[t_ps], writes=[t_o])
                        else:
                            fw.op(fw.dve, lambda: nc.vector.tensor_copy(out=o[:], in_=ps[:, :]), reads=[t_ps], writes=[t_o])
                        r0 = tb * 512 + tt * 128
                        fw.dma("sp", A.v_tm[r0:r0 + 128, (s - 12) * 512:(s - 11) * 512], o[:], reads=[t_o],
                               writes=[A.t_v_tm], acc=True)
        fw.barrier()


def phase_hgrn_scan(fw, nc, P, S, A, L):
    NT = L // 128
    NCH = L // 64
    with ExitStack() as es:
        sb = lambda n, shp, dt: es.enter_context(nc.sbuf_tensor(U(n), shp, dt))
        lbl = sb("lbl", [128, 3, 2, 16], F32); t_lbl = Tok("lbl")
        with nc.allow_non_contiguous_dma(reason="tiny param load"):
            for l in range(3):
                for d in range(2):
                    fw.dma("sp", lbl[:, l, d, :], A.lb_logits[l, d].rearrange("(h k) -> k h", k=128),
                           writes=[t_lbl], acc=True)
        ong = sb("ong", [128, 16], F32); t_ong = Tok("ong")
        with nc.allow_non_contiguous_dma(reason="tiny param load"):
            fw.dma("sp", ong[:], A.onorm.rearrange("(h k) -> k h", k=128), writes=[t_ong])
        lbe = sb("lbe", [128, 3, 32], F32); t_lbe = Tok("lbe")
        fw.op(fw.act, lambda: nc.scalar.activation(out=lbe[:], in_=lbl[:].rearrange("p l d h -> p l (d h)"), func=AF.Exp),
              reads=[t_lbl], writes=[t_lbe])
        lbs = sb("lbs", [128, 4, 32], F32); t_lbs = Tok("lbs")
        fw.op(fw.dve, lambda: nc.vector.tensor_tensor(out=lbs[:, 0, :], in0=lbe[:, 0, :], in1=lbe[:, 1, :], op=ALU.add),
              reads=[t_lbe], writes=[t_lbs])
        fw.op(fw.dve, lambda: nc.vector.tensor_tensor(out=lbs[:, 1, :], in0=lbs[:, 0, :], in1=lbe[:, 2, :], op=ALU.add),
              reads=[t_lbe, t_lbs], writes=[t_lbs])
        fw.op(fw.dve, lambda: nc.vector.reciprocal(out=lbs[:, 0, :], in_=lbs[:, 1, :]), reads=[t_lbs], writes=[t_lbs])
        fw.op(fw.dve, lambda: nc.vector.tensor_tensor(out=lbs[:, 2, :], in0=lbe[:, 0, :], in1=lbs[:, 0, :], op=ALU.mult),
              reads=[t_lbe, t_lbs], writes=[t_lbs])
        fw.op(fw.dve, lambda: nc.vector.tensor_scalar(out=lbs[:, 3, :], in0=lbs[:, 2, :], scalar1=-1.0, scalar2=1.0,
                                                      op0=ALU.mult, op1=ALU.add), reads=[t_lbs], writes=[t_lbs])
        rmask = sb("rmask", [128, L], BF16); t_rmask = Tok("rmask")
        fw.op(fw.dve, lambda: nc.vector.memset(rmask[:], 1.0), writes=[t_rmask])
        fw.op(fw.dve, lambda: nc.vector.memset(rmask[:, 0::64], 0.0), writes=[t_rmask])
        m2 = [sb("m2f", [128, 128], BF16), sb("m2b", [128, 128], BF16)]
        t_m2 = Tok("m2")
        fw.dma("sp", m2[0][:], A.consts["m2f"][:, :], writes=[t_m2], acc=True)
        fw.dma("sp", m2[1][:], A.consts["m2b"][:, :], writes=[t_m2], acc=True)
        ones = sb("ones", [128, 128], F32); t_ones = Tok("ones")
        fw.op(fw.dve, lambda: nc.vector.memset(ones[:], 1.0), writes=[t_ones])
        qs = sb("qs", [128, L], F32); t_qs = Tok("qs")
        w1 = sb("w1", [128, L], F32); t_w1 = Tok("w1")
        w2 = sb("w2", [128, L], F32); t_w2 = Tok("w2")
        w3 = sb("w3", [128, L], F32); t_w3 = Tok("w3")
        qt = [sb("qt0", [128, L], BF16), sb("qt1", [128, L], BF16)]; t_qt = [Tok("qt0"), Tok("qt1")]
        kt = [sb("kt0", [128, L], BF16), sb("kt1", [128, L], BF16)]; t_kt = [Tok("kt0"), Tok("kt1")]
        etot = [sb("etot0", [128, NCH], F32), sb("etot1", [128, NCH], F32)]; t_etot = [Tok("et0"), Tok("et1")]
        vtm = sb("vtm", [128, NT, 128], BF16); t_vtm = Tok("vtm")
        ktm = [sb("ktm0", [128, NT, 128], BF16), sb("ktm1", [128, NT, 128], BF16)]; t_ktm = [Tok("ktm0"), Tok("ktm1")]
        osum = sb("osum", [128, L], F32); t_osum = Tok("osum")
        Wf = [[sb(f"Wf{d}{k}", [128, 128], F32) for k in range(2)] for d in range(2)]
        t_Wf = [[Tok(f"Wf{d}{k}") for k in range(2)] for d in range(2)]
        wcur = [0, 0]
        Sb = [sb("Sb0", [128, 128], BF16), sb("Sb1", [128, 128], BF16)]; t_Sb = [Tok("Sb0"), Tok("Sb1")]
        tmp = [sb("tmp0", [128, 128], F32), sb("tmp1", [128, 128], F32)]; t_tmp = [Tok("tmp0"), Tok("tmp1")]
        atm = [sb("atm0", [128, 128], BF16), sb("atm1", [128, 128], BF16)]; t_atm = [Tok("atm0"), Tok("atm1")]
        oTb = sb("oTb", [128, L], BF16); t_oTb = Tok("oTb")

        for h in range(16):
            r = h * 128
            fw.dma("sp", qs[:], A.projT[r:r + 128, :], reads=[A.t_projT], writes=[t_qs])
            fw.dma("sp", vtm[:], A.v_tm[:, r:r + 128].rearrange("(n p) v -> p n v", p=128), reads=[A.t_v_tm],
                   writes=[t_vtm])
            fw.op(fw.act, lambda: nc.scalar.activation(out=qs[:], in_=qs[:], func=AF.Silu), reads=[t_qs], writes=[t_qs])
            for d in range(2):
                lbc = lbs[:, 2, d * 16 + h:d * 16 + h + 1]
                omc = lbs[:, 3, d * 16 + h:d * 16 + h + 1]
                fw.dma("sp", w1[:], A.projT[2048 * (d + 1) + r:2048 * (d + 1) + r + 128, :], reads=[A.t_projT], writes=[t_w1])
                fw.op(fw.act, lambda: nc.scalar.activation(out=w1[:], in_=w1[:], func=AF.Sigmoid),
                      reads=[t_w1], writes=[t_w1])
                fw.op(fw.dve, lambda: nc.vector.tensor_scalar(out=w1[:], in0=w1[:], scalar1=omc, scalar2=lbc,
                                                              op0=ALU.mult, op1=ALU.add), reads=[t_w1, t_lbs], writes=[t_w1])
                fw.op(fw.act, lambda: nc.scalar.activation(out=w2[:], in_=w1[:], func=AF.Ln), reads=[t_w1], writes=[t_w2])
                fw.op(fw.dve, lambda: nc.vector.tensor_scalar(out=w1[:], in0=w1[:], scalar1=-1.0, scalar2=1.0,
                                                              op0=ALU.mult, op1=ALU.add), reads=[t_w1], writes=[t_w1])
                fw.op(fw.dve, lambda: nc.vector.tensor_tensor_scan(out=w3[:], data0=rmask[:], data1=w2[:], initial=0.0,
                                                                   op0=ALU.mult, op1=ALU.add),
                      reads=[t_rmask, t_w2], writes=[t_w3])
                fw.op(fw.act, lambda: nc.scalar.activation(out=etot[d][:], in_=w3[:, 63::64], func=AF.Exp),
                      reads=[t_w3], writes=[t_etot[d]])
                if d == 1:
                    fw.op(fw.dve, lambda: nc.vector.tensor_tensor(out=w3[:], in0=w3[:], in1=w2[:], op=ALU.subtract),
                          reads=[t_w3, t_w2], writes=[t_w3])
                sgn = 1.0 if d == 0 else -1.0
                fw.op(fw.act, lambda: nc.scalar.activation(out=w2[:], in_=w3[:], func=AF.Exp, scale=sgn),
                      reads=[t_w3], writes=[t_w2])
                fw.op(fw.dve, lambda: nc.vector.tensor_tensor(out=qt[d][:], in0=qs[:], in1=w2[:], op=ALU.mult),
                      reads=[t_qs, t_w2], writes=[t_qt[d]])
                fw.op(fw.act, lambda: nc.scalar.activation(out=w2[:], in_=w3[:], func=AF.Exp, scale=-sgn),
                      reads=[t_w3], writes=[t_w2])
                fw.op(fw.dve, lambda: nc.vector.tensor_tensor(out=kt[d][:], in0=w1[:], in1=w2[:], op=ALU.mult),
                      reads=[t_w1, t_w2], writes=[t_kt[d]])
                for g in range(0, NT, 4):
                    pb, t_pb = get_psb(P)
                    fns = [lambda j=j: nc.tensor.transpose(out=pb[:, j * 128:(j + 1) * 128],
                                                           in_=kt[d][:, (g + j) * 128:(g + j + 1) * 128],
                                                           identity=S.ident[:]) for j in range(4)]
                    fw.mm_group(fns, reads=[t_kt[d], S.t_ident], writes=[t_pb])
                    fw.op(fw.act, lambda: nc.scalar.copy(out=ktm[d][:, g:g + 4, :],
                                                         in_=pb[:, 0:512].rearrange("p (j t) -> p j t", t=128)),
                          reads=[t_pb], writes=[t_ktm[d]], acc=True)
                fw.op(fw.dve, lambda: nc.vector.memset(Wf[d][wcur[d]][:], 0.0), writes=[t_Wf[d][wcur[d]]])

            def tile_steps(d, n):
                c0 = n * 128
                po, t_po = P.f32[3 * d], P.f32t[3 * d]
                tA, t_tA = P.f32[3 * d + 1], P.f32t[3 * d + 1]
                tB, t_tB = P.f32[3 * d + 2], P.f32t[3 * d + 2]
                order = [0, 1] if d == 0 else [1, 0]
                fw.mm_group([lambda: nc.tensor.matmul(tA[:, 0:128], lhsT=kt[d][:, c0:c0 + 128],
                                                      rhs=qt[d][:, c0:c0 + 128], start=True, stop=True)],
                            reads=[t_kt[d], t_qt[d]], writes=[t_tA])
                yield
                fw.op(fw.dve, lambda: nc.vector.tensor_tensor(out=atm[d][:], in0=tA[:, 0:128], in1=m2[d][:], op=ALU.mult),
                      reads=[t_tA, t_m2], writes=[t_atm[d]])
                yield
                rows_a = slice(order[0] * 64, order[0] * 64 + 64)
                rows_b = slice(order[1] * 64, order[1] * 64 + 64)
                fw.mm_group([lambda: nc.tensor.matmul(tB[:, 0:128], lhsT=ktm[d][rows_a, n, :],
                                                      rhs=vtm[rows_a, n, :], start=True, stop=True)],
                            reads=[t_ktm[d], t_vtm], writes=[t_tB])
                yield
                fw.mm_group([lambda: nc.tensor.matmul(po[:, 0:128], lhsT=vtm[:, n, :], rhs=atm[d][:],
                                                      start=True, stop=False)],
                            reads=[t_vtm, t_atm[d]], writes=[t_po])
                fw.mm_group([lambda: nc.tensor.matmul(tA[:, 0:128], lhsT=ktm[d][rows_b, n, :],
                                                      rhs=vtm[rows_b, n, :], start=True, stop=True)],
                            reads=[t_ktm[d], t_vtm], writes=[t_tA])
                yield
                for ci, half in enumerate(order):
                    ch = 2 * n + half
                    cs = c0 + half * 64
                    last = (ci == 1)
                    pk, t_pk = (tB, t_tB) if ci == 0 else (tA, t_tA)
                    ei = max(ch - 1, 0) if d == 0 else ch
                    esc = etot[d][:, ei:ei + 1]
                    cur = wcur[d]
                    nxt = 1 - cur
                    Wc, t_Wc = Wf[d][cur], t_Wf[d][cur]
                    Wn, t_Wn = Wf[d][nxt], t_Wf[d][nxt]
                    fw.op(fw.act, lambda: nc.scalar.activation(out=Sb[d][:], in_=Wc[:], func=AF.Copy, scale=esc),
                          reads=[t_Wc, t_etot[d]], writes=[t_Sb[d]])
                    fw.op(fw.dve, lambda: nc.vector.scalar_tensor_tensor(out=Wn[:], in0=Wc[:], scalar=esc, in1=pk[:, 0:128],
                                                                         op0=ALU.mult, op1=ALU.add),
                          reads=[t_Wc, t_etot[d], t_pk], writes=[t_Wn])
                    wcur[d] = nxt
                    yield
                    fw.mm_group([lambda: nc.tensor.matmul(po[:, half * 64:half * 64 + 64], lhsT=Sb[d][:],
                                                          rhs=qt[d][:, cs:cs + 64], start=False, stop=last)],
                                reads=[t_Sb[d], t_qt[d]], writes=[t_po])
                    yield
                first = (n < NT // 2) if d == 0 else (n >= NT // 2)
                if first:
                    fw.op(fw.act, lambda: nc.scalar.copy(out=osum[:, c0:c0 + 128], in_=po[:, 0:128]),
                          reads=[t_po], writes=[t_osum], acc=True)
                else:
                    fw.op(fw.dve, lambda: nc.vector.tensor_tensor(out=osum[:, c0:c0 + 128], in0=po[:, 0:128],
                                                                  in1=osum[:, c0:c0 + 128], op=ALU.add),
                          reads=[t_po, t_osum], writes=[t_osum], acc=True)
                yield

            def chain(d):
                for i in range(NT):
                    n = i if d == 0 else NT - 1 - i
                    yield from tile_steps(d, n)

            gens = [chain(0), chain(1)]
            alive = [True, True]
            while any(alive):
                for gi_, g_ in enumerate(gens):
                    if alive[gi_]:
                        try:
                            next(g_)
                        except StopIteration:
                            alive[gi_] = False
            fw.dma("sp", w1[:], A.projT[6144 + r:6144 + r + 128, :], reads=[A.t_projT], writes=[t_w1])
            fw.op(fw.act, lambda: nc.scalar.activation(out=w2[:], in_=osum[:], func=AF.Square), reads=[t_osum], writes=[t_w2])
            for b in range(L // 512):
                ps, t_ps = get_ps(P)
                fw.mm_group([lambda: nc.tensor.matmul(ps[:, :], lhsT=ones[:], rhs=w2[:, b * 512:(b + 1) * 512],
                                                      start=True, stop=True)], reads=[t_ones, t_w2], writes=[t_ps])
                fw.op(fw.act, lambda: nc.scalar.activation(out=w3[:, b * 512:(b + 1) * 512], in_=ps[:, :], func=AF.Sqrt,
                                                           scale=1.0 / 128, bias=S.eps[:, 0:1]),
                      reads=[t_ps, S.t_eps], writes=[t_w3], acc=True)
            fw.op(fw.dve, lambda: nc.vector.reciprocal(out=w3[:], in_=w3[:]), reads=[t_w3], writes=[t_w3])
            fw.op(fw.dve, lambda: nc.vector.scalar_tensor_tensor(out=w2[:], in0=osum[:], scalar=ong[:, h:h + 1], in1=w3[:],
                                                                 op0=ALU.mult, op1=ALU.mult),
                  reads=[t_osum, t_ong, t_w3], writes=[t_w2])
            fw.op(fw.act, lambda: nc.scalar.activation(out=w1[:], in_=w1[:], func=AF.Silu), reads=[t_w1], writes=[t_w1])
            fw.op(fw.dve, lambda: nc.vector.tensor_tensor(out=oTb[:], in0=w2[:], in1=w1[:], op=ALU.mult),
                  reads=[t_w2, t_w1], writes=[t_oTb])
            fw.dma("sp", A.oT[r:r + 128, :], oTb[:], reads=[t_oTb], writes=[A.t_oT], acc=True)
        fw.barrier()


def phase_wout(fw, nc, P, S, A, L, oT, t_oT, w_bf, t_w, x_in, t_xin, x_out, t_xout):
    with ExitStack() as es:
        sb = lambda n, shp, dt: es.enter_context(nc.sbuf_tensor(U(n), shp, dt))
        wo = sb("wo", [128, 16, D], BF16); t_wo = Tok("wo")
        wv = w_bf.rearrange("(c p) n -> p c n", p=128)
        for c in range(0, 16, 4):
            fw.dma("sp", wo[:, c:c + 4, :], wv[:, c:c + 4, :], reads=t_w, writes=[t_wo], acc=True)
        ob = [sb("oblk0", [128, 16, 512], BF16), sb("oblk1", [128, 16, 512], BF16)]; t_ob = [Tok("oblk0"), Tok("oblk1")]
        xt = [sb("xr0", [128, D], F32), sb("xr1", [128, D], F32)]; t_xt = [Tok("xr0"), Tok("xr1")]
        ov = oT.rearrange("(h p) t -> p h t", p=128)
        xi = 0
        for tb in range(L // 512):
            o, t_o = ob[tb % 2], t_ob[tb % 2]
            fw.dma("sp", o[:], ov[:, :, tb * 512:(tb + 1) * 512], reads=[t_oT], writes=[t_o])
            for tt in range(4):
                r0 = tb * 512 + tt * 128
                x, t_x = xt[xi % 2], t_xt[xi % 2]
                xi += 1
                fw.dma("sp", x[:], x_in[r0:r0 + 128, :], reads=[t_xin], writes=[t_x])
                for nb in range(4):
                    ps, t_ps = get_ps(P)
                    fns = [lambda h=h: nc.tensor.matmul(ps[:, :], lhsT=o[:, h, tt * 128:(tt + 1) * 128],
                                                        rhs=wo[:, h, nb * 512:(nb + 1) * 512], start=(h == 0), stop=(h == 15))
                           for h in range(16)]
                    fw.mm_group(fns, reads=[t_o, t_wo], writes=[t_ps])
                    fw.op(fw.dve, lambda: nc.vector.tensor_tensor(out=x[:, nb * 512:(nb + 1) * 512], in0=ps[:, :],
                                                                  in1=x[:, nb * 512:(nb + 1) * 512], op=ALU.add),
                          reads=[t_ps, t_x], writes=[t_x], acc=True)
                fw.dma("sp", x_out[r0:r0 + 128, :], x[:], reads=[t_x], writes=[t_xout], acc=True)
        fw.barrier()


def phase_ffn(fw, nc, P, S, L, F, wg_bf, wu_bf, wd_bf, t_wg, t_wu, t_wd, x_in, t_xin, x_out, t_xout,
              gains=None, gain_idx=None, hn_in=None, t_hn_in=None, rowscale=None, t_rowscale=None, barrier=True):
    nfc = F // 128
    h1 = (nfc + 1) // 2
    with ExitStack() as es:
        sb = lambda n, shp, dt: es.enter_context(nc.sbuf_tensor(U(n), shp, dt))
        if hn_in is None:
            alloc_norm(nc, es, S, None)
            load_gain(fw, nc, S, gains, gain_idx)
        else:
            S.hn2 = [sb("hnl0", [128, D], BF16), sb("hnl1", [128, D], BF16)]; S.t_hn2 = [Tok("hnl0"), Tok("hnl1")]
            S.hnT = sb("hnT", [128, 16, 512], BF16); S.t_hnT = Tok("hnT")
            rs = sb("rs", [128, L // 128], F32); t_rs = Tok("rs")
            with nc.allow_non_contiguous_dma(reason="tiny"):
                fw.dma("sp", rs[:], rowscale.rearrange("(n p) -> p n", p=128), reads=[t_rowscale], writes=[t_rs])
        hT = sb("hT", [128, nfc, 512], BF16); t_hT = Tok("hT")
        wgs = [sb("wgs0", [128, 16, 256], BF16), sb("wgs1", [128, 16, 256], BF16)]; t_wgs = [Tok("wgs0"), Tok("wgs1")]
        wus = [sb("wus0", [128, 16, 256], BF16), sb("wus1", [128, 16, 256], BF16)]; t_wus = [Tok("wus0"), Tok("wus1")]
        wds = [sb("wds0", [128, h1, 512], BF16), sb("wds1", [128, h1, 512], BF16)]; t_wds = [Tok("wds0"), Tok("wds1")]
        sg = [sb("sg0", [128, 512], F32), sb("sg1", [128, 512], F32)]; t_sg = [Tok("sg0"), Tok("sg1")]
        xs = [sb(f"xs{i}", [128, 512], F32) for i in range(3)]; t_xs = [Tok(f"xs{i}") for i in range(3)]
        wgv = wg_bf.rearrange("(c p) n -> p c n", p=128)
        wuv = wu_bf.rearrange("(c p) n -> p c n", p=128)
        wdv = wd_bf.rearrange("(c p) n -> p c n", p=128)
        slabs = []
        c0 = 0
        while c0 < F:
            w = min(256, F - c0)
            slabs.append((c0, w))
            c0 += w
        wi = 0; si = 0; xi = 0; di = 0
        for tb in range(L // 512):
            for tt in range(4):
                if hn_in is None:
                    norm_T(fw, nc, P, S, x_in, tb * 512 + tt * 128, tt, S.gain, S.t_gain)
                else:
                    r0 = tb * 512 + tt * 128
                    hb, t_hb = S.hn2[tt % 2], S.t_hn2[tt % 2]
                    fw.dma("sp", hb[:], hn_in[r0:r0 + 128, :], reads=[t_hn_in], writes=[t_hb])
                    for g in range(4):
                        pb, t_pb = get_psb(P)
                        fns = [lambda j=j: nc.tensor.transpose(out=pb[:, j * 128:(j + 1) * 128],
                                                               in_=hb[:, (4 * g + j) * 128:(4 * g + j + 1) * 128],
                                                               identity=S.ident[:]) for j in range(4)]
                        fw.mm_group(fns, reads=[t_hb, S.t_ident], writes=[t_pb])
                        dst = S.hnT[:, 4 * g:4 * g + 4, tt * 128:(tt + 1) * 128]
                        src = pb[:, 0:512].rearrange("p (j t) -> p j t", t=128)
                        if g % 2 == 0:
                            fw.op(fw.act, lambda: nc.scalar.copy(out=dst, in_=src), reads=[t_pb], writes=[S.t_hnT], acc=True)
                        else:
                            fw.op(fw.dve, lambda: nc.vector.tensor_copy(out=dst, in_=src), reads=[t_pb], writes=[S.t_hnT], acc=True)
            for (c0, w) in slabs:
                wg, t_wgs_ = wgs[wi % 2], t_wgs[wi % 2]
                wu, t_wus_ = wus[wi % 2], t_wus[wi % 2]
                wi += 1
                fw.dma("sp", wg[:, :, 0:w], wgv[:, :, c0:c0 + w], reads=[t_wg[c0 // 2048]], writes=[t_wgs_])
                fw.dma("sp", wu[:, :, 0:w], wuv[:, :, c0:c0 + w], reads=[t_wu[c0 // 2048]], writes=[t_wus_])
                for j in range(w // 128):
                    fc = c0 // 128 + j
                    psg, t_psg = get_ps(P)
                    fw.mm_group([lambda c=c: nc.tensor.matmul(psg[:, :], lhsT=wg[:, c, j * 128:(j + 1) * 128],
                                                              rhs=S.hnT[:, c, :], start=(c == 0), stop=(c == 15))
                                 for c in range(16)], reads=[t_wgs_, S.t_hnT], writes=[t_psg])
                    psu, t_psu = get_ps(P)
                    fw.mm_group([lambda c=c: nc.tensor.matmul(psu[:, :], lhsT=wu[:, c, j * 128:(j + 1) * 128],
                                                              rhs=S.hnT[:, c, :], start=(c == 0), stop=(c == 15))
                                 for c in range(16)], reads=[t_wus_, S.t_hnT], writes=[t_psu])
                    s_, t_s = sg[si % 2], t_sg[si % 2]
                    si += 1
                    fw.op(fw.act, lambda: nc.scalar.activation(out=s_[:], in_=psg[:, :], func=AF.Silu),
                          reads=[t_psg], writes=[t_s])
                    fw.op(fw.dve, lambda: nc.vector.tensor_tensor(out=hT[:, fc, :], in0=s_[:], in1=psu[:, :], op=ALU.mult),
                          reads=[t_s, t_psu], writes=[t_hT], acc=True)
            for nb in range(4):
                banks = [get_ps(P) for _ in range(4)]
                for half in range(2):
                    f0 = 0 if half == 0 else h1
                    f1 = h1 if half == 0 else nfc
                    wd, t_wds_ = wds[di % 2], t_wds[di % 2]
                    di += 1
                    fw.dma("sp", wd[:, 0:f1 - f0, :], wdv[:, f0:f1, nb * 512:(nb + 1) * 512], reads=t_wd, writes=[t_wds_])
                    for tt in range(4):
                        ps, t_ps = banks[tt]
                        fw.mm_group([lambda fc=fc: nc.tensor.matmul(ps[:, :], lhsT=hT[:, fc, tt * 128:(tt + 1) * 128],
                                                                    rhs=wd[:, fc - f0, :], start=(fc == 0), stop=(fc == nfc - 1))
                                     for fc in range(f0, f1)], reads=[t_hT, t_wds_], writes=[t_ps])
                for tt in range(4):
                    ps, t_ps = banks[tt]
                    r0 = tb * 512 + tt * 128
                    x, t_x = xs[xi % 3], t_xs[xi % 3]
                    xi += 1
                    if hn_in is None:
                        fw.dma("sp", x[:], x_in[r0:r0 + 128, nb * 512:(nb + 1) * 512], reads=[t_xin], writes=[t_x])
                        fw.op(fw.dve, lambda: nc.vector.tensor_tensor(out=x[:], in0=ps[:, :], in1=x[:], op=ALU.add),
                              reads=[t_ps, t_x], writes=[t_x])
                    else:
                        n = tb * 4 + tt
                        fw.op(fw.act, lambda: nc.scalar.activation(out=x[:], in_=ps[:, :], func=AF.Copy, scale=rs[:, n:n + 1]),
                              reads=[t_ps, t_rs], writes=[t_x])
                    fw.dma("sp", x_out[r0:r0 + 128, nb * 512:(nb + 1) * 512], x[:], reads=[t_x], writes=[t_xout], acc=True)
        if barrier:
            fw.barrier()


def attn_consts(L):
    c = {}
    half = 64
    inv_freq = (np.float32(10000.0) ** (-np.arange(half, dtype=np.float32) * np.float32(2.0) / np.float32(128))).astype(np.float32)
    pos = np.arange(L, dtype=np.float32)
    ang = (pos[:, None] * inv_freq[None, :]).astype(np.float32)
    c["cos"] = np.cos(ang).astype(np.float32)
    c["sin"] = np.sin(ang).astype(np.float32)
    sl = np.arange(128)[:, None, None]
    j = np.arange(23)[None, :, None]
    tl = np.arange(128)[None, None, :]
    off = (tl - sl) - 128 * (11 - j)
    a = np.abs(off)
    m = (a <= 64).astype(np.float32) + ((off % 4 == 0) & (a <= 256)).astype(np.float32) + ((off % 16 == 0) & (a <= 1024)).astype(np.float32)
    c["amask"] = m.reshape(128, 23 * 128).astype(ml_dtypes.bfloat16)
    c["ident32"] = np.eye(128, dtype=np.float32)
    return c


def get_ps_sub(P, lo, hi, key):
    n = hi - lo
    i = getattr(P, key, 0)
    setattr(P, key, i + 1)
    k = lo + (i % n)
    return P.f32[k], P.f32t[k]


def phase_attn_proj(fw, nc, P, S, A, L):
    NT = L // 128
    with ExitStack() as es:
        sb = lambda n, shp, dt: es.enter_context(nc.sbuf_tensor(U(n), shp, dt))
        alloc_norm(nc, es, S, None)
        load_gain(fw, nc, S, A.gains, 2)
        wsl = [sb("awsl0", [128, 16, 512], BF16), sb("awsl1", [128, 16, 512], BF16)]; t_wsl = [Tok("awsl0"), Tok("awsl1")]
        cosT = sb("cosT", [128, NT, 64], F32); sinT = sb("sinT", [128, NT, 64], F32); t_cs = Tok("cs")
        fw.dma("sp", cosT[:], A.consts["cos"].rearrange("(n p) f -> p n f", p=128), writes=[t_cs], acc=True)
        fw.dma("sp", sinT[:], A.consts["sin"].rearrange("(n p) f -> p n f", p=128), writes=[t_cs], acc=True)
        gqk = sb("gqk", [128, 2, 128], F32); t_gqk = Tok("gqk")
        fw.dma("sp", gqk[:, 0, :], A.q_gain.partition_broadcast(128), writes=[t_gqk], acc=True)
        fw.dma("sp", gqk[:, 1, :], A.k_gain.partition_broadcast(128), writes=[t_gqk], acc=True)
        fw.op(fw.act, lambda: nc.scalar.mul(out=gqk[:, 0, :], in_=gqk[:, 0, :], mul=float(128 ** -0.5)), reads=[t_gqk], writes=[t_gqk])
        sq = [sb(f"asq{i}", [128, 512], F32) for i in range(4)]; t_sq = [Tok(f"asq{i}") for i in range(4)]
        st = [sb(f"ast{i}", [128, 12], F32) for i in range(4)]; t_st = [Tok(f"ast{i}") for i in range(4)]
        t1 = [sb(f"at1{i}", [128, 512], F32) for i in range(4)]; t_t1 = [Tok(f"at1{i}") for i in range(4)]
        ra = [sb(f"ara{i}", [128, 2, 4, 64], F32) for i in range(4)]; t_ra = [Tok(f"ara{i}") for i in range(4)]
        rb = [sb(f"arb{i}", [128, 2, 4, 64], F32) for i in range(4)]; t_rb = [Tok(f"arb{i}") for i in range(4)]
        qr = [sb(f"aqr{i}", [128, 512], BF16) for i in range(4)]; t_qr = [Tok(f"aqr{i}") for i in range(4)]
        stg = [sb("astg0", [128, 4, 512], BF16), sb("astg1", [128, 4, 512], BF16)]; t_stg = [Tok("astg0"), Tok("astg1")]
        vo = [sb("avo0", [128, 512], BF16), sb("avo1", [128, 512], BF16)]; t_vo = [Tok("avo0"), Tok("avo1")]
        wv = A.wqkv_bf.rearrange("(c p) n -> p c n", p=128)
        wi = 0; qi = 0; vi = 0; gi = 0
        for tb in range(L // 512):
            for tt in range(4):
                norm_T(fw, nc, P, S, A.x2, tb * 512 + tt * 128, tt, S.gain, S.t_gain)
            for s in range(12):
                w, t_w = wsl[wi % 2], t_wsl[wi % 2]
                wi += 1
                fw.dma("sp", w[:], wv[:, :, s * 512:(s + 1) * 512], reads=[A.t_wqkv[s // 4]], writes=[t_w])
                if s < 8:
                    sg_, t_sg = stg[gi % 2], t_stg[gi % 2]
                    gi += 1
                def tile_gen(tt):
                    n = tb * 4 + tt
                    r0 = n * 128
                    ps, t_ps = P.f32[tt], P.f32t[tt]
                    fw.mm_group([lambda c=c: nc.tensor.matmul(ps[:, :], lhsT=S.hnT[:, c, tt * 128:(tt + 1) * 128],
                                                              rhs=w[:, c, :], start=(c == 0), stop=(c == 15))
                                 for c in range(16)], reads=[t_w, S.t_hnT], writes=[t_ps])
                    yield
                    if s >= 8:
                        o, t_o = vo[tt % 2], t_vo[tt % 2]
                        fw.op(fw.act, lambda: nc.scalar.copy(out=o[:], in_=ps[:, :]), reads=[t_ps], writes=[t_o])
                        fw.dma("sp", A.v_att[r0:r0 + 128, (s - 8) * 512:(s - 7) * 512], o[:], reads=[t_o],
                               writes=[A.t_v_att], acc=True)
                        return
                    which = 0 if s < 4 else 1
                    sq_, t_sq_ = sq[tt], t_sq[tt]
                    st_, t_st_ = st[tt], t_st[tt]
                    t1_, t_t1_ = t1[tt], t_t1[tt]
                    ra_, t_ra_ = ra[tt], t_ra[tt]
                    rb_, t_rb_ = rb[tt], t_rb[tt]
                    q_, t_q = qr[tt], t_qr[tt]
                    fw.op(fw.act, lambda: nc.scalar.activation(out=sq_[:], in_=ps[:, :], func=AF.Square), reads=[t_ps], writes=[t_sq_])
                    yield
                    fw.op(fw.dve, lambda: nc.vector.tensor_reduce(out=st_[:, 0:4], in_=sq_[:].rearrange("p (h d) -> p h d", d=128),
                                                                  axis=AX.X, op=ALU.add), reads=[t_sq_], writes=[t_st_])
                    yield
                    fw.op(fw.act, lambda: nc.scalar.activation(out=st_[:, 4:8], in_=st_[:, 0:4], func=AF.Sqrt, scale=1.0 / 128,
                                                               bias=S.eps[:, 0:1]), reads=[t_st_, S.t_eps], writes=[t_st_])
                    yield
                    fw.op(fw.dve, lambda: nc.vector.reciprocal(out=st_[:, 8:12], in_=st_[:, 4:8]), reads=[t_st_], writes=[t_st_])
                    yield
                    t1v = t1_[:].rearrange("p (h d) -> p h d", d=128)
                    fw.op(fw.dve, lambda: nc.vector.tensor_tensor(out=t1v, in0=ps[:, :].rearrange("p (h d) -> p h d", d=128),
                                                                  in1=st_[:, 8:12].unsqueeze(2).to_broadcast([128, 4, 128]), op=ALU.mult),
                          reads=[t_ps, t_st_], writes=[t_t1_])
                    yield
                    fw.op(fw.dve, lambda: nc.vector.tensor_tensor(out=t1v, in0=t1v,
                                                                   in1=gqk[:, which:which + 1, :].to_broadcast([128, 4, 128]), op=ALU.mult),
                          reads=[t_t1_, t_gqk], writes=[t_t1_])
                    yield
                    x1v = t1v[:, :, 0:64]
                    x2v = t1v[:, :, 64:128]
                    cb = cosT[:, n:n + 1, :].to_broadcast([128, 4, 64])
                    sbb = sinT[:, n:n + 1, :].to_broadcast([128, 4, 64])
                    qv = q_[:].rearrange("p (h d) -> p h d", d=128)
                    fw.op(fw.dve, lambda: nc.vector.tensor_tensor(out=ra_[:, 0], in0=x1v, in1=cb, op=ALU.mult), reads=[t_t1_, t_cs], writes=[t_ra_])
                    fw.op(fw.dve, lambda: nc.vector.tensor_tensor(out=rb_[:, 0], in0=x2v, in1=sbb, op=ALU.mult), reads=[t_t1_, t_cs], writes=[t_rb_])
                    yield
                    fw.op(fw.dve, lambda: nc.vector.tensor_tensor(out=ra_[:, 1], in0=x2v, in1=cb, op=ALU.mult), reads=[t_t1_, t_cs], writes=[t_ra_], acc=True)
                    fw.op(fw.dve, lambda: nc.vector.tensor_tensor(out=rb_[:, 1], in0=x1v, in1=sbb, op=ALU.mult), reads=[t_t1_, t_cs], writes=[t_rb_], acc=True)
                    yield
                    fw.op(fw.dve, lambda: nc.vector.tensor_tensor(out=qv[:, :, 0:64], in0=ra_[:, 0], in1=rb_[:, 0], op=ALU.subtract),
                          reads=[t_ra_, t_rb_], writes=[t_q])
                    fw.op(fw.dve, lambda: nc.vector.tensor_tensor(out=qv[:, :, 64:128], in0=ra_[:, 1], in1=rb_[:, 1], op=ALU.add),
                          reads=[t_ra_, t_rb_], writes=[t_q], acc=True)
                    yield
                    pb, t_pb = get_psb(P)
                    fw.mm_group([lambda j=j: nc.tensor.transpose(out=pb[:, j * 128:(j + 1) * 128], in_=q_[:, j * 128:(j + 1) * 128],
                                                                 identity=S.ident[:]) for j in range(4)],
                                reads=[t_q, S.t_ident], writes=[t_pb])
                    fw.op(fw.act, lambda: nc.scalar.copy(out=sg_[:, :, tt * 128:(tt + 1) * 128],
                                                         in_=pb[:, 0:512].rearrange("p (j t) -> p j t", t=128)),
                          reads=[t_pb], writes=[t_sg], acc=True)
                    yield

                gens = [tile_gen(tt) for tt in range(4)]
                alive = [True] * 4
                while any(alive):
                    for gi_, g_ in enumerate(gens):
                        if alive[gi_]:
                            try:
                                next(g_)
                            except StopIteration:
                                alive[gi_] = False
                if s < 8:
                    dstT = A.qT if s < 4 else A.kT
                    t_dst = A.t_qT if s < 4 else A.t_kT
                    h0 = (s % 4) * 4
                    for j in range(4):
                        fw.dma("sp", dstT[(h0 + j) * 128:(h0 + j + 1) * 128, tb * 512:(tb + 1) * 512], sg_[:, j, :],
                               reads=[t_sg], writes=[t_dst], acc=True)
        fw.barrier()


def phase_attn_core(fw, nc, P, S, A, L):
    NT = L // 128
    NG = L // 512
    with ExitStack() as es:
        sb = lambda n, shp, dt: es.enter_context(nc.sbuf_tensor(U(n), shp, dt))
        amask = sb("amask", [128, 23 * 128], BF16); t_amask = Tok("amask")
        fw.dma("sp", amask[:], A.consts["amask"][:, :], writes=[t_amask])
        onesb = sb("onesb", [128, 128], BF16); t_onesb = Tok("onesb")
        fw.op(fw.dve, lambda: nc.vector.memset(onesb[:], 1.0), writes=[t_onesb])
        qT = [sb("aq0", [128, L], BF16), sb("aq1", [128, L], BF16)]; t_q = [Tok("aq0"), Tok("aq1")]
        kT = [sb("ak0", [128, L], BF16), sb("ak1", [128, L], BF16)]; t_k = [Tok("ak0"), Tok("ak1")]
        vh = [sb("av0", [128, NT, 128], BF16), sb("av1", [128, NT, 128], BF16)]; t_v = [Tok("av0"), Tok("av1")]
        oh = [sb("ao0", [128, L], BF16), sb("ao1", [128, L], BF16)]; t_o = [Tok("ao0"), Tok("ao1")]
        pe_ = [sb(f"ape{i}", [128, 512], BF16) for i in range(4)]; t_pe = [Tok(f"ape{i}") for i in range(4)]
        pm = [sb(f"apm{i}", [128, 512], BF16) for i in range(4)]; t_pm = [Tok(f"apm{i}") for i in range(4)]
        rden = [sb("arden0", [128, 512], F32), sb("arden1", [128, 512], F32)]; t_rden = [Tok("arden0"), Tok("arden1")]
        LA = 3
        iters = []
        for h in range(16):
            for g in range(NG):
                qb0 = 4 * g
                kbs = list(range(max(0, qb0 - 8), min(NT, qb0 + 12)))
                for ki, kb in enumerate(kbs):
                    iters.append((h, g, ki, kb, len(kbs)))
        po, t_po = P.f32[4], P.f32t[4]
        pd, t_pd = P.f32[5], P.f32t[5]
        rden_ = rden[0]; t_rden_ = t_rden[0]

        def load_head(h):
            b = h % 2
            r = h * 128
            fw.dma("sp", qT[b][:], A.qT[r:r + 128, :], reads=[A.t_qT], writes=[t_q[b]])
            fw.dma("sp", kT[b][:], A.kT[r:r + 128, :], reads=[A.t_kT], writes=[t_k[b]])
            fw.dma("sp", vh[b][:], A.v_att[:, r:r + 128].rearrange("(n p) v -> p n v", p=128), reads=[A.t_v_att], writes=[t_v[b]])

        def emit_st(i):
            h, g, ki, kb, nk = iters[i]
            b = h % 2
            if g == 0 and ki == 0 and h == 0:
                load_head(0)
            ps, t_ps = P.f32[i % 4], P.f32t[i % 4]
            t0 = g * 512
            fw.mm_group([lambda: nc.tensor.matmul(ps[:, :], lhsT=kT[b][:, kb * 128:(kb + 1) * 128],
                                                  rhs=qT[b][:, t0:t0 + 512], start=True, stop=True)],
                        reads=[t_k[b], t_q[b]], writes=[t_ps])

        def emit_rest(i):
            h, g, ki, kb, nk = iters[i]
            b = h % 2
            qb0 = 4 * g
            t0 = g * 512
            ps, t_ps = P.f32[i % 4], P.f32t[i % 4]
            e_, t_e = pe_[i % 4], t_pe[i % 4]
            m_, t_m = pm[i % 4], t_pm[i % 4]
            if g == 0 and ki == 0 and h + 1 < 16:
                load_head(h + 1)
            fw.op(fw.act, lambda: nc.scalar.activation(out=e_[:], in_=ps[:, :], func=AF.Exp), reads=[t_ps], writes=[t_e])
            j0 = 11 - (kb - qb0)
            use_dve = (i % 3) != 2
            eng = fw.dve if use_dve else fw.pool
            raw = nc.vector if use_dve else nc.gpsimd
            fw.op(eng, lambda: raw.tensor_tensor(out=m_[:], in0=e_[:], in1=amask[:, j0 * 128:j0 * 128 + 512], op=ALU.mult),
                  reads=[t_e, t_amask], writes=[t_m])
            first = ki == 0
            last = ki == nk - 1
            fw.mm_group([lambda: nc.tensor.matmul(po[:, :], lhsT=vh[b][:, kb, :], rhs=m_[:], start=first, stop=last)],
                        reads=[t_v[b], t_m], writes=[t_po])
            fw.mm_group([lambda: nc.tensor.matmul(pd[:, :], lhsT=onesb[:], rhs=m_[:], start=first, stop=last)],
                        reads=[t_onesb, t_m], writes=[t_pd])
            if last:
                fw.op(fw.dve, lambda: nc.vector.reciprocal(out=rden_[:], in_=pd[:, :]), reads=[t_pd], writes=[t_rden_])
                fw.op(fw.dve, lambda: nc.vector.tensor_tensor(out=oh[b][:, t0:t0 + 512], in0=po[:, :], in1=rden_[:], op=ALU.mult),
                      reads=[t_po, t_rden_], writes=[t_o[b]], acc=True)
                if g == NG - 1:
                    r = h * 128
                    fw.dma("sp", A.oT[r:r + 128, :], oh[b][:], reads=[t_o[b]], writes=[A.t_oT], acc=True)

        n_it = len(iters)
        for i in range(min(LA, n_it)):
            emit_st(i)
        for i in range(n_it):
            emit_rest(i)
            if i + LA < n_it:
                emit_st(i + LA)
        fw.barrier()


def phase_router(fw, nc, P, S, A, L):
    with ExitStack() as es:
        sb = lambda n, shp, dt: es.enter_context(nc.sbuf_tensor(U(n), shp, dt))
        alloc_norm(nc, es, S, None)
        load_gain(fw, nc, S, A.gains, 3)
        id32 = sb("id32", [128, 128], F32); t_id32 = Tok("id32")
        fw.dma("sp", id32[:], A.consts["ident32"][:, :], writes=[t_id32])
        wr = sb("wr", [128, 16, 8], F32); t_wr = Tok("wr")
        with nc.allow_non_contiguous_dma(reason="small router weight"):
            fw.dma("sp", wr[:], A.w_router.rearrange("(c p) e -> p c e", p=128), writes=[t_wr])
        hn32 = sb("hn32", [128, D], F32); t_hn32 = Tok("hn32")
        hT32 = sb("hT32", [128, 16, 128], F32); t_hT32 = Tok("hT32")
        lg = sb("lg", [128, 8], F32); t_lg = Tok("lg")
        l2 = sb("l2", [128, 8], F32); t_l2 = Tok("l2")
        sm = sb("sm", [128, 8], F32); t_sm = Tok("sm")
        gt = sb("gt", [128, 8], F32); t_gt = Tok("gt")
        for n in range(L // 128):
            r0 = n * 128
            k = S.xi % 2
            S.xi += 1
            xt, t_x = S.xt[k], S.t_xt[k]
            fw.dma("sp", xt[:], A.x3[r0:r0 + 128, :], reads=[A.t_x3], writes=[t_x])
            fw.op(fw.act, lambda: nc.scalar.activation(out=S.junk[:], in_=xt[:], func=AF.Square), reads=[t_x], writes=[S.t_junk])
            fw.op(fw.dve, lambda: nc.vector.tensor_reduce(out=S.ss[:, 0:1], in_=S.junk[:], axis=AX.X, op=ALU.add),
                  reads=[S.t_junk], writes=[S.t_ss])
            fw.op(fw.act, lambda: nc.scalar.activation(out=S.ss[:, 1:2], in_=S.ss[:, 0:1], func=AF.Sqrt, scale=1.0 / D,
                                                       bias=S.eps[:, 0:1]), reads=[S.t_ss, S.t_eps], writes=[S.t_ss2])
            fw.op(fw.dve, lambda: nc.vector.reciprocal(out=S.ss[:, 2:3], in_=S.ss[:, 1:2]), reads=[S.t_ss2], writes=[S.t_rstd])
            fw.op(fw.dve, lambda: nc.vector.scalar_tensor_tensor(out=hn32[:], in0=xt[:], scalar=S.ss[:, 2:3], in1=S.gain[:],
                                                                 op0=ALU.mult, op1=ALU.mult),
                  reads=[t_x, S.t_rstd, S.t_gain], writes=[t_hn32])
            fw.op(fw.act, lambda: nc.scalar.copy(out=S.hn[:], in_=hn32[:]), reads=[t_hn32], writes=[S.t_hn])
            fw.dma("sp", A.hn3[r0:r0 + 128, :], S.hn[:], reads=[S.t_hn], writes=[A.t_hn3], acc=True)
            for g in range(4):
                ps, t_ps = get_ps(P)
                fw.mm_group([lambda j=j: nc.tensor.transpose(out=ps[:, j * 128:(j + 1) * 128],
                                                             in_=hn32[:, (4 * g + j) * 128:(4 * g + j + 1) * 128],
                                                             identity=id32[:]) for j in range(4)],
                            reads=[t_hn32, t_id32], writes=[t_ps])
                fw.op(fw.act if g % 2 else fw.dve,
                      (lambda: nc.scalar.copy(out=hT32[:, 4 * g:4 * g + 4, :], in_=ps[:, :].rearrange("p (j t) -> p j t", t=128))) if g % 2 else
                      (lambda: nc.vector.tensor_copy(out=hT32[:, 4 * g:4 * g + 4, :], in_=ps[:, :].rearrange("p (j t) -> p j t", t=128))),
                      reads=[t_ps], writes=[t_hT32], acc=True)
            ps, t_ps = get_ps(P)
            fw.mm_group([lambda c=c: nc.tensor.matmul(ps[:, 0:8], lhsT=hT32[:, c, :], rhs=wr[:, c, :], start=(c == 0), stop=(c == 15))
                         for c in range(16)], reads=[t_hT32, t_wr], writes=[t_ps])
            fw.op(fw.dve, lambda: nc.vector.tensor_copy(out=lg[:], in_=ps[:, 0:8]), reads=[t_ps], writes=[t_lg])
            fw.op(fw.dve, lambda: nc.vector.tensor_reduce(out=sm[:, 0:1], in_=lg[:], axis=AX.X, op=ALU.max), reads=[t_lg], writes=[t_sm])
            fw.op(fw.dve, lambda: nc.vector.tensor_scalar(out=l2[:], in0=lg[:], scalar1=sm[:, 0:1], scalar2=-1e30,
                                                          op0=ALU.is_equal, op1=ALU.mult), reads=[t_lg, t_sm], writes=[t_l2])
            fw.op(fw.dve, lambda: nc.vector.tensor_tensor(out=l2[:], in0=l2[:], in1=lg[:], op=ALU.add), reads=[t_l2, t_lg], writes=[t_l2])
            fw.op(fw.dve, lambda: nc.vector.tensor_reduce(out=sm[:, 1:2], in_=l2[:], axis=AX.X, op=ALU.max), reads=[t_l2], writes=[t_sm])
            fw.op(fw.dve, lambda: nc.vector.tensor_scalar(out=l2[:], in0=lg[:], scalar1=sm[:, 1:2], scalar2=None, op0=ALU.is_ge),
                  reads=[t_lg, t_sm], writes=[t_l2])
            fw.op(fw.dve, lambda: nc.vector.tensor_scalar(out=sm[:, 2:3], in0=sm[:, 0:1], scalar1=-1.0, scalar2=None, op0=ALU.mult),
                  reads=[t_sm], writes=[t_sm])
            fw.op(fw.act, lambda: nc.scalar.activation(out=gt[:], in_=lg[:], func=AF.Exp, bias=sm[:, 2:3], scale=1.0),
                  reads=[t_lg, t_sm], writes=[t_gt])
            fw.op(fw.dve, lambda: nc.vector.tensor_tensor(out=gt[:], in0=gt[:], in1=l2[:], op=ALU.mult), reads=[t_gt, t_l2], writes=[t_gt])
            fw.op(fw.dve, lambda: nc.vector.tensor_reduce(out=sm[:, 3:4], in_=gt[:], axis=AX.X, op=ALU.add), reads=[t_gt], writes=[t_sm])
            fw.op(fw.dve, lambda: nc.vector.reciprocal(out=sm[:, 4:5], in_=sm[:, 3:4]), reads=[t_sm], writes=[t_sm])
            fw.op(fw.dve, lambda: nc.vector.tensor_scalar(out=gt[:], in0=gt[:], scalar1=sm[:, 4:5], scalar2=None, op0=ALU.mult),
                  reads=[t_gt, t_sm], writes=[t_gt])
            fw.dma("sp", A.gates[r0:r0 + 128, :], gt[:], reads=[t_gt], writes=[A.t_gates], acc=True)
        fw.barrier()


def build_A(L, stages=("hgrn", "ffn", "attn", "router"), dbg=False, stop_after=8):
    nc = bass.Bass("TRN2", target_bir_lowering=False)
    A = Ctx(); S = Ctx()
    din = lambda n, shp, dt=F32: nc.dram_tensor(n, shp, dt, kind="ExternalInput").ap()
    dint = lambda n, shp, dt: nc.dram_tensor(n, shp, dt).ap()
    dout = lambda n, shp, dt: nc.dram_tensor(n, shp, dt, kind="ExternalOutput").ap()
    A.x = din("x", [L, D]); A.gains = din("gains", [4, D])
    w_in = din("w_in", [D, 10240]); A.lb_logits = din("lb_logits", [3, 2, 2048]); A.onorm = din("onorm", [2048])
    w_out0 = din("w_out0", [D, D])
    wg = din("wg", [D, FF]); wu = din("wu", [D, FF]); wd = din("wd", [FF, D])
    wqkv = din("wqkv", [D, 3 * D]); A.q_gain = din("q_gain", [128]); A.k_gain = din("k_gain", [128]); w_out1 = din("w_out1", [D, D])
    A.w_router = din("w_router", [D, 8])
    cn = make_consts(); cn.update(attn_consts(L))
    A.consts = {k: din("c_" + k, list(v.shape), BF16 if v.dtype == ml_dtypes.bfloat16 else F32) for k, v in cn.items()}
    A.w_in_bf = dint("w_in_bf", [D, 10240], BF16); A.t_w_in = Tok("w_in")
    w_out0_bf = dint("w_out0_bf", [D, D], BF16); t_w_out0 = Tok("w_out0")
    wg_bf = dint("wg_bf", [D, FF], BF16); wu_bf = dint("wu_bf", [D, FF], BF16); wd_bf = dint("wd_bf", [FF, D], BF16); t_wffn = Tok("wffn")
    A.wqkv_bf = dint("wqkv_bf", [D, 3 * D], BF16); A.t_wqkv = Tok("wqkv")
    w_out1_bf = dint("w_out1_bf", [D, D], BF16); t_w_out1 = Tok("w_out1")
    A.projT = dint("projT", [8192, L], F32); A.t_projT = Tok("projT")
    A.v_tm = dint("v_tm", [L, 2048], BF16); A.t_v_tm = Tok("v_tm")
    A.oT = dint("oT", [2048, L], BF16); A.t_oT = Tok("oT")
    mk = (lambda n, shp, dt: dout(n, shp, dt)) if dbg else (lambda n, shp, dt: dint(n, shp, dt))
    x1 = mk("x1", [L, D], F32); t_x1 = Tok("x1")
    A.x2 = mk("x2", [L, D], F32); A.t_x2 = Tok("x2")
    A.qT = dint("qT", [2048, L], BF16); A.t_qT = Tok("qT")
    A.kT = dint("kT", [2048, L], BF16); A.t_kT = Tok("kT")
    A.v_att = dint("v_att", [L, 2048], BF16); A.t_v_att = Tok("v_att")
    A.x3 = dout("x3", [L, D], F32); A.t_x3 = Tok("x3")
    A.hn3 = dout("hn3", [L, D], BF16); A.t_hn3 = Tok("hn3")
    A.gates = dout("gates", [L, 8], F32); A.t_gates = Tok("gates")
    t_x = Tok("x")
    with ExitStack() as es:
        fw = FW(nc, es)
        P = psum_pools(nc, es, fw)
        alloc_common(nc, es, fw, S, A.consts)
        A.t_w_in = cast_weight(fw, nc, A.w_in_bf, w_in, D, 10240, "w_in")
        t_w_out0 = cast_weight(fw, nc, w_out0_bf, w_out0, D, D, "w_out0")
        t_wg, t_wu = cast_weights_interleaved(fw, nc, [(wg_bf, wg, D, FF, "wg"), (wu_bf, wu, D, FF, "wu")])
        t_wd = cast_weight(fw, nc, wd_bf, wd, FF, D, "wd")
        A.t_wqkv = cast_weight(fw, nc, A.wqkv_bf, wqkv, D, 3 * D, "wqkv")
        t_w_out1 = cast_weight(fw, nc, w_out1_bf, w_out1, D, D, "w_out1")
        phases = [
            lambda: phase_hgrn_proj(fw, nc, P, S, A, L),
            lambda: phase_hgrn_scan(fw, nc, P, S, A, L),
            lambda: phase_wout(fw, nc, P, S, A, L, A.oT, A.t_oT, w_out0_bf, t_w_out0, A.x, t_x, x1, t_x1),
            lambda: phase_ffn(fw, nc, P, S, L, FF, wg_bf, wu_bf, wd_bf, t_wg, t_wu, t_wd, x1, t_x1, A.x2, A.t_x2, gains=A.gains, gain_idx=1),
            lambda: phase_attn_proj(fw, nc, P, S, A, L),
            lambda: phase_attn_core(fw, nc, P, S, A, L),
            lambda: phase_wout(fw, nc, P, S, A, L, A.oT, A.t_oT, w_out1_bf, t_w_out1, A.x2, A.t_x2, A.x3, A.t_x3),
            lambda: phase_router(fw, nc, P, S, A, L),
        ]
        fw.barrier()
        for ph in phases[:stop_after]:
            ph()
        fw.final_wait([A.t_x3, A.t_hn3, A.t_gates] + ([t_x1, A.t_x2] if dbg else []))
        print("ninst", fw.ninst, "counts", [(e.name, e.count) for e in fw.engs], flush=True)
    return nc, cn


def inputs_A(d, b, L, cn):
    inm = {"x": np.ascontiguousarray(d['x'][b, :L]), "gains": d['norm_gains'].reshape(4, D), "w_in": d['hgrn_w_in'][0],
           "lb_logits": d['hgrn_lb_logits'], "onorm": d['hgrn_onorm'][0], "w_out0": d['hgrn_w_out'][0],
           "wg": d['ffn_w_gate'][0], "wu": d['ffn_w_up'][0], "wd": d['ffn_w_down'][0],
           "wqkv": d['attn_w_qkv'][0], "q_gain": d['attn_q_gain'][0], "k_gain": d['attn_k_gain'][0], "w_out1": d['attn_w_out'][0],
           "w_router": d['moe_w_router'][0]}
    for k, v in cn.items():
        inm["c_" + k] = v
    return inm


FE = 7168


def build_B(cap):
    nc = bass.Bass("TRN2", target_bir_lowering=False)
    S = Ctx()
    din = lambda n, shp, dt=F32: nc.dram_tensor(n, shp, dt, kind="ExternalInput").ap()
    dint = lambda n, shp, dt: nc.dram_tensor(n, shp, dt).ap()
    hn_g = din("hn_g", [cap, D], BF16); rs = din("rs", [cap]); t_in = Tok("in")
    wg = din("wg", [D, FE]); wu = din("wu", [D, FE]); wd = din("wd", [FE, D])
    cn = make_consts()
    consts = {k: din("c_" + k, list(v.shape), BF16) for k, v in cn.items()}
    wg_bf = dint("wg_bf", [D, FE], BF16); wu_bf = dint("wu_bf", [D, FE], BF16); wd_bf = dint("wd_bf", [FE, D], BF16)
    t_w = Tok("w")
    y = nc.dram_tensor("y", [cap, D], F32, kind="ExternalOutput").ap(); t_y = Tok("y")
    with ExitStack() as es:
        fw = FW(nc, es)
        P = psum_pools(nc, es, fw)
        alloc_common(nc, es, fw, S, consts)
        t_wg, t_wu = cast_weights_interleaved(fw, nc, [(wg_bf, wg, D, FE, "wg"), (wu_bf, wu, D, FE, "wu")])
        t_wd = cast_weight(fw, nc, wd_bf, wd, FE, D, "wd")
        phase_ffn(fw, nc, P, S, cap, FE, wg_bf, wu_bf, wd_bf, t_wg, t_wu, t_wd, None, None, y, t_y,
                  hn_in=hn_g, t_hn_in=t_in, rowscale=rs, t_rowscale=t_in)
        fw.final_wait([t_y])
        print("B ninst", fw.ninst, "counts", [(e.name, e.count) for e in fw.engs], flush=True)
    return nc, cn


def build_C(T):
    nc = bass.Bass("TRN2", target_bir_lowering=False)
    din = lambda n, shp, dt=F32: nc.dram_tensor(n, shp, dt, kind="ExternalInput").ap()
    x3 = din("x3", [T, D]); y0 = din("y0", [T, D]); y1 = din("y1", [T, D])
    out = nc.dram_tensor("out", [T, D], F32, kind="ExternalOutput").ap(); t_out = Tok("out")
    with ExitStack() as es:
        fw = FW(nc, es)
        sb = lambda n, shp, dt: es.enter_context(nc.sbuf_tensor(U(n), shp, dt))
        a = [sb(f"ca{i}", [128, D], F32) for i in range(2)]; ta = [Tok("a0"), Tok("a1")]
        b = [sb(f"cb{i}", [128, D], F32) for i in range(2)]; tb_ = [Tok("b0"), Tok("b1")]
        c = [sb(f"cc{i}", [128, D], F32) for i in range(2)]; tc = [Tok("c0"), Tok("c1")]
        for n in range(T // 128):
            k = n % 2
            r0 = n * 128
            fw.dma("sp", a[k][:], x3[r0:r0 + 128, :], writes=[ta[k]])
            fw.dma("sp", b[k][:], y0[r0:r0 + 128, :], writes=[tb_[k]])
            fw.dma("sp", c[k][:], y1[r0:r0 + 128, :], writes=[tc[k]])
            fw.op(fw.dve, lambda: nc.vector.tensor_tensor(out=b[k][:], in0=b[k][:], in1=c[k][:], op=ALU.add),
                  reads=[tb_[k], tc[k]], writes=[tb_[k]])
            fw.op(fw.dve, lambda: nc.vector.tensor_tensor(out=a[k][:], in0=a[k][:], in1=b[k][:], op=ALU.add),
                  reads=[ta[k], tb_[k]], writes=[ta[k]])
            fw.dma("sp", out[r0:r0 + 128, :], a[k][:], reads=[ta[k]], writes=[t_out], acc=True)
        fw.final_wait([t_out])
    return nc


def kernel(**inputs):
    from concourse.bass_utils import run_bass_kernel_spmd
    d = {k: np.asarray(v) for k, v in inputs.items()}
    B, L = d['x'].shape[0], d['x'].shape[1]
    T = B * L
    ncA, cnA = build_A(L)
    in_maps = [inputs_A(d, b, L, cnA) for b in range(B)]
    resA = run_bass_kernel_spmd(ncA, in_maps, core_ids=list(range(B))).results
    x3 = np.concatenate([np.asarray(r["x3"]) for r in resA], axis=0)
    hn3 = np.concatenate([np.asarray(r["hn3"]) for r in resA], axis=0)
    gates = np.concatenate([np.asarray(r["gates"]) for r in resA], axis=0)
    sel = gates > 0
    idx = [np.nonzero(sel[:, e])[0] for e in range(8)]
    cap = max(512, int(-(-max(len(i) for i in idx) // 512) * 512))
    ncB, cnB = build_B(cap)
    in_maps = []
    for e in range(8):
        hg = np.zeros((cap, D), dtype=hn3.dtype)
        hg[:len(idx[e])] = hn3[idx[e]]
        rs = np.zeros((cap,), np.float32)
        rs[:len(idx[e])] = gates[idx[e], e]
        m = {"hn_g": hg, "rs": rs, "wg": d['moe_w_gate'][0, e], "wu": d['moe_w_up'][0, e], "wd": d['moe_w_down'][0, e]}
        for k, v in cnB.items():
            m["c_" + k] = v
        in_maps.append(m)
    resB = run_bass_kernel_spmd(ncB, in_maps, core_ids=list(range(8))).results
    y0 = np.zeros((T, D), np.float32)
    y1 = np.zeros((T, D), np.float32)
    rank = np.cumsum(sel, axis=1) - 1
    for e in range(8):
        ye = np.asarray(resB[e]["y"])[:len(idx[e])]
        sl = rank[idx[e], e]
        y0[idx[e][sl == 0]] = ye[sl == 0]
        y1[idx[e][sl >= 1]] = ye[sl >= 1]
    TC = T // 8
    ncC = build_C(TC)
    in_maps = [{"x3": x3[c * TC:(c + 1) * TC], "y0": y0[c * TC:(c + 1) * TC], "y1": y1[c * TC:(c + 1) * TC]} for c in range(8)]
    resC = run_bass_kernel_spmd(ncC, in_maps, core_ids=list(range(8))).results
    out = np.concatenate([np.asarray(r["out"]) for r in resC], axis=0).reshape(B, L, D).astype(np.float32)
    return out
```

```python
import numpy as np
from contextlib import ExitStack
import concourse.bass as bass
import concourse.mybir as mybir

F32 = mybir.dt.float32
BF16 = mybir.dt.bfloat16
I32 = mybir.dt.int32
U32 = mybir.dt.uint32
AF = mybir.ActivationFunctionType
ALU = mybir.AluOpType
AX = mybir.AxisListType


class Tok:
    __slots__ = ("w", "r", "name")

    def __init__(self, name=""):
        self.w = {}
        self.r = {}
        self.name = name


class Eng:
    def __init__(self, raw, sem, name):
        self.raw = raw
        self.sem = sem
        self.name = name
        self.count = 0
        self.seen = {}


class FW:
    def __init__(self, nc, es, n_dma_sems=24, n_gdma_sems=12):
        self.nc = nc
        self.es = es
        mk = lambda n: es.enter_context(nc.semaphore(n))
        self.pe = Eng(nc.tensor, mk("s_pe"), "pe")
        self.act = Eng(nc.scalar, mk("s_act"), "act")
        self.dve = Eng(nc.vector, mk("s_dve"), "dve")
        self.pool = Eng(nc.gpsimd, mk("s_pool"), "pool")
        self.sp = Eng(nc.sync, mk("s_sp"), "sp")
        self.engs = [self.pe, self.act, self.dve, self.pool, self.sp]
        self.dsems = {"sp": [[mk(f"d_sp{i}"), 0] for i in range(n_dma_sems)],
                      "pool": [[mk(f"d_pl{i}"), 0] for i in range(n_gdma_sems)],
                      "act": [[mk(f"d_ac{i}"), 0] for i in range(24)]}
        self.drr = {"sp": 0, "pool": 0, "act": 0}
        self.ninst = 0

    def _wait(self, eng, sem, val):
        key = id(sem)
        if eng.seen.get(key, 0) >= val:
            return
        eng.raw.wait_ge(sem, val)
        eng.seen[key] = val
        self.ninst += 1

    def _deps(self, eng, reads, writes, skip_self=False):
        for t in reads:
            for k, (s, v) in t.w.items():
                if skip_self and s is eng.sem:
                    continue
                self._wait(eng, s, v)
        for t in writes:
            for k, (s, v) in t.w.items():
                if skip_self and s is eng.sem:
                    continue
                self._wait(eng, s, v)
            for k, (s, v) in t.r.items():
                if skip_self and s is eng.sem:
                    continue
                self._wait(eng, s, v)

    def _record(self, ev, reads, writes, accumulate=False):
        s, v = ev
        for t in writes:
            if not accumulate:
                t.w = {}
            t.w[id(s)] = (s, v)
            t.r = {}
        for t in reads:
            t.r[id(s)] = (s, v)

    def op(self, eng, fn, reads=(), writes=(), skip_self=False, acc=False):
        self._deps(eng, reads, writes, skip_self=skip_self)
        ins = fn()
        eng.count += 1
        ins.then_inc(eng.sem, 1)
        self.ninst += 1
        self._record((eng.sem, eng.count), reads, writes, accumulate=acc)
        return ins

    def mm_group(self, fns, reads=(), writes=()):
        eng = self.pe
        self._deps(eng, reads, writes, skip_self=True)
        ins = None
        for f in fns:
            ins = f()
            self.ninst += 1
        eng.count += 1
        ins.then_inc(eng.sem, 1)
        self._record((eng.sem, eng.count), reads, writes)

    def dma(self, q, out, in_, reads=(), writes=(), acc=False, **kw):
        eng = {"sp": self.sp, "pool": self.pool, "act": self.act}[q]
        pool = self.dsems[q]
        j = self.drr[q]
        self.drr[q] = (j + 1) % len(pool)
        sem, tot = pool[j]
        if tot:
            self._wait(eng, sem, tot)
        self._deps(eng, reads, writes)
        ins = eng.raw.dma_start(out=out, in_=in_, **kw)
        tot += 16
        pool[j][1] = tot
        ins.then_inc(sem, 16)
        self.ninst += 1
        self._record((sem, tot), reads, writes, accumulate=acc)
        return ins

    def all_events(self):
        evs = [(e.sem, e.count) for e in self.engs if e.count]
        for q in self.dsems:
            for sem, tot in self.dsems[q]:
                if tot:
                    evs.append((sem, tot))
        return evs

    def barrier(self, engines=None):
        evs = self.all_events()
        for e in (engines or self.engs):
            for s, v in evs:
                if s is e.sem:
                    continue
                self._wait(e, s, v)

    def final_wait(self, toks):
        for t in toks:
            for k, (s, v) in t.w.items():
                self._wait(self.sp, s, v)


import numpy as np
import ml_dtypes

D = 2048
EPS = 1e-6
FF = 5504
NFC = FF // 128


class Ctx:
    pass


_UC = [0]


def U(n):
    _UC[0] += 1
    return f"{n}_{_UC[0]}"


def make_consts():
    c = {}
    c["ident"] = np.eye(128, dtype=np.float32).astype(ml_dtypes.bfloat16)
    s = np.arange(128)[:, None]
    t = np.arange(128)[None, :]
    same = (s // 64) == (t // 64)
    c["m2f"] = (same & (s <= t)).astype(np.float32).astype(ml_dtypes.bfloat16)
    c["m2b"] = (same & (s >= t)).astype(np.float32).astype(ml_dtypes.bfloat16)
    return c


def psum_pools(nc, es, fw):
    P = Ctx()
    P.f32 = [es.enter_context(nc.psum_tensor(f"psf{i}", [128, 512], F32)) for i in range(6)]
    P.f32t = [Tok(f"psf{i}") for i in range(6)]
    P.bf = [es.enter_context(nc.psum_tensor(f"psb{i}", [128, 1024], BF16)) for i in range(2)]
    P.bft = [Tok(f"psb{i}") for i in range(2)]
    P.i = 0
    P.j = 0
    return P


def get_ps(P):
    k = P.i % len(P.f32)
    P.i += 1
    return P.f32[k], P.f32t[k]


def get_psb(P):
    k = P.j % len(P.bf)
    P.j += 1
    return P.bf[k], P.bft[k]


def cast_weight(fw, nc, dst, src, rows, cols, name="w", chunks=None):
    toks = []
    for c in range(0, cols, 2048):
        if chunks is not None and (c // 2048) not in chunks:
            toks.append(None)
            continue
        w = min(2048, cols - c)
        tok = Tok(f"{name}{c}")
        for r in range(0, rows, 128):
            fw.dma("pool", dst[r:r + 128, c:c + w], src[r:r + 128, c:c + w], writes=[tok], acc=True)
        toks.append(tok)
    return toks


def cast_weights_interleaved(fw, nc, items):
    nch = max(-(-it[3] // 2048) for it in items)
    toks = [[None] * (-(-it[3] // 2048)) for it in items]
    for ch in range(nch):
        for k, (dst, src, rows, cols, name) in enumerate(items):
            if ch * 2048 < cols:
                t = cast_weight(fw, nc, dst, src, rows, cols, name, chunks=[ch])
                toks[k][ch] = t[ch]
    return toks


def norm_T(fw, nc, P, S, x_src, row0, tt, gain_bc, t_gain):
    k = S.xi % 2
    S.xi += 1
    xt, t_x = S.xt[k], S.t_xt[k]
    fw.dma("sp", xt[:], x_src[row0:row0 + 128, :], writes=[t_x])
    fw.op(fw.act, lambda: nc.scalar.activation(out=S.junk[:], in_=xt[:], func=AF.Square),
          reads=[t_x], writes=[S.t_junk])
    fw.op(fw.dve, lambda: nc.vector.tensor_reduce(out=S.ss[:, 0:1], in_=S.junk[:], axis=AX.X, op=ALU.add),
          reads=[S.t_junk], writes=[S.t_ss])
    fw.op(fw.act, lambda: nc.scalar.activation(out=S.ss[:, 1:2], in_=S.ss[:, 0:1], func=AF.Sqrt,
                                               scale=1.0 / D, bias=S.eps[:, 0:1]),
          reads=[S.t_ss, S.t_eps], writes=[S.t_ss2])
    fw.op(fw.dve, lambda: nc.vector.reciprocal(out=S.ss[:, 2:3], in_=S.ss[:, 1:2]),
          reads=[S.t_ss2], writes=[S.t_rstd])
    fw.op(fw.dve, lambda: nc.vector.scalar_tensor_tensor(out=S.hn[:], in0=xt[:], scalar=S.ss[:, 2:3],
                                                         in1=gain_bc[:], op0=ALU.mult, op1=ALU.mult),
          reads=[t_x, S.t_rstd, t_gain], writes=[S.t_hn])
    for g in range(4):
        pb, t_pb = get_psb(P)
        fns = []
        for j in range(4):
            c = 4 * g + j
            fns.append(lambda j=j, c=c: nc.tensor.transpose(out=pb[:, j * 128:(j + 1) * 128],
                                                            in_=S.hn[:, c * 128:(c + 1) * 128],
                                                            identity=S.ident[:]))
        fw.mm_group(fns, reads=[S.t_hn, S.t_ident], writes=[t_pb])
        dst = S.hnT[:, 4 * g:4 * g + 4, tt * 128:(tt + 1) * 128]
        src = pb[:, 0:512].rearrange("p (j t) -> p j t", t=128)
        if g % 2 == 0:
            fw.op(fw.act, lambda: nc.scalar.copy(out=dst, in_=src), reads=[t_pb], writes=[S.t_hnT], acc=True)
        else:
            fw.op(fw.dve, lambda: nc.vector.tensor_copy(out=dst, in_=src), reads=[t_pb], writes=[S.t_hnT], acc=True)


def alloc_norm(nc, es, S, consts_ap):
    S.xt = [es.enter_context(nc.sbuf_tensor(U(f"xt{i}"), [128, D], F32)) for i in range(2)]
    S.t_xt = [Tok("xt0"), Tok("xt1")]
    S.xi = 0
    S.junk = es.enter_context(nc.sbuf_tensor(U("junk"), [128, D], F32)); S.t_junk = Tok("junk")
    S.ss = es.enter_context(nc.sbuf_tensor(U("ss"), [128, 4], F32))
    S.t_ss = Tok("ss"); S.t_ss2 = Tok("ss2"); S.t_rstd = Tok("rstd")
    S.hn = es.enter_context(nc.sbuf_tensor(U("hn"), [128, D], BF16)); S.t_hn = Tok("hn")
    S.hnT = es.enter_context(nc.sbuf_tensor(U("hnT"), [128, 16, 512], BF16)); S.t_hnT = Tok("hnT")


def alloc_common(nc, es, fw, S, consts):
    S.eps = es.enter_context(nc.sbuf_tensor(U("eps"), [128, 1], F32)); S.t_eps = Tok("eps")
    fw.op(fw.dve, lambda: nc.vector.memset(S.eps[:], EPS), writes=[S.t_eps])
    S.ident = es.enter_context(nc.sbuf_tensor(U("ident"), [128, 128], BF16)); S.t_ident = Tok("ident")
    fw.dma("sp", S.ident[:], consts["ident"][:, :], writes=[S.t_ident])
    S.gain = es.enter_context(nc.sbuf_tensor(U("gainbc"), [128, D], F32)); S.t_gain = Tok("gain")


def load_gain(fw, nc, S, gains, idx):
    fw.dma("sp", S.gain[:], gains[idx].partition_broadcast(128), writes=[S.t_gain])


def phase_hgrn_proj(fw, nc, P, S, A, L):
    with ExitStack() as es:
        alloc_norm(nc, es, S, None)
        wsl = [es.enter_context(nc.sbuf_tensor(U(f"wsl{i}"), [128, 16, 512], BF16)) for i in range(3)]
        t_wsl = [Tok("wsl0"), Tok("wsl1"), Tok("wsl2")]
        ob = [es.enter_context(nc.sbuf_tensor(U(f"ob{i}"), [128, 512], F32)) for i in range(3)]
        t_ob = [Tok(f"ob{i}") for i in range(3)]
        ob16 = [es.enter_context(nc.sbuf_tensor(U(f"obh{i}"), [128, 512], BF16)) for i in range(2)]
        t_ob16 = [Tok(f"obh{i}") for i in range(2)]
        load_gain(fw, nc, S, A.gains, 0)
        win = A.w_in_bf.rearrange("(c p) n -> p c n", p=128)
        oi = 0
        wi = 0
        for tb in range(L // 512):
            for tt in range(4):
                norm_T(fw, nc, P, S, A.x, tb * 512 + tt * 128, tt, S.gain, S.t_gain)
            for s in range(20):
                w, t_w = wsl[wi % 3], t_wsl[wi % 3]
                wi += 1
                fw.dma("sp", w[:], win[:, :, s * 512:(s + 1) * 512], reads=[A.t_w_in[s // 4]], writes=[t_w])
                tokmajor = 12 <= s < 16
                for j in range(4):
                    ps, t_ps = get_ps(P)
                    if not tokmajor:
                        fns = [lambda c=c: nc.tensor.matmul(ps[:, :], lhsT=w[:, c, j * 128:(j + 1) * 128],
                                                            rhs=S.hnT[:, c, :], start=(c == 0), stop=(c == 15))
                               for c in range(16)]
                        fw.mm_group(fns, reads=[t_w, S.t_hnT], writes=[t_ps])
                        o, t_o = ob[oi % 3], t_ob[oi % 3]
                        oi += 1
                        if oi % 2:
                            fw.op(fw.act, lambda: nc.scalar.copy(out=o[:], in_=ps[:, :]), reads=[t_ps], writes=[t_o])
                        else:
                            fw.op(fw.dve, lambda: nc.vector.tensor_copy(out=o[:], in_=ps[:, :]), reads=[t_ps], writes=[t_o])
                        srow = s if s < 12 else s - 4
                        r0 = srow * 512 + j * 128
                        fw.dma("act", A.projT[r0:r0 + 128, tb * 512:(tb + 1) * 512], o[:], reads=[t_o],
                               writes=[A.t_projT], acc=True)
                    else:
                        tt = j
                        fns = [lambda c=c: nc.tensor.matmul(ps[:, :], lhsT=S.hnT[:, c, tt * 128:(tt + 1) * 128],
                                                            rhs=w[:, c, :], start=(c == 0), stop=(c == 15))
                               for c in range(16)]
                        fw.mm_group(fns, reads=[t_w, S.t_hnT], writes=[t_ps])
                        o, t_o = ob16[oi % 2], t_ob16[oi % 2]
                        oi += 1
                        if oi % 2:
                            fw.op(fw.act, lambda: nc.scalar.copy(out=o[:], in_=ps[:, :]), reads=[t_ps], writes=[t_o])
                        else:
                            fw.op(fw.dve, lambda: nc.vector.tensor_copy(out=o[:], in_=ps[:, :]), reads=[t_ps], writes=[t_o])
                        r0 = tb * 512 + tt * 128
                        fw.dma("act", A.v_tm[r0:r0 + 128, (s - 12) * 512:(s - 11) * 512], o[:], reads=[t_o],
                               writes=[A.t_v_tm], acc=True)
        fw.barrier()


def phase_hgrn_scan(fw, nc, P, S, A, L):
    NT = L // 128
    NCH = L // 64
    with ExitStack() as es:
        sb = lambda n, shp, dt: es.enter_context(nc.sbuf_tensor(U(n), shp, dt))
        lbl = sb("lbl", [128, 3, 2, 16], F32); t_lbl = Tok("lbl")
        with nc.allow_non_contiguous_dma(reason="tiny param load"):
            for l in range(3):
                for d in range(2):
                    fw.dma("sp", lbl[:, l, d, :], A.lb_logits[l, d].rearrange("(h k) -> k h", k=128),
                           writes=[t_lbl], acc=True)
        ong = sb("ong", [128, 16], F32); t_ong = Tok("ong")
        with nc.allow_non_contiguous_dma(reason="tiny param load"):
            fw.dma("sp", ong[:], A.onorm.rearrange("(h k) -> k h", k=128), writes=[t_ong])
        lbe = sb("lbe", [128, 3, 32], F32); t_lbe = Tok("lbe")
        fw.op(fw.act, lambda: nc.scalar.activation(out=lbe[:], in_=lbl[:].rearrange("p l d h -> p l (d h)"), func=AF.Exp),
              reads=[t_lbl], writes=[t_lbe])
        lbs = sb("lbs", [128, 4, 32], F32); t_lbs = Tok("lbs")
        fw.op(fw.dve, lambda: nc.vector.tensor_tensor(out=lbs[:, 0, :], in0=lbe[:, 0, :], in1=lbe[:, 1, :], op=ALU.add),
              reads=[t_lbe], writes=[t_lbs])
        fw.op(fw.dve, lambda: nc.vector.tensor_tensor(out=lbs[:, 1, :], in0=lbs[:, 0, :], in1=lbe[:, 2, :], op=ALU.add),
              reads=[t_lbe, t_lbs], writes=[t_lbs])
        fw.op(fw.dve, lambda: nc.vector.reciprocal(out=lbs[:, 0, :], in_=lbs[:, 1, :]), reads=[t_lbs], writes=[t_lbs])
        fw.op(fw.dve, lambda: nc.vector.tensor_tensor(out=lbs[:, 2, :], in0=lbe[:, 0, :], in1=lbs[:, 0, :], op=ALU.mult),
              reads=[t_lbe, t_lbs], writes=[t_lbs])
        fw.op(fw.dve, lambda: nc.vector.tensor_scalar(out=lbs[:, 3, :], in0=lbs[:, 2, :], scalar1=-1.0, scalar2=1.0,
                                                      op0=ALU.mult, op1=ALU.add), reads=[t_lbs], writes=[t_lbs])
        rmask = sb("rmask", [128, L], BF16); t_rmask = Tok("rmask")
        fw.op(fw.dve, lambda: nc.vector.memset(rmask[:], 1.0), writes=[t_rmask])
        fw.op(fw.dve, lambda: nc.vector.memset(rmask[:, 0::64], 0.0), writes=[t_rmask])
        m2 = [sb("m2f", [128, 128], BF16), sb("m2b", [128, 128], BF16)]
        t_m2 = Tok("m2")
        fw.dma("sp", m2[0][:], A.consts["m2f"][:, :], writes=[t_m2], acc=True)
        fw.dma("sp", m2[1][:], A.consts["m2b"][:, :], writes=[t_m2], acc=True)
        ones = sb("ones", [128, 128], F32); t_ones = Tok("ones")
        fw.op(fw.dve, lambda: nc.vector.memset(ones[:], 1.0), writes=[t_ones])
        qs = sb("qs", [128, L], F32); t_qs = Tok("qs")
        w1 = sb("w1", [128, L], F32); t_w1 = Tok("w1")
        w2 = sb("w2", [128, L], F32); t_w2 = Tok("w2")
        w3 = sb("w3", [128, L], F32); t_w3 = Tok("w3")
        qt = [sb("qt0", [128, L], BF16), sb("qt1", [128, L], BF16)]; t_qt = [Tok("qt0"), Tok("qt1")]
        kt = [sb("kt0", [128, L], BF16), sb("kt1", [128, L], BF16)]; t_kt = [Tok("kt0"), Tok("kt1")]
        etot = [sb("etot0", [128, NCH], F32), sb("etot1", [128, NCH], F32)]; t_etot = [Tok("et0"), Tok("et1")]
        vtm = sb("vtm", [128, NT, 128], BF16); t_vtm = Tok("vtm")
        ktm = [sb("ktm0", [128, NT, 128], BF16), sb("ktm1", [128, NT, 128], BF16)]; t_ktm = [Tok("ktm0"), Tok("ktm1")]
        osum = sb("osum", [128, L], F32); t_osum = Tok("osum")
        Wf = [[sb(f"Wf{d}{k}", [128, 128], F32) for k in range(2)] for d in range(2)]
        t_Wf = [[Tok(f"Wf{d}{k}") for k in range(2)] for d in range(2)]
        wcur = [0, 0]
        Sb = [sb("Sb0", [128, 128], BF16), sb("Sb1", [128, 128], BF16)]; t_Sb = [Tok("Sb0"), Tok("Sb1")]
        tmp = [sb("tmp0", [128, 128], F32), sb("tmp1", [128, 128], F32)]; t_tmp = [Tok("tmp0"), Tok("tmp1")]
        atm = [sb("atm0", [128, 128], BF16), sb("atm1", [128, 128], BF16)]; t_atm = [Tok("atm0"), Tok("atm1")]
        oTb = sb("oTb", [128, L], BF16); t_oTb = Tok("oTb")

        for h in range(16):
            r = h * 128
            fw.dma("sp", qs[:], A.projT[r:r + 128, :], reads=[A.t_projT], writes=[t_qs])
            fw.dma("sp", vtm[:], A.v_tm[:, r:r + 128].rearrange("(n p) v -> p n v", p=128), reads=[A.t_v_tm],
                   writes=[t_vtm])
            fw.op(fw.act, lambda: nc.scalar.activation(out=qs[:], in_=qs[:], func=AF.Silu), reads=[t_qs], writes=[t_qs])
            for d in range(2):
                lbc = lbs[:, 2, d * 16 + h:d * 16 + h + 1]
                omc = lbs[:, 3, d * 16 + h:d * 16 + h + 1]
                fw.dma("sp", w1[:], A.projT[2048 * (d + 1) + r:2048 * (d + 1) + r + 128, :], reads=[A.t_projT], writes=[t_w1])
                fw.op(fw.act, lambda: nc.scalar.activation(out=w1[:], in_=w1[:], func=AF.Sigmoid),
                      reads=[t_w1], writes=[t_w1])
                fw.op(fw.dve, lambda: nc.vector.tensor_scalar(out=w1[:], in0=w1[:], scalar1=omc, scalar2=lbc,
                                                              op0=ALU.mult, op1=ALU.add), reads=[t_w1, t_lbs], writes=[t_w1])
                fw.op(fw.act, lambda: nc.scalar.activation(out=w2[:], in_=w1[:], func=AF.Ln), reads=[t_w1], writes=[t_w2])
                fw.op(fw.dve, lambda: nc.vector.tensor_scalar(out=w1[:], in0=w1[:], scalar1=-1.0, scalar2=1.0,
                                                              op0=ALU.mult, op1=ALU.add), reads=[t_w1], writes=[t_w1])
                fw.op(fw.dve, lambda: nc.vector.tensor_tensor_scan(out=w3[:], data0=rmask[:], data1=w2[:], initial=0.0,
                                                                   op0=ALU.mult, op1=ALU.add),
                      reads=[t_rmask, t_w2], writes=[t_w3])
                fw.op(fw.act, lambda: nc.scalar.activation(out=etot[d][:], in_=w3[:, 63::64], func=AF.Exp),
                      reads=[t_w3], writes=[t_etot[d]])
                if d == 1:
                    fw.op(fw.dve, lambda: nc.vector.tensor_tensor(out=w3[:], in0=w3[:], in1=w2[:], op=ALU.subtract),
                          reads=[t_w3, t_w2], writes=[t_w3])
                sgn = 1.0 if d == 0 else -1.0
                fw.op(fw.act, lambda: nc.scalar.activation(out=w2[:], in_=w3[:], func=AF.Exp, scale=sgn),
                      reads=[t_w3], writes=[t_w2])
                fw.op(fw.dve, lambda: nc.vector.tensor_tensor(out=qt[d][:], in0=qs[:], in1=w2[:], op=ALU.mult),
                      reads=[t_qs, t_w2], writes=[t_qt[d]])
                fw.op(fw.act, lambda: nc.scalar.activation(out=w2[:], in_=w3[:], func=AF.Exp, scale=-sgn),
                      reads=[t_w3], writes=[t_w2])
                fw.op(fw.dve, lambda: nc.vector.tensor_tensor(out=kt[d][:], in0=w1[:], in1=w2[:], op=ALU.mult),
                      reads=[t_w1, t_w2], writes=[t_kt[d]])
                for g in range(0, NT, 4):
                    pb, t_pb = get_psb(P)
                    fns = [lambda j=j: nc.tensor.transpose(out=pb[:, j * 128:(j + 1) * 128],
                                                           in_=kt[d][:, (g + j) * 128:(g + j + 1) * 128],
                                                           identity=S.ident[:]) for j in range(4)]
                    fw.mm_group(fns, reads=[t_kt[d], S.t_ident], writes=[t_pb])
                    fw.op(fw.act, lambda: nc.scalar.copy(out=ktm[d][:, g:g + 4, :],
                                                         in_=pb[:, 0:512].rearrange("p (j t) -> p j t", t=128)),
                          reads=[t_pb], writes=[t_ktm[d]], acc=True)
                fw.op(fw.dve, lambda: nc.vector.memset(Wf[d][wcur[d]][:], 0.0), writes=[t_Wf[d][wcur[d]]])

            def tile_steps(d, n):
                c0 = n * 128
                po, t_po = P.f32[3 * d], P.f32t[3 * d]
                tA, t_tA = P.f32[3 * d + 1], P.f32t[3 * d + 1]
                tB, t_tB = P.f32[3 * d + 2], P.f32t[3 * d + 2]
                order = [0, 1] if d == 0 else [1, 0]
                fw.mm_group([lambda: nc.tensor.matmul(tA[:, 0:128], lhsT=kt[d][:, c0:c0 + 128],
                                                      rhs=qt[d][:, c0:c0 + 128], start=True, stop=True)],
                            reads=[t_kt[d], t_qt[d]], writes=[t_tA])
                yield
                fw.op(fw.dve, lambda: nc.vector.tensor_tensor(out=atm[d][:], in0=tA[:, 0:128], in1=m2[d][:], op=ALU.mult),
                      reads=[t_tA, t_m2], writes=[t_atm[d]])
                yield
                rows_a = slice(order[0] * 64, order[0] * 64 + 64)
                rows_b = slice(order[1] * 64, order[1] * 64 + 64)
                fw.mm_group([lambda: nc.tensor.matmul(tB[:, 0:128], lhsT=ktm[d][rows_a, n, :],
                                                      rhs=vtm[rows_a, n, :], start=True, stop=True)],
                            reads=[t_ktm[d], t_vtm], writes=[t_tB])
                yield
                fw.mm_group([lambda: nc.tensor.matmul(po[:, 0:128], lhsT=vtm[:, n, :], rhs=atm[d][:],
                                                      start=True, stop=False)],
                            reads=[t_vtm, t_atm[d]], writes=[t_po])
                fw.mm_group([lambda: nc.tensor.matmul(tA[:, 0:128], lhsT=ktm[d][rows_b, n, :],
                                                      rhs=vtm[rows_b, n, :], start=True, stop=True)],
                            reads=[t_ktm[d], t_vtm], writes=[t_tA])
                yield
                for ci, half in enumerate(order):
                    ch = 2 * n + half
                    cs = c0 + half * 64
                    last = (ci == 1)
                    pk, t_pk = (tB, t_tB) if ci == 0 else (tA, t_tA)
                    ei = max(ch - 1, 0) if d == 0 else ch
                    esc = etot[d][:, ei:ei + 1]
                    cur = wcur[d]
                    nxt = 1 - cur
                    Wc, t_Wc = Wf[d][cur], t_Wf[d][cur]
                    Wn, t_Wn = Wf[d][nxt], t_Wf[d][nxt]
                    fw.op(fw.act, lambda: nc.scalar.activation(out=Sb[d][:], in_=Wc[:], func=AF.Copy, scale=esc),
                          reads=[t_Wc, t_etot[d]], writes=[t_Sb[d]])
                    fw.op(fw.dve, lambda: nc.vector.scalar_tensor_tensor(out=Wn[:], in0=Wc[:], scalar=esc, in1=pk[:, 0:128],
                                                                         op0=ALU.mult, op1=ALU.add),
                          reads=[t_Wc, t_etot[d], t_pk], writes=[t_Wn])
                    wcur[d] = nxt
                    yield
                    fw.mm_group([lambda: nc.tensor.matmul(po[:, half * 64:half * 64 + 64], lhsT=Sb[d][:],
                                                          rhs=qt[d][:, cs:cs + 64], start=False, stop=last)],
                                reads=[t_Sb[d], t_qt[d]], writes=[t_po])
                    yield
                first = (n < NT // 2) if d == 0 else (n >= NT // 2)
                if first:
                    fw.op(fw.act, lambda: nc.scalar.copy(out=osum[:, c0:c0 + 128], in_=po[:, 0:128]),
                          reads=[t_po], writes=[t_osum], acc=True)
                else:
                    fw.op(fw.dve, lambda: nc.vector.tensor_tensor(out=osum[:, c0:c0 + 128], in0=po[:, 0:128],
                                                                  in1=osum[:, c0:c0 + 128], op=ALU.add),
                          reads=[t_po, t_osum], writes=[t_osum], acc=True)
                yield

            def chain(d):
                for i in range(NT):
                    n = i if d == 0 else NT - 1 - i
                    yield from tile_steps(d, n)

            gens = [chain(0), chain(1)]
            alive = [True, True]
            while any(alive):
                for gi_, g_ in enumerate(gens):
                    if alive[gi_]:
                        try:
                            next(g_)
                        except StopIteration:
                            alive[gi_] = False
            fw.dma("sp", w1[:], A.projT[6144 + r:6144 + r + 128, :], reads=[A.t_projT], writes=[t_w1])
            fw.op(fw.act, lambda: nc.scalar.activation(out=w2[:], in_=osum[:], func=AF.Square), reads=[t_osum], writes=[t_w2])
            for b in range(L // 512):
                ps, t_ps = get_ps(P)
                fw.mm_group([lambda: nc.tensor.matmul(ps[:, :], lhsT=ones[:], rhs=w2[:, b * 512:(b + 1) * 512],
                                                      start=True, stop=True)], reads=[t_ones, t_w2], writes=[t_ps])
                fw.op(fw.act, lambda: nc.scalar.activation(out=w3[:, b * 512:(b + 1) * 512], in_=ps[:, :], func=AF.Sqrt,
                                                           scale=1.0 / 128, bias=S.eps[:, 0:1]),
                      reads=[t_ps, S.t_eps], writes=[t_w3], acc=True)
            fw.op(fw.dve, lambda: nc.vector.reciprocal(out=w3[:], in_=w3[:]), reads=[t_w3], writes=[t_w3])
            fw.op(fw.dve, lambda: nc.vector.scalar_tensor_tensor(out=w2[:], in0=osum[:], scalar=ong[:, h:h + 1], in1=w3[:],
                                                                 op0=ALU.mult, op1=ALU.mult),
                  reads=[t_osum, t_ong, t_w3], writes=[t_w2])
            fw.op(fw.act, lambda: nc.scalar.activation(out=w1[:], in_=w1[:], func=AF.Silu), reads=[t_w1], writes=[t_w1])
            fw.op(fw.dve, lambda: nc.vector.tensor_tensor(out=oTb[:], in0=w2[:], in1=w1[:], op=ALU.mult),
                  reads=[t_w2, t_w1], writes=[t_oTb])
            fw.dma("sp", A.oT[r:r + 128, :], oTb[:], reads=[t_oTb], writes=[A.t_oT], acc=True)
        fw.barrier()


def phase_wout(fw, nc, P, S, A, L, oT, t_oT, w_bf, t_w, x_in, t_xin, x_out, t_xout):
    with ExitStack() as es:
        sb = lambda n, shp, dt: es.enter_context(nc.sbuf_tensor(U(n), shp, dt))
        wo = sb("wo", [128, 16, D], BF16); t_wo = Tok("wo")
        wv = w_bf.rearrange("(c p) n -> p c n", p=128)
        for c in range(0, 16, 4):
            fw.dma("sp", wo[:, c:c + 4, :], wv[:, c:c + 4, :], reads=t_w, writes=[t_wo], acc=True)
        ob = [sb("oblk0", [128, 16, 512], BF16), sb("oblk1", [128, 16, 512], BF16)]; t_ob = [Tok("oblk0"), Tok("oblk1")]
        xt = [sb("xr0", [128, D], F32), sb("xr1", [128, D], F32)]; t_xt = [Tok("xr0"), Tok("xr1")]
        ov = oT.rearrange("(h p) t -> p h t", p=128)
        xi = 0
        for tb in range(L // 512):
            o, t_o = ob[tb % 2], t_ob[tb % 2]
            fw.dma("sp", o[:], ov[:, :, tb * 512:(tb + 1) * 512], reads=[t_oT], writes=[t_o])
            for tt in range(4):
                r0 = tb * 512 + tt * 128
                x, t_x = xt[xi % 2], t_xt[xi % 2]
                xi += 1
                fw.dma("sp", x[:], x_in[r0:r0 + 128, :], reads=[t_xin], writes=[t_x])
                for nb in range(4):
                    ps, t_ps = get_ps(P)
                    fns = [lambda h=h: nc.tensor.matmul(ps[:, :], lhsT=o[:, h, tt * 128:(tt + 1) * 128],
                                                        rhs=wo[:, h, nb * 512:(nb + 1) * 512], start=(h == 0), stop=(h == 15))
                           for h in range(16)]
                    fw.mm_group(fns, reads=[t_o, t_wo], writes=[t_ps])
                    fw.op(fw.dve, lambda: nc.vector.tensor_tensor(out=x[:, nb * 512:(nb + 1) * 512], in0=ps[:, :],
                                                                  in1=x[:, nb * 512:(nb + 1) * 512], op=ALU.add),
                          reads=[t_ps, t_x], writes=[t_x], acc=True)
                fw.dma("act", x_out[r0:r0 + 128, :], x[:], reads=[t_x], writes=[t_xout], acc=True)
        fw.barrier()


def phase_ffn(fw, nc, P, S, L, F, wg_bf, wu_bf, wd_bf, t_wg, t_wu, t_wd, x_in, t_xin, x_out, t_xout,
              gains=None, gain_idx=None, hn_in=None, t_hn_in=None, rowscale=None, t_rowscale=None, barrier=True):
    nfc = F // 128
    h1 = (nfc + 1) // 2
    with ExitStack() as es:
        sb = lambda n, shp, dt: es.enter_context(nc.sbuf_tensor(U(n), shp, dt))
        if hn_in is None:
            alloc_norm(nc, es, S, None)
            load_gain(fw, nc, S, gains, gain_idx)
        else:
            S.hn2 = [sb("hnl0", [128, D], BF16), sb("hnl1", [128, D], BF16)]; S.t_hn2 = [Tok("hnl0"), Tok("hnl1")]
            S.hnT = sb("hnT", [128, 16, 512], BF16); S.t_hnT = Tok("hnT")
            rs = sb("rs", [128, L // 128], F32); t_rs = Tok("rs")
            with nc.allow_non_contiguous_dma(reason="tiny"):
                fw.dma("sp", rs[:], rowscale.rearrange("(n p) -> p n", p=128), reads=[t_rowscale], writes=[t_rs])
        hT = sb("hT", [128, nfc, 512], BF16); t_hT = Tok("hT")
        wgs = [sb("wgs0", [128, 16, 256], BF16), sb("wgs1", [128, 16, 256], BF16)]; t_wgs = [Tok("wgs0"), Tok("wgs1")]
        wus = [sb("wus0", [128, 16, 256], BF16), sb("wus1", [128, 16, 256], BF16)]; t_wus = [Tok("wus0"), Tok("wus1")]
        wds = [sb("wds0", [128, h1, 512], BF16), sb("wds1", [128, h1, 512], BF16)]; t_wds = [Tok("wds0"), Tok("wds1")]
        sg = [sb("sg0", [128, 512], F32), sb("sg1", [128, 512], F32)]; t_sg = [Tok("sg0"), Tok("sg1")]
        xs = [sb(f"xs{i}", [128, 512], F32) for i in range(3)]; t_xs = [Tok(f"xs{i}") for i in range(3)]
        wgv = wg_bf.rearrange("(c p) n -> p c n", p=128)
        wuv = wu_bf.rearrange("(c p) n -> p c n", p=128)
        wdv = wd_bf.rearrange("(c p) n -> p c n", p=128)
        slabs = []
        c0 = 0
        while c0 < F:
            w = min(256, F - c0)
            slabs.append((c0, w))
            c0 += w
        wi = 0; si = 0; xi = 0; di = 0
        for tb in range(L // 512):
            for tt in range(4):
                if hn_in is None:
                    norm_T(fw, nc, P, S, x_in, tb * 512 + tt * 128, tt, S.gain, S.t_gain)
                else:
                    r0 = tb * 512 + tt * 128
                    hb, t_hb = S.hn2[tt % 2], S.t_hn2[tt % 2]
                    fw.dma("sp", hb[:], hn_in[r0:r0 + 128, :], reads=[t_hn_in], writes=[t_hb])
                    for g in range(4):
                        pb, t_pb = get_psb(P)
                        fns = [lambda j=j: nc.tensor.transpose(out=pb[:, j * 128:(j + 1) * 128],
                                                               in_=hb[:, (4 * g + j) * 128:(4 * g + j + 1) * 128],
                                                               identity=S.ident[:]) for j in range(4)]
                        fw.mm_group(fns, reads=[t_hb, S.t_ident], writes=[t_pb])
                        dst = S.hnT[:, 4 * g:4 * g + 4, tt * 128:(tt + 1) * 128]
                        src = pb[:, 0:512].rearrange("p (j t) -> p j t", t=128)
                        if g % 2 == 0:
                            fw.op(fw.act, lambda: nc.scalar.copy(out=dst, in_=src), reads=[t_pb], writes=[S.t_hnT], acc=True)
                        else:
                            fw.op(fw.dve, lambda: nc.vector.tensor_copy(out=dst, in_=src), reads=[t_pb], writes=[S.t_hnT], acc=True)
            for (c0, w) in slabs:
                wg, t_wgs_ = wgs[wi % 2], t_wgs[wi % 2]
                wu, t_wus_ = wus[wi % 2], t_wus[wi % 2]
                wi += 1
                fw.dma("sp", wg[:, :, 0:w], wgv[:, :, c0:c0 + w], reads=[t_wg[c0 // 2048]], writes=[t_wgs_])
                fw.dma("sp", wu[:, :, 0:w], wuv[:, :, c0:c0 + w], reads=[t_wu[c0 // 2048]], writes=[t_wus_])
                for j in range(w // 128):
                    fc = c0 // 128 + j
                    psg, t_psg = get_ps(P)
                    fw.mm_group([lambda c=c: nc.tensor.matmul(psg[:, :], lhsT=wg[:, c, j * 128:(j + 1) * 128],
                                                              rhs=S.hnT[:, c, :], start=(c == 0), stop=(c == 15))
                                 for c in range(16)], reads=[t_wgs_, S.t_hnT], writes=[t_psg])
                    psu, t_psu = get_ps(P)
                    fw.mm_group([lambda c=c: nc.tensor.matmul(psu[:, :], lhsT=wu[:, c, j * 128:(j + 1) * 128],
                                                              rhs=S.hnT[:, c, :], start=(c == 0), stop=(c == 15))
                                 for c in range(16)], reads=[t_wus_, S.t_hnT], writes=[t_psu])
                    s_, t_s = sg[si % 2], t_sg[si % 2]
                    si += 1
                    fw.op(fw.act, lambda: nc.scalar.activation(out=s_[:], in_=psg[:, :], func=AF.Silu),
                          reads=[t_psg], writes=[t_s])
                    fw.op(fw.dve, lambda: nc.vector.tensor_tensor(out=hT[:, fc, :], in0=s_[:], in1=psu[:, :], op=ALU.mult),
                          reads=[t_s, t_psu], writes=[t_hT], acc=True)
            for nb in range(4):
                banks = [get_ps(P) for _ in range(4)]
                for half in range(2):
                    f0 = 0 if half == 0 else h1
                    f1 = h1 if half == 0 else nfc
                    wd, t_wds_ = wds[di % 2], t_wds[di % 2]
                    di += 1
                    fw.dma("sp", wd[:, 0:f1 - f0, :], wdv[:, f0:f1, nb * 512:(nb + 1) * 512], reads=t_wd, writes=[t_wds_])
                    for tt in range(4):
                        ps, t_ps = banks[tt]
                        fw.mm_group([lambda fc=fc: nc.tensor.matmul(ps[:, :], lhsT=hT[:, fc, tt * 128:(tt + 1) * 128],
                                                                    rhs=wd[:, fc - f0, :], start=(fc == 0), stop=(fc == nfc - 1))
                                     for fc in range(f0, f1)], reads=[t_hT, t_wds_], writes=[t_ps])
                for tt in range(4):
                    ps, t_ps = banks[tt]
                    r0 = tb * 512 + tt * 128
                    x, t_x = xs[xi % 3], t_xs[xi % 3]
                    xi += 1
                    if hn_in is None:
                        fw.dma("sp", x[:], x_in[r0:r0 + 128, nb * 512:(nb + 1) * 512], reads=[t_xin], writes=[t_x])
                        fw.op(fw.dve, lambda: nc.vector.tensor_tensor(out=x[:], in0=ps[:, :], in1=x[:], op=ALU.add),
                              reads=[t_ps, t_x], writes=[t_x])
                    else:
                        n = tb * 4 + tt
                        fw.op(fw.act, lambda: nc.scalar.activation(out=x[:], in_=ps[:, :], func=AF.Copy, scale=rs[:, n:n + 1]),
                              reads=[t_ps, t_rs], writes=[t_x])
                    fw.dma("act", x_out[r0:r0 + 128, nb * 512:(nb + 1) * 512], x[:], reads=[t_x], writes=[t_xout], acc=True)
        if barrier:
            fw.barrier()


def attn_consts(L):
    c = {}
    half = 64
    inv_freq = (np.float32(10000.0) ** (-np.arange(half, dtype=np.float32) * np.float32(2.0) / np.float32(128))).astype(np.float32)
    pos = np.arange(L, dtype=np.float32)
    ang = (pos[:, None] * inv_freq[None, :]).astype(np.float32)
    c["cos"] = np.cos(ang).astype(np.float32)
    c["sin"] = np.sin(ang).astype(np.float32)
    sl = np.arange(128)[:, None, None]
    j = np.arange(23)[None, :, None]
    tl = np.arange(128)[None, None, :]
    off = (tl - sl) - 128 * (11 - j)
    a = np.abs(off)
    m = (a <= 64).astype(np.float32) + ((off % 4 == 0) & (a <= 256)).astype(np.float32) + ((off % 16 == 0) & (a <= 1024)).astype(np.float32)
    c["amask"] = m.reshape(128, 23 * 128).astype(ml_dtypes.bfloat16)
    c["ident32"] = np.eye(128, dtype=np.float32)
    return c


def get_ps_sub(P, lo, hi, key):
    n = hi - lo
    i = getattr(P, key, 0)
    setattr(P, key, i + 1)
    k = lo + (i % n)
    return P.f32[k], P.f32t[k]


def phase_attn_proj(fw, nc, P, S, A, L):
    NT = L // 128
    with ExitStack() as es:
        sb = lambda n, shp, dt: es.enter_context(nc.sbuf_tensor(U(n), shp, dt))
        alloc_norm(nc, es, S, None)
        load_gain(fw, nc, S, A.gains, 2)
        wsl = [sb("awsl0", [128, 16, 512], BF16), sb("awsl1", [128, 16, 512], BF16)]; t_wsl = [Tok("awsl0"), Tok("awsl1")]
        cosT = sb("cosT", [128, NT, 64], F32); sinT = sb("sinT", [128, NT, 64], F32); t_cs = Tok("cs")
        fw.dma("sp", cosT[:], A.consts["cos"].rearrange("(n p) f -> p n f", p=128), writes=[t_cs], acc=True)
        fw.dma("sp", sinT[:], A.consts["sin"].rearrange("(n p) f -> p n f", p=128), writes=[t_cs], acc=True)
        gqk = sb("gqk", [128, 2, 128], F32); t_gqk = Tok("gqk")
        fw.dma("sp", gqk[:, 0, :], A.q_gain.partition_broadcast(128), writes=[t_gqk], acc=True)
        fw.dma("sp", gqk[:, 1, :], A.k_gain.partition_broadcast(128), writes=[t_gqk], acc=True)
        fw.op(fw.act, lambda: nc.scalar.mul(out=gqk[:, 0, :], in_=gqk[:, 0, :], mul=float(128 ** -0.5)), reads=[t_gqk], writes=[t_gqk])
        sq = [sb(f"asq{i}", [128, 512], F32) for i in range(4)]; t_sq = [Tok(f"asq{i}") for i in range(4)]
        st = [sb(f"ast{i}", [128, 12], F32) for i in range(4)]; t_st = [Tok(f"ast{i}") for i in range(4)]
        t1 = [sb(f"at1{i}", [128, 512], F32) for i in range(4)]; t_t1 = [Tok(f"at1{i}") for i in range(4)]
        ra = [sb(f"ara{i}", [128, 2, 4, 64], F32) for i in range(4)]; t_ra = [Tok(f"ara{i}") for i in range(4)]
        rb = [sb(f"arb{i}", [128, 2, 4, 64], F32) for i in range(4)]; t_rb = [Tok(f"arb{i}") for i in range(4)]
        qr = [sb(f"aqr{i}", [128, 512], BF16) for i in range(4)]; t_qr = [Tok(f"aqr{i}") for i in range(4)]
        stg = [sb("astg0", [128, 4, 512], BF16), sb("astg1", [128, 4, 512], BF16)]; t_stg = [Tok("astg0"), Tok("astg1")]
        vo = [sb("avo0", [128, 512], BF16), sb("avo1", [128, 512], BF16)]; t_vo = [Tok("avo0"), Tok("avo1")]
        wv = A.wqkv_bf.rearrange("(c p) n -> p c n", p=128)
        wi = 0; qi = 0; vi = 0; gi = 0
        for tb in range(L // 512):
            for tt in range(4):
                norm_T(fw, nc, P, S, A.x2, tb * 512 + tt * 128, tt, S.gain, S.t_gain)
            for s in range(12):
                w, t_w = wsl[wi % 2], t_wsl[wi % 2]
                wi += 1
                fw.dma("sp", w[:], wv[:, :, s * 512:(s + 1) * 512], reads=[A.t_wqkv[s // 4]], writes=[t_w])
                if s < 8:
                    sg_, t_sg = stg[gi % 2], t_stg[gi % 2]
                    gi += 1
                def tile_gen(tt):
                    n = tb * 4 + tt
                    r0 = n * 128
                    ps, t_ps = P.f32[tt], P.f32t[tt]
                    fw.mm_group([lambda c=c: nc.tensor.matmul(ps[:, :], lhsT=S.hnT[:, c, tt * 128:(tt + 1) * 128],
                                                              rhs=w[:, c, :], start=(c == 0), stop=(c == 15))
                                 for c in range(16)], reads=[t_w, S.t_hnT], writes=[t_ps])
                    yield
                    if s >= 8:
                        o, t_o = vo[tt % 2], t_vo[tt % 2]
                        fw.op(fw.act, lambda: nc.scalar.copy(out=o[:], in_=ps[:, :]), reads=[t_ps], writes=[t_o])
                        fw.dma("act", A.v_att[r0:r0 + 128, (s - 8) * 512:(s - 7) * 512], o[:], reads=[t_o],
                               writes=[A.t_v_att], acc=True)
                        return
                    which = 0 if s < 4 else 1
                    sq_, t_sq_ = sq[tt], t_sq[tt]
                    st_, t_st_ = st[tt], t_st[tt]
                    t1_, t_t1_ = t1[tt], t_t1[tt]
                    ra_, t_ra_ = ra[tt], t_ra[tt]
                    rb_, t_rb_ = rb[tt], t_rb[tt]
                    q_, t_q = qr[tt], t_qr[tt]
                    fw.op(fw.act, lambda: nc.scalar.activation(out=sq_[:], in_=ps[:, :], func=AF.Square), reads=[t_ps], writes=[t_sq_])
                    yield
                    fw.op(fw.dve, lambda: nc.vector.tensor_reduce(out=st_[:, 0:4], in_=sq_[:].rearrange("p (h d) -> p h d", d=128),
                                                                  axis=AX.X, op=ALU.add), reads=[t_sq_], writes=[t_st_])
                    yield
                    fw.op(fw.act, lambda: nc.scalar.activation(out=st_[:, 4:8], in_=st_[:, 0:4], func=AF.Sqrt, scale=1.0 / 128,
                                                               bias=S.eps[:, 0:1]), reads=[t_st_, S.t_eps], writes=[t_st_])
                    yield
                    fw.op(fw.dve, lambda: nc.vector.reciprocal(out=st_[:, 8:12], in_=st_[:, 4:8]), reads=[t_st_], writes=[t_st_])
                    yield
                    t1v = t1_[:].rearrange("p (h d) -> p h d", d=128)
                    fw.op(fw.dve, lambda: nc.vector.tensor_tensor(out=t1v, in0=ps[:, :].rearrange("p (h d) -> p h d", d=128),
                                                                  in1=st_[:, 8:12].unsqueeze(2).to_broadcast([128, 4, 128]), op=ALU.mult),
                          reads=[t_ps, t_st_], writes=[t_t1_])
                    yield
                    fw.op(fw.dve, lambda: nc.vector.tensor_tensor(out=t1v, in0=t1v,
                                                                   in1=gqk[:, which:which + 1, :].to_broadcast([128, 4, 128]), op=ALU.mult),
                          reads=[t_t1_, t_gqk], writes=[t_t1_])
                    yield
                    x1v = t1v[:, :, 0:64]
                    x2v = t1v[:, :, 64:128]
                    cb = cosT[:, n:n + 1, :].to_broadcast([128, 4, 64])
                    sbb = sinT[:, n:n + 1, :].to_broadcast([128, 4, 64])
                    qv = q_[:].rearrange("p (h d) -> p h d", d=128)
                    fw.op(fw.dve, lambda: nc.vector.tensor_tensor(out=ra_[:, 0], in0=x1v, in1=cb, op=ALU.mult), reads=[t_t1_, t_cs], writes=[t_ra_])
                    fw.op(fw.dve, lambda: nc.vector.tensor_tensor(out=rb_[:, 0], in0=x2v, in1=sbb, op=ALU.mult), reads=[t_t1_, t_cs], writes=[t_rb_])
                    yield
                    fw.op(fw.dve, lambda: nc.vector.tensor_tensor(out=ra_[:, 1], in0=x2v, in1=cb, op=ALU.mult), reads=[t_t1_, t_cs], writes=[t_ra_], acc=True)
                    fw.op(fw.dve, lambda: nc.vector.tensor_tensor(out=rb_[:, 1], in0=x1v, in1=sbb, op=ALU.mult), reads=[t_t1_, t_cs], writes=[t_rb_], acc=True)
                    yield
                    fw.op(fw.dve, lambda: nc.vector.tensor_tensor(out=qv[:, :, 0:64], in0=ra_[:, 0], in1=rb_[:, 0], op=ALU.subtract),
                          reads=[t_ra_, t_rb_], writes=[t_q])
                    fw.op(fw.dve, lambda: nc.vector.tensor_tensor(out=qv[:, :, 64:128], in0=ra_[:, 1], in1=rb_[:, 1], op=ALU.add),
                          reads=[t_ra_, t_rb_], writes=[t_q], acc=True)
                    yield
                    pb, t_pb = get_psb(P)
                    fw.mm_group([lambda j=j: nc.tensor.transpose(out=pb[:, j * 128:(j + 1) * 128], in_=q_[:, j * 128:(j + 1) * 128],
                                                                 identity=S.ident[:]) for j in range(4)],
                                reads=[t_q, S.t_ident], writes=[t_pb])
                    fw.op(fw.act, lambda: nc.scalar.copy(out=sg_[:, :, tt * 128:(tt + 1) * 128],
                                                         in_=pb[:, 0:512].rearrange("p (j t) -> p j t", t=128)),
                          reads=[t_pb], writes=[t_sg], acc=True)
                    yield

                gens = [tile_gen(tt) for tt in range(4)]
                alive = [True] * 4
                while any(alive):
                    for gi_, g_ in enumerate(gens):
                        if alive[gi_]:
                            try:
                                next(g_)
                            except StopIteration:
                                alive[gi_] = False
                if s < 8:
                    dstT = A.qT if s < 4 else A.kT
                    t_dst = A.t_qT if s < 4 else A.t_kT
                    h0 = (s % 4) * 4
                    for j in range(4):
                        fw.dma("act", dstT[(h0 + j) * 128:(h0 + j + 1) * 128, tb * 512:(tb + 1) * 512], sg_[:, j, :],
                               reads=[t_sg], writes=[t_dst], acc=True)
        fw.barrier()


def phase_attn_core(fw, nc, P, S, A, L):
    NT = L // 128
    NG = L // 512
    with ExitStack() as es:
        sb = lambda n, shp, dt: es.enter_context(nc.sbuf_tensor(U(n), shp, dt))
        amask = sb("amask", [128, 23 * 128], BF16); t_amask = Tok("amask")
        fw.dma("sp", amask[:], A.consts["amask"][:, :], writes=[t_amask])
        onesb = sb("onesb", [128, 128], BF16); t_onesb = Tok("onesb")
        fw.op(fw.dve, lambda: nc.vector.memset(onesb[:], 1.0), writes=[t_onesb])
        qT = [sb("aq0", [128, L], BF16), sb("aq1", [128, L], BF16)]; t_q = [Tok("aq0"), Tok("aq1")]
        kT = [sb("ak0", [128, L], BF16), sb("ak1", [128, L], BF16)]; t_k = [Tok("ak0"), Tok("ak1")]
        vh = [sb("av0", [128, NT, 128], BF16), sb("av1", [128, NT, 128], BF16)]; t_v = [Tok("av0"), Tok("av1")]
        oh = [sb("ao0", [128, L], BF16), sb("ao1", [128, L], BF16)]; t_o = [Tok("ao0"), Tok("ao1")]
        pe_ = [sb(f"ape{i}", [128, 512], BF16) for i in range(4)]; t_pe = [Tok(f"ape{i}") for i in range(4)]
        pm = [sb(f"apm{i}", [128, 512], BF16) for i in range(4)]; t_pm = [Tok(f"apm{i}") for i in range(4)]
        rden = [sb("arden0", [128, 512], F32), sb("arden1", [128, 512], F32)]; t_rden = [Tok("arden0"), Tok("arden1")]
        LA = 3
        iters = []
        for h in range(16):
            for g in range(NG):
                qb0 = 4 * g
                kbs = list(range(max(0, qb0 - 8), min(NT, qb0 + 12)))
                for ki, kb in enumerate(kbs):
                    iters.append((h, g, ki, kb, len(kbs)))
        po, t_po = P.f32[4], P.f32t[4]
        pd, t_pd = P.f32[5], P.f32t[5]
        rden_ = rden[0]; t_rden_ = t_rden[0]

        def load_head(h):
            b = h % 2
            r = h * 128
            fw.dma("sp", qT[b][:], A.qT[r:r + 128, :], reads=[A.t_qT], writes=[t_q[b]])
            fw.dma("sp", kT[b][:], A.kT[r:r + 128, :], reads=[A.t_kT], writes=[t_k[b]])
            fw.dma("sp", vh[b][:], A.v_att[:, r:r + 128].rearrange("(n p) v -> p n v", p=128), reads=[A.t_v_att], writes=[t_v[b]])

        def emit_st(i):
            h, g, ki, kb, nk = iters[i]
            b = h % 2
            if g == 0 and ki == 0 and h == 0:
                load_head(0)
            ps, t_ps = P.f32[i % 4], P.f32t[i % 4]
            t0 = g * 512
            fw.mm_group([lambda: nc.tensor.matmul(ps[:, :], lhsT=kT[b][:, kb * 128:(kb + 1) * 128],
                                                  rhs=qT[b][:, t0:t0 + 512], start=True, stop=True)],
                        reads=[t_k[b], t_q[b]], writes=[t_ps])

        def emit_rest(i):
            h, g, ki, kb, nk = iters[i]
            b = h % 2
            qb0 = 4 * g
            t0 = g * 512
            ps, t_ps = P.f32[i % 4], P.f32t[i % 4]
            e_, t_e = pe_[i % 4], t_pe[i % 4]
            m_, t_m = pm[i % 4], t_pm[i % 4]
            if g == 0 and ki == 0 and h + 1 < 16:
                load_head(h + 1)
            fw.op(fw.act, lambda: nc.scalar.activation(out=e_[:], in_=ps[:, :], func=AF.Exp), reads=[t_ps], writes=[t_e])
            j0 = 11 - (kb - qb0)
            use_dve = True
            eng = fw.dve if use_dve else fw.pool
            raw = nc.vector if use_dve else nc.gpsimd
            fw.op(eng, lambda: raw.tensor_tensor(out=m_[:], in0=e_[:], in1=amask[:, j0 * 128:j0 * 128 + 512], op=ALU.mult),
                  reads=[t_e, t_amask], writes=[t_m])
            first = ki == 0
            last = ki == nk - 1
            fw.mm_group([lambda: nc.tensor.matmul(po[:, :], lhsT=vh[b][:, kb, :], rhs=m_[:], start=first, stop=last)],
                        reads=[t_v[b], t_m], writes=[t_po])
            fw.mm_group([lambda: nc.tensor.matmul(pd[:, :], lhsT=onesb[:], rhs=m_[:], start=first, stop=last)],
                        reads=[t_onesb, t_m], writes=[t_pd])
            if last:
                fw.op(fw.dve, lambda: nc.vector.reciprocal(out=rden_[:], in_=pd[:, :]), reads=[t_pd], writes=[t_rden_])
                fw.op(fw.dve, lambda: nc.vector.tensor_tensor(out=oh[b][:, t0:t0 + 512], in0=po[:, :], in1=rden_[:], op=ALU.mult),
                      reads=[t_po, t_rden_], writes=[t_o[b]], acc=True)
                if g == NG - 1:
                    r = h * 128
                    fw.dma("sp", A.oT[r:r + 128, :], oh[b][:], reads=[t_o[b]], writes=[A.t_oT], acc=True)

        n_it = len(iters)
        for i in range(min(LA, n_it)):
            emit_st(i)
        for i in range(n_it):
            emit_rest(i)
            if i + LA < n_it:
                emit_st(i + LA)
        fw.barrier()


def phase_router(fw, nc, P, S, A, L):
    with ExitStack() as es:
        sb = lambda n, shp, dt: es.enter_context(nc.sbuf_tensor(U(n), shp, dt))
        alloc_norm(nc, es, S, None)
        load_gain(fw, nc, S, A.gains, 3)
        id32 = sb("id32", [128, 128], F32); t_id32 = Tok("id32")
        fw.dma("sp", id32[:], A.consts["ident32"][:, :], writes=[t_id32])
        wr = sb("wr", [128, 16, 8], F32); t_wr = Tok("wr")
        with nc.allow_non_contiguous_dma(reason="small router weight"):
            fw.dma("sp", wr[:], A.w_router.rearrange("(c p) e -> p c e", p=128), writes=[t_wr])
        hn32 = sb("hn32", [128, D], F32); t_hn32 = Tok("hn32")
        hT32 = sb("hT32", [128, 16, 128], F32); t_hT32 = Tok("hT32")
        lg = sb("lg", [128, 8], F32); t_lg = Tok("lg")
        l2 = sb("l2", [128, 8], F32); t_l2 = Tok("l2")
        sm = sb("sm", [128, 8], F32); t_sm = Tok("sm")
        gt = sb("gt", [128, 8], F32); t_gt = Tok("gt")
        for n in range(L // 128):
            r0 = n * 128
            k = S.xi % 2
            S.xi += 1
            xt, t_x = S.xt[k], S.t_xt[k]
            fw.dma("sp", xt[:], A.x3[r0:r0 + 128, :], reads=[A.t_x3], writes=[t_x])
            fw.op(fw.act, lambda: nc.scalar.activation(out=S.junk[:], in_=xt[:], func=AF.Square), reads=[t_x], writes=[S.t_junk])
            fw.op(fw.dve, lambda: nc.vector.tensor_reduce(out=S.ss[:, 0:1], in_=S.junk[:], axis=AX.X, op=ALU.add),
                  reads=[S.t_junk], writes=[S.t_ss])
            fw.op(fw.act, lambda: nc.scalar.activation(out=S.ss[:, 1:2], in_=S.ss[:, 0:1], func=AF.Sqrt, scale=1.0 / D,
                                                       bias=S.eps[:, 0:1]), reads=[S.t_ss, S.t_eps], writes=[S.t_ss2])
            fw.op(fw.dve, lambda: nc.vector.reciprocal(out=S.ss[:, 2:3], in_=S.ss[:, 1:2]), reads=[S.t_ss2], writes=[S.t_rstd])
            fw.op(fw.dve, lambda: nc.vector.scalar_tensor_tensor(out=hn32[:], in0=xt[:], scalar=S.ss[:, 2:3], in1=S.gain[:],
                                                                 op0=ALU.mult, op1=ALU.mult),
                  reads=[t_x, S.t_rstd, S.t_gain], writes=[t_hn32])
            fw.op(fw.act, lambda: nc.scalar.copy(out=S.hn[:], in_=hn32[:]), reads=[t_hn32], writes=[S.t_hn])
            fw.dma("sp", A.hn3[r0:r0 + 128, :], S.hn[:], reads=[S.t_hn], writes=[A.t_hn3], acc=True)
            for g in range(4):
                ps, t_ps = get_ps(P)
                fw.mm_group([lambda j=j: nc.tensor.transpose(out=ps[:, j * 128:(j + 1) * 128],
                                                             in_=hn32[:, (4 * g + j) * 128:(4 * g + j + 1) * 128],
                                                             identity=id32[:]) for j in range(4)],
                            reads=[t_hn32, t_id32], writes=[t_ps])
                fw.op(fw.act if g % 2 else fw.dve,
                      (lambda: nc.scalar.copy(out=hT32[:, 4 * g:4 * g + 4, :], in_=ps[:, :].rearrange("p (j t) -> p j t", t=128))) if g % 2 else
                      (lambda: nc.vector.tensor_copy(out=hT32[:, 4 * g:4 * g + 4, :], in_=ps[:, :].rearrange("p (j t) -> p j t", t=128))),
                      reads=[t_ps], writes=[t_hT32], acc=True)
            ps, t_ps = get_ps(P)
            fw.mm_group([lambda c=c: nc.tensor.matmul(ps[:, 0:8], lhsT=hT32[:, c, :], rhs=wr[:, c, :], start=(c == 0), stop=(c == 15))
                         for c in range(16)], reads=[t_hT32, t_wr], writes=[t_ps])
            fw.op(fw.dve, lambda: nc.vector.tensor_copy(out=lg[:], in_=ps[:, 0:8]), reads=[t_ps], writes=[t_lg])
            fw.op(fw.dve, lambda: nc.vector.tensor_reduce(out=sm[:, 0:1], in_=lg[:], axis=AX.X, op=ALU.max), reads=[t_lg], writes=[t_sm])
            fw.op(fw.dve, lambda: nc.vector.tensor_scalar(out=l2[:], in0=lg[:], scalar1=sm[:, 0:1], scalar2=-1e30,
                                                          op0=ALU.is_equal, op1=ALU.mult), reads=[t_lg, t_sm], writes=[t_l2])
            fw.op(fw.dve, lambda: nc.vector.tensor_tensor(out=l2[:], in0=l2[:], in1=lg[:], op=ALU.add), reads=[t_l2, t_lg], writes=[t_l2])
            fw.op(fw.dve, lambda: nc.vector.tensor_reduce(out=sm[:, 1:2], in_=l2[:], axis=AX.X, op=ALU.max), reads=[t_l2], writes=[t_sm])
            fw.op(fw.dve, lambda: nc.vector.tensor_scalar(out=l2[:], in0=lg[:], scalar1=sm[:, 1:2], scalar2=None, op0=ALU.is_ge),
                  reads=[t_lg, t_sm], writes=[t_l2])
            fw.op(fw.dve, lambda: nc.vector.tensor_scalar(out=sm[:, 2:3], in0=sm[:, 0:1], scalar1=-1.0, scalar2=None, op0=ALU.mult),
                  reads=[t_sm], writes=[t_sm])
            fw.op(fw.act, lambda: nc.scalar.activation(out=gt[:], in_=lg[:], func=AF.Exp, bias=sm[:, 2:3], scale=1.0),
                  reads=[t_lg, t_sm], writes=[t_gt])
            fw.op(fw.dve, lambda: nc.vector.tensor_tensor(out=gt[:], in0=gt[:], in1=l2[:], op=ALU.mult), reads=[t_gt, t_l2], writes=[t_gt])
            fw.op(fw.dve, lambda: nc.vector.tensor_reduce(out=sm[:, 3:4], in_=gt[:], axis=AX.X, op=ALU.add), reads=[t_gt], writes=[t_sm])
            fw.op(fw.dve, lambda: nc.vector.reciprocal(out=sm[:, 4:5], in_=sm[:, 3:4]), reads=[t_sm], writes=[t_sm])
            fw.op(fw.dve, lambda: nc.vector.tensor_scalar(out=gt[:], in0=gt[:], scalar1=sm[:, 4:5], scalar2=None, op0=ALU.mult),
                  reads=[t_gt, t_sm], writes=[t_gt])
            fw.dma("sp", A.gates[r0:r0 + 128, :], gt[:], reads=[t_gt], writes=[A.t_gates], acc=True)
        fw.barrier()


def build_A(L, stages=("hgrn", "ffn", "attn", "router"), dbg=False, stop_after=8):
    nc = bass.Bass("TRN2", target_bir_lowering=False)
    A = Ctx(); S = Ctx()
    din = lambda n, shp, dt=F32: nc.dram_tensor(n, shp, dt, kind="ExternalInput").ap()
    dint = lambda n, shp, dt: nc.dram_tensor(n, shp, dt).ap()
    dout = lambda n, shp, dt: nc.dram_tensor(n, shp, dt, kind="ExternalOutput").ap()
    A.x = din("x", [L, D]); A.gains = din("gains", [4, D])
    w_in = din("w_in", [D, 10240]); A.lb_logits = din("lb_logits", [3, 2, 2048]); A.onorm = din("onorm", [2048])
    w_out0 = din("w_out0", [D, D])
    wg = din("wg", [D, FF]); wu = din("wu", [D, FF]); wd = din("wd", [FF, D])
    wqkv = din("wqkv", [D, 3 * D]); A.q_gain = din("q_gain", [128]); A.k_gain = din("k_gain", [128]); w_out1 = din("w_out1", [D, D])
    A.w_router = din("w_router", [D, 8])
    cn = make_consts(); cn.update(attn_consts(L))
    A.consts = {k: din("c_" + k, list(v.shape), BF16 if v.dtype == ml_dtypes.bfloat16 else F32) for k, v in cn.items()}
    A.w_in_bf = dint("w_in_bf", [D, 10240], BF16); A.t_w_in = Tok("w_in")
    w_out0_bf = dint("w_out0_bf", [D, D], BF16); t_w_out0 = Tok("w_out0")
    wg_bf = dint("wg_bf", [D, FF], BF16); wu_bf = dint("wu_bf", [D, FF], BF16); wd_bf = dint("wd_bf", [FF, D], BF16); t_wffn = Tok("wffn")
    A.wqkv_bf = dint("wqkv_bf", [D, 3 * D], BF16); A.t_wqkv = Tok("wqkv")
    w_out1_bf = dint("w_out1_bf", [D, D], BF16); t_w_out1 = Tok("w_out1")
    A.projT = dint("projT", [8192, L], F32); A.t_projT = Tok("projT")
    A.v_tm = dint("v_tm", [L, 2048], BF16); A.t_v_tm = Tok("v_tm")
    A.oT = dint("oT", [2048, L], BF16); A.t_oT = Tok("oT")
    mk = (lambda n, shp, dt: dout(n, shp, dt)) if dbg else (lambda n, shp, dt: dint(n, shp, dt))
    x1 = mk("x1", [L, D], F32); t_x1 = Tok("x1")
    A.x2 = mk("x2", [L, D], F32); A.t_x2 = Tok("x2")
    A.qT = dint("qT", [2048, L], BF16); A.t_qT = Tok("qT")
    A.kT = dint("kT", [2048, L], BF16); A.t_kT = Tok("kT")
    A.v_att = dint("v_att", [L, 2048], BF16); A.t_v_att = Tok("v_att")
    A.x3 = dout("x3", [L, D], F32); A.t_x3 = Tok("x3")
    A.hn3 = dout("hn3", [L, D], BF16); A.t_hn3 = Tok("hn3")
    A.gates = dout("gates", [L, 8], F32); A.t_gates = Tok("gates")
    t_x = Tok("x")
    with ExitStack() as es:
        fw = FW(nc, es)
        P = psum_pools(nc, es, fw)
        alloc_common(nc, es, fw, S, A.consts)
        A.t_w_in = cast_weight(fw, nc, A.w_in_bf, w_in, D, 10240, "w_in")
        t_w_out0 = cast_weight(fw, nc, w_out0_bf, w_out0, D, D, "w_out0")
        t_wg, t_wu = cast_weights_interleaved(fw, nc, [(wg_bf, wg, D, FF, "wg"), (wu_bf, wu, D, FF, "wu")])
        t_wd = cast_weight(fw, nc, wd_bf, wd, FF, D, "wd")
        A.t_wqkv = cast_weight(fw, nc, A.wqkv_bf, wqkv, D, 3 * D, "wqkv")
        t_w_out1 = cast_weight(fw, nc, w_out1_bf, w_out1, D, D, "w_out1")
        phases = [
            lambda: phase_hgrn_proj(fw, nc, P, S, A, L),
            lambda: phase_hgrn_scan(fw, nc, P, S, A, L),
            lambda: phase_wout(fw, nc, P, S, A, L, A.oT, A.t_oT, w_out0_bf, t_w_out0, A.x, t_x, x1, t_x1),
            lambda: phase_ffn(fw, nc, P, S, L, FF, wg_bf, wu_bf, wd_bf, t_wg, t_wu, t_wd, x1, t_x1, A.x2, A.t_x2, gains=A.gains, gain_idx=1),
            lambda: phase_attn_proj(fw, nc, P, S, A, L),
            lambda: phase_attn_core(fw, nc, P, S, A, L),
            lambda: phase_wout(fw, nc, P, S, A, L, A.oT, A.t_oT, w_out1_bf, t_w_out1, A.x2, A.t_x2, A.x3, A.t_x3),
            lambda: phase_router(fw, nc, P, S, A, L),
        ]
        fw.barrier()
        for ph in phases[:stop_after]:
            ph()
        fw.final_wait([A.t_x3, A.t_hn3, A.t_gates] + ([t_x1, A.t_x2] if dbg else []))
        print("ninst", fw.ninst, "counts", [(e.name, e.count) for e in fw.engs], flush=True)
    return nc, cn


def inputs_A(d, b, L, cn):
    inm = {"x": np.ascontiguousarray(d['x'][b, :L]), "gains": d['norm_gains'].reshape(4, D), "w_in": d['hgrn_w_in'][0],
           "lb_logits": d['hgrn_lb_logits'], "onorm": d['hgrn_onorm'][0], "w_out0": d['hgrn_w_out'][0],
           "wg": d['ffn_w_gate'][0], "wu": d['ffn_w_up'][0], "wd": d['ffn_w_down'][0],
           "wqkv": d['attn_w_qkv'][0], "q_gain": d['attn_q_gain'][0], "k_gain": d['attn_k_gain'][0], "w_out1": d['attn_w_out'][0],
           "w_router": d['moe_w_router'][0]}
    for k, v in cn.items():
        inm["c_" + k] = v
    return inm


FE = 7168


def build_B(cap):
    nc = bass.Bass("TRN2", target_bir_lowering=False)
    S = Ctx()
    din = lambda n, shp, dt=F32: nc.dram_tensor(n, shp, dt, kind="ExternalInput").ap()
    dint = lambda n, shp, dt: nc.dram_tensor(n, shp, dt).ap()
    hn_g = din("hn_g", [cap, D], BF16); rs = din("rs", [cap]); t_in = Tok("in")
    wg = din("wg", [D, FE]); wu = din("wu", [D, FE]); wd = din("wd", [FE, D])
    cn = make_consts()
    consts = {k: din("c_" + k, list(v.shape), BF16) for k, v in cn.items()}
    wg_bf = dint("wg_bf", [D, FE], BF16); wu_bf = dint("wu_bf", [D, FE], BF16); wd_bf = dint("wd_bf", [FE, D], BF16)
    t_w = Tok("w")
    y = nc.dram_tensor("y", [cap, D], F32, kind="ExternalOutput").ap(); t_y = Tok("y")
    with ExitStack() as es:
        fw = FW(nc, es)
        P = psum_pools(nc, es, fw)
        alloc_common(nc, es, fw, S, consts)
        t_wg, t_wu = cast_weights_interleaved(fw, nc, [(wg_bf, wg, D, FE, "wg"), (wu_bf, wu, D, FE, "wu")])
        t_wd = cast_weight(fw, nc, wd_bf, wd, FE, D, "wd")
        phase_ffn(fw, nc, P, S, cap, FE, wg_bf, wu_bf, wd_bf, t_wg, t_wu, t_wd, None, None, y, t_y,
                  hn_in=hn_g, t_hn_in=t_in, rowscale=rs, t_rowscale=t_in)
        fw.final_wait([t_y])
        print("B ninst", fw.ninst, "counts", [(e.name, e.count) for e in fw.engs], flush=True)
    return nc, cn


def build_C(T):
    nc = bass.Bass("TRN2", target_bir_lowering=False)
    din = lambda n, shp, dt=F32: nc.dram_tensor(n, shp, dt, kind="ExternalInput").ap()
    x3 = din("x3", [T, D]); y0 = din("y0", [T, D]); y1 = din("y1", [T, D])
    out = nc.dram_tensor("out", [T, D], F32, kind="ExternalOutput").ap(); t_out = Tok("out")
    with ExitStack() as es:
        fw = FW(nc, es)
        sb = lambda n, shp, dt: es.enter_context(nc.sbuf_tensor(U(n), shp, dt))
        a = [sb(f"ca{i}", [128, D], F32) for i in range(2)]; ta = [Tok("a0"), Tok("a1")]
        b = [sb(f"cb{i}", [128, D], F32) for i in range(2)]; tb_ = [Tok("b0"), Tok("b1")]
        c = [sb(f"cc{i}", [128, D], F32) for i in range(2)]; tc = [Tok("c0"), Tok("c1")]
        for n in range(T // 128):
            k = n % 2
            r0 = n * 128
            fw.dma("sp", a[k][:], x3[r0:r0 + 128, :], writes=[ta[k]])
            fw.dma("sp", b[k][:], y0[r0:r0 + 128, :], writes=[tb_[k]])
            fw.dma("sp", c[k][:], y1[r0:r0 + 128, :], writes=[tc[k]])
            fw.op(fw.dve, lambda: nc.vector.tensor_tensor(out=b[k][:], in0=b[k][:], in1=c[k][:], op=ALU.add),
                  reads=[tb_[k], tc[k]], writes=[tb_[k]])
            fw.op(fw.dve, lambda: nc.vector.tensor_tensor(out=a[k][:], in0=a[k][:], in1=b[k][:], op=ALU.add),
                  reads=[ta[k], tb_[k]], writes=[ta[k]])
            fw.dma("sp", out[r0:r0 + 128, :], a[k][:], reads=[ta[k]], writes=[t_out], acc=True)
        fw.final_wait([t_out])
    return nc


def kernel(**inputs):
    from concourse.bass_utils import run_bass_kernel_spmd
    d = {k: np.asarray(v) for k, v in inputs.items()}
    B, L = d['x'].shape[0], d['x'].shape[1]
    T = B * L
    ncA, cnA = build_A(L)
    in_maps = [inputs_A(d, b, L, cnA) for b in range(B)]
    resA = run_bass_kernel_spmd(ncA, in_maps, core_ids=list(range(B))).results
    x3 = np.concatenate([np.asarray(r["x3"]) for r in resA], axis=0)
    hn3 = np.concatenate([np.asarray(r["hn3"]) for r in resA], axis=0)
    gates = np.concatenate([np.asarray(r["gates"]) for r in resA], axis=0)
    sel = gates > 0
    idx = [np.nonzero(sel[:, e])[0] for e in range(8)]
    cap = max(512, int(-(-max(len(i) for i in idx) // 512) * 512))
    ncB, cnB = build_B(cap)
    in_maps = []
    for e in range(8):
        hg = np.zeros((cap, D), dtype=hn3.dtype)
        hg[:len(idx[e])] = hn3[idx[e]]
        rs = np.zeros((cap,), np.float32)
        rs[:len(idx[e])] = gates[idx[e], e]
        m = {"hn_g": hg, "rs": rs, "wg": d['moe_w_gate'][0, e], "wu": d['moe_w_up'][0, e], "wd": d['moe_w_down'][0, e]}
        for k, v in cnB.items():
            m["c_" + k] = v
        in_maps.append(m)
    resB = run_bass_kernel_spmd(ncB, in_maps, core_ids=list(range(8))).results
    y0 = np.zeros((T, D), np.float32)
    y1 = np.zeros((T, D), np.float32)
    rank = np.cumsum(sel, axis=1) - 1
    for e in range(8):
        ye = np.asarray(resB[e]["y"])[:len(idx[e])]
        sl = rank[idx[e], e]
        y0[idx[e][sl == 0]] = ye[sl == 0]
        y1[idx[e][sl >= 1]] = ye[sl >= 1]
    TC = T // 8
    ncC = build_C(TC)
    in_maps = [{"x3": x3[c * TC:(c + 1) * TC], "y0": y0[c * TC:(c + 1) * TC], "y1": y1[c * TC:(c + 1) * TC]} for c in range(8)]
    resC = run_bass_kernel_spmd(ncC, in_maps, core_ids=list(range(8))).results
    out = np.concatenate([np.asarray(r["out"]) for r in resC], axis=0).reshape(B, L, D).astype(np.float32)
    return out
```
